# Optimizing a Trainium2 kernel written in Bass

```python
import jax
import jax.numpy as jnp
from jax import lax
import numpy as np

D_MODEL = 1024
BATCH = 16
SEQ = 2048
DEPTH = 2

Q_BLOCK = 128
LN_EPS = 1e-5
GLA_HEADS = 4
GLA_DK = 48
GLA_DV = 96
GLA_GATE_RANK = 16
GLA_TAU = 16.0
GLA_CHUNK = 64
DSA_HEADS = 5
DSA_DH = 64
DSA_LATENT = 128
IDX_HEADS = 8
IDX_DIM = 64
DSA_TOPK_MAX = 256
SB_HEADS = 5
SB_DH = 64
GLA_WIDTH = GLA_HEADS * GLA_DV
DSA_WIDTH = DSA_HEADS * DSA_DH
SB_WIDTH = SB_HEADS * SB_DH
MIX_WIDTH = GLA_WIDTH + DSA_WIDTH + SB_WIDTH
MEM_TOKENS = 256
MEM_HEADS = 4
MEM_DH = D_MODEL // MEM_HEADS
D_FF = 4 * D_MODEL
ALPHA = (2.0 * DEPTH) ** 0.25
BETA = (8.0 * DEPTH) ** -0.25
ALIBI_SLOPES = tuple(2.0 ** (-8.0 * (i + 1) / DSA_HEADS) for i in range(DSA_HEADS))
COL_SIZES = (
    GLA_HEADS * GLA_DK,
    GLA_HEADS * GLA_DK,
    GLA_WIDTH,
    GLA_GATE_RANK,
    GLA_WIDTH,
    DSA_WIDTH,
    DSA_DH,
    DSA_LATENT,
    IDX_HEADS * IDX_DIM,
    IDX_DIM,
    IDX_HEADS,
    SB_WIDTH,
    SB_WIDTH,
    SB_WIDTH,
)
P_IN = sum(COL_SIZES)
SPLIT_POINTS = tuple(int(v) for v in np.cumsum(COL_SIZES)[:-1])

kernel_name = 'hybrid_gla_dsa_stickbreaking_deepnorm'


def layer_norm(x, g, b):
    xf = x.astype(jnp.float32)
    mu = jnp.mean(xf, axis=-1, keepdims=True)
    var = jnp.mean(jnp.square(xf - mu), axis=-1, keepdims=True)
    return ((xf - mu) * lax.rsqrt(var + LN_EPS) * g + b).astype(x.dtype)


def gla_mixer(q, k, v, gate_lr, g, gate_w2, gate_b, norm_g):
    B, L = q.shape[0], q.shape[1]
    H, dk, dv, C = GLA_HEADS, GLA_DK, GLA_DV, GLA_CHUNK
    N = L // C
    f32 = jnp.float32
    log_a = jax.nn.log_sigmoid((gate_lr @ gate_w2 + gate_b).astype(f32)) / GLA_TAU
    log_a = log_a.reshape(B, N, C, H, dk)
    qf = q.astype(f32).reshape(B, N, C, H, dk) * (dk ** -0.5)
    kf = k.astype(f32).reshape(B, N, C, H, dk)
    vf = v.astype(f32).reshape(B, N, C, H, dv)
    bcum = jnp.cumsum(log_a, axis=2)
    b_last = bcum[:, :, -1]
    q_t = qf * jnp.exp(bcum)
    k_t = kf * jnp.exp(-bcum)
    k_end = kf * jnp.exp(b_last[:, :, None] - bcum)
    causal = jnp.tril(jnp.ones((C, C), dtype=bool))
    scores = jnp.einsum('bnchd,bnshd->bnhcs', q_t, k_t)
    scores = jnp.where(causal, scores, 0.0)
    o_intra = jnp.einsum('bnhcs,bnshv->bnchv', scores, vf)
    dS = jnp.einsum('bnchd,bnchv->nbhdv', k_end, vf)
    decay = jnp.exp(jnp.moveaxis(b_last, 1, 0))

    def step(S, inp):
        dS_n, dec_n = inp
        return dec_n[..., None] * S + dS_n, S

    S0 = jnp.zeros((B, H, dk, dv), f32)
    _, S_prev = lax.scan(step, S0, (dS, decay))
    o_inter = jnp.einsum('bnchd,nbhdv->bnchv', q_t, S_prev)
    o = (o_intra + o_inter).reshape(B, L, H, dv)
    o = o * lax.rsqrt(jnp.mean(jnp.square(o), axis=-1, keepdims=True) + LN_EPS) * norm_g
    o = o * jax.nn.silu(g.astype(f32))
    return o.reshape(B, L, H * dv).astype(q.dtype)


def dsa_mixer(q, k_sh, v_lat, iq, ik, iw, w_uv):
    B, L = q.shape[0], q.shape[1]
    f32 = jnp.float32
    topk = min(DSA_TOPK_MAX, L // 4)
    nb = L // Q_BLOCK
    key_pos = jnp.arange(L)
    kv = jnp.concatenate([k_sh, v_lat], axis=-1)
    slopes = jnp.asarray(ALIBI_SLOPES, f32)
    idx_scale = (IDX_HEADS ** -0.5) * (IDX_DIM ** -0.5)

    def block(i):
        t0 = i * Q_BLOCK
        qb = lax.dynamic_slice_in_dim(q, t0, Q_BLOCK, axis=1)
        iqb = lax.dynamic_slice_in_dim(iq, t0, Q_BLOCK, axis=1)
        iwb = lax.dynamic_slice_in_dim(iw, t0, Q_BLOCK, axis=1)
        qpos = t0 + jnp.arange(Q_BLOCK)
        causal = key_pos[None, :] <= qpos[:, None]
        rel = jax.nn.relu(jnp.einsum('bthd,bsd->bths', iqb, ik).astype(f32))
        isc = jnp.einsum('bths,bth->bts', rel, iwb.astype(f32)) * idx_scale
        isc = jnp.where(causal[None], isc, -jnp.inf)
        _, idx = lax.top_k(isc, topk)
        valid = idx <= qpos[None, :, None]
        sel = jax.vmap(lambda a, j: a[j])(kv, idx)
        k_sel = sel[..., :DSA_DH]
        v_sel = sel[..., DSA_DH:]
        dist = (qpos[None, :, None] - idx).astype(f32)
        s = jnp.einsum('bthd,btkd->bhtk', qb, k_sel).astype(f32) * (DSA_DH ** -0.5)
        s = s - slopes[None, :, None, None] * dist[:, None]
        s = jnp.where(valid[:, None], s, -jnp.inf)
        p = jax.nn.softmax(s, axis=-1).astype(v_sel.dtype)
        o_lat = jnp.einsum('bhtk,btkc->bthc', p, v_sel)
        o = jnp.einsum('bthc,hcd->bthd', o_lat, w_uv)
        return o.reshape(B, Q_BLOCK, DSA_WIDTH)

    out = lax.map(block, jnp.arange(nb))
    return jnp.moveaxis(out, 0, 1).reshape(B, L, DSA_WIDTH)


def sb_mixer(q, k, v):
    B, L = q.shape[0], q.shape[1]
    f32 = jnp.float32
    nb = L // Q_BLOCK
    key_pos = jnp.arange(L)

    def block(i):
        t0 = i * Q_BLOCK
        qb = lax.dynamic_slice_in_dim(q, t0, Q_BLOCK, axis=1)
        qpos = t0 + jnp.arange(Q_BLOCK)
        strict = key_pos[None, :] < qpos[:, None]
        z = jnp.einsum('bthd,bshd->bhts', qb, k).astype(f32) * (SB_DH ** -0.5)
        log_1mb = jnp.where(strict, jax.nn.log_sigmoid(-z), 0.0)
        rev = lax.cumsum(log_1mb, axis=3, reverse=True)
        suffix = jnp.concatenate([rev[..., 1:], jnp.zeros_like(rev[..., :1])], axis=-1)
        w = jnp.where(strict, jnp.exp(jax.nn.log_sigmoid(z) + suffix), 0.0)
        o = jnp.einsum('bhts,bshd->bthd', w.astype(v.dtype), v)
        return o.reshape(B, Q_BLOCK, SB_WIDTH)

    out = lax.map(block, jnp.arange(nb))
    return jnp.moveaxis(out, 0, 1).reshape(B, L, SB_WIDTH)


def hybrid_mixer(h, w_in, gate_w2, gate_b, gla_norm_g, w_uv, w_out):
    B, L = h.shape[0], h.shape[1]
    proj = h @ w_in
    (gq, gk, gv, glr, gg, dq, dk, dv, iq, ik, iw, sq, sk, sv) = jnp.split(proj, SPLIT_POINTS, axis=-1)
    o_gla = gla_mixer(gq.reshape(B, L, GLA_HEADS, GLA_DK), gk.reshape(B, L, GLA_HEADS, GLA_DK),
                      gv.reshape(B, L, GLA_HEADS, GLA_DV), glr, gg.reshape(B, L, GLA_HEADS, GLA_DV),
                      gate_w2, gate_b, gla_norm_g)
    o_dsa = dsa_mixer(dq.reshape(B, L, DSA_HEADS, DSA_DH), dk, dv,
                      iq.reshape(B, L, IDX_HEADS, IDX_DIM), ik, iw, w_uv)
    o_sb = sb_mixer(sq.reshape(B, L, SB_HEADS, SB_DH), sk.reshape(B, L, SB_HEADS, SB_DH),
                    sv.reshape(B, L, SB_HEADS, SB_DH))
    o = jnp.concatenate([o_gla, o_dsa, o_sb], axis=-1)
    return o @ w_out


def mem_attn(h, mem, w_q, w_kv, w_o):
    B, L = h.shape[0], h.shape[1]
    M = mem.shape[1]
    q = (h @ w_q).reshape(B, L, MEM_HEADS, MEM_DH)
    kv = mem @ w_kv
    k = kv[..., :D_MODEL].reshape(B, M, MEM_HEADS, MEM_DH)
    v = kv[..., D_MODEL:].reshape(B, M, MEM_HEADS, MEM_DH)
    s = jnp.einsum('blhd,bmhd->bhlm', q, k).astype(jnp.float32) * (MEM_DH ** -0.5)
    p = jax.nn.softmax(s, axis=-1).astype(v.dtype)
    o = jnp.einsum('bhlm,bmhd->blhd', p, v).reshape(B, L, D_MODEL)
    return o @ w_o


def sq_relu_mlp(h, w_up, b_up, w_down, b_down):
    u = jax.nn.relu(h @ w_up + b_up)
    return (u * u) @ w_down + b_down


def setup_inputs(seed: int = 0) -> dict:
    key = jax.random.key(seed)
    ks = jax.random.split(key, 26)
    f32 = jnp.float32

    def nrm(k, shape, scale):
        return jax.random.normal(k, shape, f32) * scale

    def gain(k, shape):
        return 1.0 + 0.02 * jax.random.normal(k, shape, f32)

    return {
        'x': nrm(ks[0], (BATCH, SEQ, D_MODEL), 1.0),
        'mem': nrm(ks[1], (BATCH, MEM_TOKENS, D_MODEL), 1.0),
        'ln_in_g': gain(ks[2], (D_MODEL,)),
        'ln_in_b': nrm(ks[3], (D_MODEL,), 0.02),
        'w_in': nrm(ks[4], (DEPTH, D_MODEL, P_IN), D_MODEL ** -0.5),
        'gla_gate_w2': nrm(ks[5], (DEPTH, GLA_GATE_RANK, GLA_HEADS * GLA_DK), GLA_GATE_RANK ** -0.5),
        'gla_gate_b': nrm(ks[6], (DEPTH, GLA_HEADS * GLA_DK), 0.1),
        'gla_norm_g': gain(ks[7], (DEPTH, GLA_DV)),
        'dsa_w_uv': nrm(ks[8], (DEPTH, DSA_HEADS, DSA_LATENT, DSA_DH), DSA_LATENT ** -0.5),
        'w_out': nrm(ks[9], (DEPTH, MIX_WIDTH, D_MODEL), BETA * MIX_WIDTH ** -0.5),
        'ln_mix_g': gain(ks[10], (DEPTH, D_MODEL)),
        'ln_mix_b': nrm(ks[11], (DEPTH, D_MODEL), 0.02),
        'w_mem_q': nrm(ks[12], (DEPTH, D_MODEL, D_MODEL), D_MODEL ** -0.5),
        'w_mem_kv': nrm(ks[13], (DEPTH, D_MODEL, 2 * D_MODEL), D_MODEL ** -0.5),
        'w_mem_o': nrm(ks[14], (DEPTH, D_MODEL, D_MODEL), BETA * D_MODEL ** -0.5),
        'ln_mem_g': gain(ks[15], (DEPTH, D_MODEL)),
        'ln_mem_b': nrm(ks[16], (DEPTH, D_MODEL), 0.02),
        'w_up': nrm(ks[17], (DEPTH, D_MODEL, D_FF), D_MODEL ** -0.5),
        'b_up': nrm(ks[18], (DEPTH, D_FF), 0.02),
        'w_down': nrm(ks[19], (DEPTH, D_FF, D_MODEL), BETA * D_FF ** -0.5),
        'b_down': nrm(ks[20], (DEPTH, D_MODEL), 0.02),
        'ln_ffn_g': gain(ks[21], (DEPTH, D_MODEL)),
        'ln_ffn_b': nrm(ks[22], (DEPTH, D_MODEL), 0.02),
    }


def reference(x, mem, ln_in_g, ln_in_b, w_in, gla_gate_w2, gla_gate_b, gla_norm_g, dsa_w_uv,
              w_out, ln_mix_g, ln_mix_b, w_mem_q, w_mem_kv, w_mem_o, ln_mem_g, ln_mem_b,
              w_up, b_up, w_down, b_down, ln_ffn_g, ln_ffn_b):
    h = layer_norm(x, ln_in_g, ln_in_b)
    for l in range(DEPTH):
        f = hybrid_mixer(h, w_in[l], gla_gate_w2[l], gla_gate_b[l], gla_norm_g[l], dsa_w_uv[l], w_out[l])
        h = layer_norm(ALPHA * h + f, ln_mix_g[l], ln_mix_b[l])
        f = mem_attn(h, mem, w_mem_q[l], w_mem_kv[l], w_mem_o[l])
        h = layer_norm(ALPHA * h + f, ln_mem_g[l], ln_mem_b[l])
        f = sq_relu_mlp(h, w_up[l], b_up[l], w_down[l], b_down[l])
        h = layer_norm(ALPHA * h + f, ln_ffn_g[l], ln_ffn_b[l])
    return h
```

```python
import numpy as np
from contextlib import ExitStack
import concourse.bass as bass
import concourse.mybir as mybir
from concourse.bass_utils import run_bass_kernel_spmd

F32 = mybir.dt.float32
BF16 = mybir.dt.bfloat16
AF = mybir.ActivationFunctionType
ALU = mybir.AluOpType

D = 1024
SEQ = 2048
NSEQ = 2
TOK = NSEQ * SEQ
DEPTH = 2
PIN = 3224
DFF = 4096
MEMT = 256
ALPHA = (2.0 * DEPTH) ** 0.25
LN_EPS = 1e-5
SLOPES = [2.0 ** (-8.0 * (i + 1) / 5) for i in range(5)]
NEG = -1.0e30

C_ID, C_TRI, C_ONE, C_MS, C_BT, C_BO, C_NEGI, C_CS, C_DIST = 0, 128, 256, 384, 512, 640, 768, 896, 1024
CST_N = 1024 + 2048


def make_consts():
    c = np.zeros((128, CST_N), np.float32)
    p = np.arange(128)[:, None]
    q = np.arange(128)[None, :]
    c[:, C_ID:C_ID + 128] = (p == q)
    c[:, C_TRI:C_TRI + 128] = (p >= q)
    c[:, C_ONE:C_ONE + 128] = 1.0
    c[:, C_MS:C_MS + 128] = (p < q)
    c[:, C_BT:C_BT + 128] = (p <= q) & ((p // 64) == (q // 64))
    c[:, C_BO:C_BO + 128] = ((p // 64) == (q // 64))
    c[:, C_NEGI:C_NEGI + 128] = np.where(q <= p, 0.0, NEG)
    c[:, C_CS + 0] = (np.arange(128) < 64)
    c[:, C_CS + 1] = (np.arange(128) >= 64)
    m = np.arange(2048)[None, :]
    c[:, C_DIST:C_DIST + 2048] = m - p
    return c


class Prog:
    ENGS = ['pe', 'act', 'dve', 'pool', 'sp']

    def __init__(self, nc):
        self.nc = nc
        self.ops = []
        self.res = {}
        self.dma_cnt = {}
        self.last_dma = {}
        self.last_op = {}

    def add(self, eng, fn, R=(), W=(), dma=None, extra=()):
        i = len(self.ops)
        deps = {}
        for k in R:
            st = self.res.get(k)
            if st is not None and st[0] is not None:
                deps[st[0]] = deps.get(st[0], 0) | 1
        for k in W:
            st = self.res.get(k)
            if st is not None:
                if st[0] is not None:
                    deps[st[0]] = deps.get(st[0], 0) | 2
                for r in st[1]:
                    deps[r] = deps.get(r, 0) | 2
        for e in extra:
            deps[e] = deps.get(e, 0) | 1
        for k in R:
            st = self.res.setdefault(k, [None, []])
            st[1].append(i)
        for k in W:
            st = self.res.setdefault(k, [None, []])
            st[0] = i
            st[1] = []
        deps.pop(i, None)
        op = dict(id=i, eng=eng, fn=fn, deps=deps, dma=dma, sig=False, val=0)
        if dma is not None:
            c = self.dma_cnt.get(dma, 0) + 16
            self.dma_cnt[dma] = c
            op['val'] = c
            self.last_dma[dma] = i
        else:
            self.last_op[eng] = i
        self.ops.append(op)
        return i

    def barrier(self):
        ex = list(self.last_dma.values()) + list(self.last_op.values())
        x = self.add('sp', lambda e: e.nop(), extra=ex)
        for e in ['pe', 'act', 'dve', 'pool']:
            self.add(e, lambda en: en.nop(), extra=[x])
        self.res = {}

    def emit(self):
        nc = self.nc
        ops = self.ops
        for op in ops:
            need = []
            for pid, kind in op['deps'].items():
                p = ops[pid]
                if p['dma'] is None:
                    if op['dma'] is None and p['eng'] == op['eng']:
                        if op['eng'] in ('pe', 'sp'):
                            continue
                    p['sig'] = True
                need.append(pid)
            op['need'] = need
        cnt = {e: 0 for e in self.ENGS}
        for op in ops:
            if op['dma'] is None and op['sig']:
                cnt[op['eng']] += 1
                op['val'] = cnt[op['eng']]
        with ExitStack() as es:
            esem = {e: es.enter_context(nc.semaphore("s_" + e)) for e in self.ENGS}
            dsem = {k: es.enter_context(nc.semaphore("d_%d" % i)) for i, k in enumerate(self.dma_cnt)}
            block = es.enter_context(nc.Block())

            def run(e, eobj):
                waited = {}
                for op in ops:
                    if op['eng'] != e:
                        continue
                    waits = {}
                    for pid in op['need']:
                        p = ops[pid]
                        s = dsem[p['dma']] if p['dma'] is not None else esem[p['eng']]
                        v = p['val']
                        if waited.get(s.name, 0) >= v:
                            continue
                        if waits.get(s.name, (None, 0))[1] < v:
                            waits[s.name] = (s, v)
                    wl = list(waits.values())
                    for s, v in wl[1:]:
                        eobj.wait_ge(s, v)
                        waited[s.name] = v
                    ins = op['fn'](eobj)
                    if wl:
                        ins._wait_ge(wl[0][0], wl[0][1])
                        waited[wl[0][0].name] = wl[0][1]
                    if op['dma'] is not None:
                        ins.then_inc(dsem[op['dma']], 16)
                    elif op['sig']:
                        ins.then_inc(esem[e], 1)
                if e == 'sp':
                    for k, c in self.dma_cnt.items():
                        if waited.get(dsem[k].name, 0) < c:
                            eobj.wait_ge(dsem[k], c)

            block.tensor(lambda t: run('pe', t))
            block.scalar(lambda t: run('act', t))
            block.vector(lambda t: run('dve', t))
            block.gpsimd(lambda t: run('pool', t))
            block.sync(lambda t: run('sp', t))


class Arena:
    def __init__(self, nc, nwords):
        self.t = nc.alloc_sbuf_tensor("arena", [128, nwords], F32)
        self.n = nwords
        self.top = 0
        self.marks = []
        self.peak = 0

    def _alloc(self, nbytes):
        w = (nbytes + 3) // 4
        w = (w + 7) // 8 * 8
        off = self.top
        self.top += w
        self.peak = max(self.peak, self.top)
        assert self.top <= self.n, ("SBUF arena overflow", self.top * 4)
        return off

    def f32(self, cols):
        off = self._alloc(cols * 4)
        return self.t[:, off:off + cols]

    def bf16(self, cols):
        off = self._alloc(cols * 2)
        return self.t[:, off:off + (cols + 1) // 2].bitcast(BF16)

    def mark(self):
        self.marks.append(self.top)

    def release(self):
        self.top = self.marks.pop()


def build(dbg=None, phases=None):
    nc = bass.Bass("TRN2", target_bir_lowering=False)
    try:
        nc.allow_low_precision("bf16 matmul operands with fp32 accumulation")
    except Exception:
        pass
    try:
        nc.allow_non_contiguous_dma("small strided parameter loads")
    except Exception:
        pass
    P = Prog(nc)
    A = Arena(nc, 52000)
    ps = nc.alloc_psum_tensor("ps", [128, 4096], F32)

    def dt(name, shape, dtype=F32, kind="ExternalInput"):
        return nc.dram_tensor(name, shape, dtype, kind=kind).ap()

    x_d = dt("x", [TOK, D])
    mem_d = dt("mem", [NSEQ * MEMT, D])
    cst_d = dt("cst", [128, CST_N])
    ln_in_g = dt("ln_in_g", [1, D]); ln_in_b = dt("ln_in_b", [1, D])
    w_in = dt("w_in", [DEPTH, D, PIN])
    gate_w2 = dt("gla_gate_w2", [DEPTH, 16, 192]); gate_b = dt("gla_gate_b", [DEPTH, 192])
    norm_g = dt("gla_norm_g", [DEPTH, 96])
    w_uv = dt("dsa_w_uv", [DEPTH, 5, 128, 64])
    w_out = dt("w_out", [DEPTH, D, D])
    ln_mix_g = dt("ln_mix_g", [DEPTH, D]); ln_mix_b = dt("ln_mix_b", [DEPTH, D])
    w_mem_q = dt("w_mem_q", [DEPTH, D, D]); w_mem_kv = dt("w_mem_kv", [DEPTH, D, 2 * D]); w_mem_o = dt("w_mem_o", [DEPTH, D, D])
    ln_mem_g = dt("ln_mem_g", [DEPTH, D]); ln_mem_b = dt("ln_mem_b", [DEPTH, D])
    w_up = dt("w_up", [DEPTH, D, DFF]); b_up = dt("b_up", [DEPTH, DFF])
    w_down = dt("w_down", [DEPTH, DFF, D]); b_down = dt("b_down", [DEPTH, D])
    ln_ffn_g = dt("ln_ffn_g", [DEPTH, D]); ln_ffn_b = dt("ln_ffn_b", [DEPTH, D])
    out_d = dt("out", [TOK, D], kind="ExternalOutput")
    skind = "ExternalOutput" if dbg else "Internal"
    h32 = dt("h32", [TOK, D], kind=skind)
    hT16 = dt("hT16", [128, 8, TOK], BF16, kind=skind)
    oT16 = dt("oT16", [128, 8, TOK], BF16, kind=skind)

    def bank(b, n=512):
        return ps[:, b * 512:b * 512 + n]

    def bankbf(b):
        return ps[:, b * 512:(b + 1) * 512].bitcast(BF16)

    def MM(out, lhsT, rhs, start, stop, R, W):
        P.add('pe', lambda e: e.matmul(out, lhsT, rhs, start=start, stop=stop), R, W)

    def TR(out, in_, ident, R, W):
        P.add('pe', lambda e: e.transpose(out, in_, ident), R, W)

    def ACT(out, in_, func, R, W, bias=None, scale=1.0, accum=None):
        kw = {}
        if bias is not None:
            kw['bias'] = bias
        if accum is not None:
            kw['accum_out'] = accum
        P.add('act', lambda e: e.activation(out, in_, func, scale=scale, **kw), R, W)

    def DVE(fn, R, W):
        P.add('dve', fn, R, W)

    def POOL(fn, R, W):
        P.add('pool', fn, R, W)

    def DMA(out, in_, key, R, W, q='sp', slow=False):
        if slow:
            P.add(q, lambda e: e.dma_start(out=out, in_=in_, allow_slow_non_contiguous=True), R, W, dma=key)
        else:
            P.add(q, lambda e: e.dma_start(out=out, in_=in_), R, W, dma=key)

    def bcast_row(ap_row, n):
        return ap_row.to_broadcast([128, n])

    cst = A.f32(1024)
    DMA(cst, cst_d[:, 0:1024], 'cst', ['cst_d'], ['cst'])
    cbf = A.bf16(1024)
    DVE(lambda e: e.tensor_copy(cbf, cst[:, 0:1024]), ['cst'], ['cbf'])
    ident_bf = cbf[:, C_ID:C_ID + 128]
    ones_bf = cbf[:, C_ONE:C_ONE + 128]
    tri_bf = cbf[:, C_TRI:C_TRI + 128]
    kc = A.f32(8)
    POOL(lambda e: e.memset(kc[:, 0:1], LN_EPS), [], ['kc'])
    POOL(lambda e: e.memset(kc[:, 1:2], 1.0), [], ['kc'])
    eps_t = kc[:, 0:1]
    one_t = kc[:, 1:2]
    A.mark()

    class LNState:
        pass

    def ln_setup(g_row, b_row, tag):
        st = LNState()
        st.g = A.f32(D); st.b = A.f32(D)
        DMA(st.g, bcast_row(g_row, D), ('lnp', 'g'), [], ['ln_g'])
        DMA(st.b, bcast_row(b_row, D), ('lnp', 'b'), [], ['ln_b'])
        st.ybf = [A.bf16(D) for _ in range(2)]
        st.stats = [A.f32(16) for _ in range(2)]
        st.hTb = [A.bf16(8 * 512) for _ in range(2)]
        st.n = 0
        return st

    def ln_tile(st, z, zkey, tok0, final=False):
        i = st.n; st.n += 1
        s = i % 2
        y = z; ybf = st.ybf[s]; stt = st.stats[s]
        ky, kyb, kst = zkey, ('ln_ybf', s), ('ln_st', s)
        DVE(lambda e: e.bn_stats(stt[:, 0:6], z[:, 0:512]), [zkey], [kst])
        DVE(lambda e: e.bn_stats(stt[:, 6:12], z[:, 512:1024]), [zkey], [kst])
        DVE(lambda e: e.bn_aggr(stt[:, 12:14], stt[:, 0:12].rearrange("p (a b) -> p a b", a=2)), [kst], [kst])
        ACT(stt[:, 15:16], stt[:, 13:14], AF.Ln, [kst, 'kc'], [kst], bias=eps_t)
        ACT(stt[:, 14:15], stt[:, 15:16], AF.Exp, [kst], [kst], scale=-0.5)
        DVE(lambda e: e.tensor_scalar(y, z, stt[:, 12:13], stt[:, 14:15], ALU.subtract, ALU.mult), [kst], [ky])
        POOL(lambda e: e.tensor_tensor(y, y, st.g, ALU.mult), [ky, 'ln_g'], [ky])
        POOL(lambda e: e.tensor_tensor(y, y, st.b, ALU.add), [ky, 'ln_b'], [ky])
        if final:
            DMA(out_d[tok0:tok0 + 128, :], y, ('st_y', zkey), [ky], [('out', tok0)])
            return
        DMA(h32[tok0:tok0 + 128, :], y, ('st_y', zkey), [ky], [('h32', tok0)])
        ACT(ybf, y, AF.Copy, [ky], [kyb])
        q = (tok0 // 128) % 4
        blk = tok0 // 512
        hs = blk % 2
        hTb = st.hTb[hs].rearrange("p (k t) -> p k t", k=8)
        pb = 6 + (i % 2)
        pv = bankbf(pb)
        for k in range(8):
            TR(pv[:, k * 128:(k + 1) * 128], ybf[:, k * 128:(k + 1) * 128], ident_bf, [kyb, 'cbf'], [('ps', pb)])
        DVE(lambda e: e.tensor_copy(hTb[:, :, q * 128:(q + 1) * 128], pv.rearrange("p (k t) -> p k t", k=8)),
            [('ps', pb)], [('hTb', hs)])
        if q == 3:
            DMA(hT16[:, :, blk * 512:(blk + 1) * 512], hTb, ('st_hT', hs), [('hTb', hs)], [('hT16', blk)])

    def load_w(dst3, src_fn, nk, ncols, stage, tag, wkey, engs=('pool', 'act')):
        for k in range(nk):
            s = k % 2
            sk = ('wst', tag, s)
            DMA(stage[s][:, 0:ncols], src_fn(k), ('wst', tag, s), [], [sk])
            eng = engs[k % len(engs)]
            if eng == 'act':
                ACT(dst3[:, k, :], stage[s][:, 0:ncols], AF.Copy, [sk], [wkey])
            elif eng == 'pool':
                POOL(lambda e, k=k, s=s: e.tensor_copy(dst3[:, k, :], stage[s][:, 0:ncols]), [sk], [wkey])
            else:
                DVE(lambda e, k=k, s=s: e.tensor_copy(dst3[:, k, :], stage[s][:, 0:ncols]), [sk], [wkey])

    def phase_ln_in():
        A.mark()
        st = ln_setup(ln_in_g, ln_in_b, 'in')
        xt = [A.f32(D) for _ in range(2)]
        for i in range(TOK // 128):
            s = i % 2
            DMA(xt[s], x_d[i * 128:(i + 1) * 128, :], ('ld_x', s), [], [('xt', s)])
            ln_tile(st, xt[s], ('xt', s), i * 128)
        A.release()
        P.barrier()

    def res_ln_tiles(st, lhs_fn, lhs_keys, wbf, wkey, nk, tok0_blk, hbuf, zbuf, bias_tile=None, final=False):
        for tt in range(4):
            tok0 = tok0_blk + tt * 128
            s = tt % 2
            DMA(hbuf[s], h32[tok0:tok0 + 128, :], ('ld_h', s), [('h32', tok0)], [('hb', s)])
            for nh in range(2):
                pb = 4 + nh
                for k in range(nk):
                    MM(bank(pb), lhs_fn(k, tt), wbf[:, k, nh * 512:(nh + 1) * 512], k == 0, k == nk - 1,
                       lhs_keys + [wkey], [('ps', pb)])
                DVE(lambda e, s=s, nh=nh, pb=pb: e.scalar_tensor_tensor(
                    zbuf[s][:, nh * 512:(nh + 1) * 512], hbuf[s][:, nh * 512:(nh + 1) * 512], ALPHA, bank(pb),
                    ALU.mult, ALU.add), [('hb', s), ('ps', pb)], [('zb', s)])
            if bias_tile is not None:
                POOL(lambda e, s=s: e.tensor_tensor(zbuf[s], zbuf[s], bias_tile, ALU.add), [('zb', s), 'bias_t'], [('zb', s)])
            ln_tile(st, zbuf[s], ('zb', s), tok0, final=final)

    def phase_wout(l):
        A.mark()
        st = ln_setup(ln_mix_g[l:l + 1, :], ln_mix_b[l:l + 1, :], 'mix')
        stage = [A.f32(1024) for _ in range(2)]
        wbf = A.bf16(8 * 1024).rearrange("p (k n) -> p k n", k=8)
        load_w(wbf, lambda k: w_out[l, k * 128:(k + 1) * 128, :], 8, 1024, stage, 'wo', 'wbf')
        src = [A.bf16(8 * 512).rearrange("p (k t) -> p k t", k=8) for _ in range(2)]
        hbuf = [A.f32(D) for _ in range(2)]
        zbuf = [A.f32(D) for _ in range(2)]
        for blk in range(TOK // 512):
            s = blk % 2
            DMA(src[s], oT16[:, :, blk * 512:(blk + 1) * 512], ('ld_src', s), [('oT16', blk)], [('src', s)])
            res_ln_tiles(st, lambda k, tt, s=s: src[s][:, k, tt * 128:(tt + 1) * 128], [('src', s)], wbf, 'wbf', 8,
                         blk * 512, hbuf, zbuf)
        A.release()
        P.barrier()

    def phase_mlp(l, final):
        A.mark()
        st = ln_setup(ln_ffn_g[l:l + 1, :], ln_ffn_b[l:l + 1, :], 'ffn')
        bdt = A.f32(D)
        DMA(bdt, bcast_row(b_down[l:l + 1, :], D), ('lnp', 'bd'), [], ['bias_t'])
        bupT = A.f32(32)
        DMA(bupT, b_up[l].rearrange("(j p) -> p j", p=128), ('lnp', 'bu'), [], ['bupT'], slow=True)
        stage = [A.f32(1024) for _ in range(2)]
        wdn = A.bf16(32 * 1024).rearrange("p (k n) -> p k n", k=32)
        load_w(wdn, lambda k: w_down[l, k * 128:(k + 1) * 128, :], 32, 1024, stage, 'wd', 'wdn')
        ustage = [A.f32(512) for _ in range(2)]
        wup = [A.bf16(8 * 512).rearrange("p (k n) -> p k n", k=8) for _ in range(2)]
        src1 = A.bf16(8 * 512).rearrange("p (k t) -> p k t", k=8)
        src = [src1, src1]
        uT = A.bf16(32 * 512).rearrange("p (j t) -> p j t", j=32)
        rbuf = [A.f32(512) for _ in range(2)]
        hbuf = [A.f32(D) for _ in range(2)]
        zbuf = [A.f32(D) for _ in range(2)]
        gi = 0
        for blk in range(TOK // 512):
            s = 0
            DMA(src[s], hT16[:, :, blk * 512:(blk + 1) * 512], ('ld_src', s), [('hT16', blk)], [('src', s)])
            for g in range(8):
                ws = gi % 2; gi += 1
                load_w(wup[ws], lambda k, g=g: w_up[l, k * 128:(k + 1) * 128, g * 512:(g + 1) * 512], 8, 512,
                       ustage, 'wu', ('wup', ws))
                for jj in range(4):
                    j = g * 4 + jj
                    pb = j % 4
                    for k in range(8):
                        MM(bank(pb), wup[ws][:, k, jj * 128:(jj + 1) * 128], src[s][:, k, :], k == 0, k == 7,
                           [('wup', ws), ('src', s)], [('ps', pb)])
                    rs = j % 2
                    ACT(rbuf[rs], bank(pb), AF.Relu, [('ps', pb), 'bupT'], [('rb', rs)], bias=bupT[:, j:j + 1])
                    POOL(lambda e, rs=rs, j=j: e.tensor_tensor(uT[:, j, :], rbuf[rs], rbuf[rs], ALU.mult),
                         [('rb', rs)], ['uT'])
            res_ln_tiles(st, lambda k, tt: uT[:, k, tt * 128:(tt + 1) * 128], ['uT'], wdn, 'wdn', 32,
                         blk * 512, hbuf, zbuf, bias_tile=bdt, final=final)
        A.release()
        P.barrier()

    def phase_mem(l):
        A.mark()
        st = ln_setup(ln_mem_g[l:l + 1, :], ln_mem_b[l:l + 1, :], 'mem')
        stage = [A.f32(1024) for _ in range(2)]
        wkv = A.bf16(8 * 2048).rearrange("p (k n) -> p k n", k=8)
        load_w(wkv[:, :, 0:1024], lambda k: w_mem_kv[l, k * 128:(k + 1) * 128, 0:1024], 8, 1024, stage, 'wkv', 'wkv')
        load_w(wkv[:, :, 1024:2048], lambda k: w_mem_kv[l, k * 128:(k + 1) * 128, 1024:2048], 8, 1024, stage, 'wkv', 'wkv')
        wq = A.bf16(8 * 1024).rearrange("p (k n) -> p k n", k=8)
        load_w(wq, lambda k: w_mem_q[l, k * 128:(k + 1) * 128, :], 8, 1024, stage, 'wkv', 'wq')
        wo = A.bf16(8 * 1024).rearrange("p (k n) -> p k n", k=8)
        load_w(wo, lambda k: w_mem_o[l, k * 128:(k + 1) * 128, :], 8, 1024, stage, 'wkv', 'wo')
        hbuf = [A.f32(D) for _ in range(2)]
        memf = hbuf
        membf = [A.bf16(D) for _ in range(2)]
        memT = A.bf16(8 * 256).rearrange("p (k m) -> p k m", k=8)
        kT = A.bf16(8 * 256).rearrange("p (c m) -> p c m", c=8)
        vv = A.bf16(2 * 1024).rearrange("p (m n) -> p m n", m=2)
        src = [A.bf16(8 * 512).rearrange("p (k t) -> p k t", k=8) for _ in range(2)]
        qT = A.bf16(8 * 512).rearrange("p (c t) -> p c t", c=8)
        E = [A.bf16(512) for _ in range(2)]
        rden = A.f32(512)
        omT = A.bf16(8 * 512).rearrange("p (c t) -> p c t", c=8)
        zbuf = [A.f32(D) for _ in range(2)]
        for b in range(NSEQ):
            for mt in range(2):
                DMA(memf[mt], mem_d[b * MEMT + mt * 128:b * MEMT + (mt + 1) * 128, :], ('ld_mem', mt), [], [('hb', mt)])
                ACT(membf[mt], memf[mt], AF.Copy, [('hb', mt)], [('membf', mt)])
                pv = bankbf(mt)
                for k in range(8):
                    TR(pv[:, k * 128:(k + 1) * 128], membf[mt][:, k * 128:(k + 1) * 128], ident_bf,
                       [('membf', mt), 'cbf'], [('ps', mt)])
                DVE(lambda e, mt=mt, pv=pv: e.tensor_copy(memT[:, :, mt * 128:(mt + 1) * 128],
                                                       pv.rearrange("p (k t) -> p k t", k=8)),
                    [('ps', mt)], ['memT'])
            for c in range(8):
                pb = c % 2
                for k in range(8):
                    MM(bank(pb, 256), wkv[:, k, c * 128:(c + 1) * 128], memT[:, k, :], k == 0, k == 7,
                       ['wkv', 'memT'], [('ps', pb)])
                DVE(lambda e, c=c, pb=pb: e.tensor_copy(kT[:, c, :], bank(pb, 256)), [('ps', pb)], ['kT'])
            for mt in range(2):
                for nh in range(2):
                    pb = 2 + nh
                    for k in range(8):
                        MM(bank(pb), memT[:, k, mt * 128:(mt + 1) * 128], wkv[:, k, 1024 + nh * 512:1024 + (nh + 1) * 512],
                           k == 0, k == 7, ['wkv', 'memT'], [('ps', pb)])
                    ACT(vv[:, mt, nh * 512:(nh + 1) * 512], bank(pb), AF.Copy, [('ps', pb)], ['vv'])
            for bb in range(4):
                blk = b * 4 + bb
                s = blk % 2
                DMA(src[s], hT16[:, :, blk * 512:(blk + 1) * 512], ('ld_src', s), [('hT16', blk)], [('src', s)])
                for c in range(8):
                    pb = c % 2
                    for k in range(8):
                        MM(bank(pb), wq[:, k, c * 128:(c + 1) * 128], src[s][:, k, :], k == 0, k == 7,
                           ['wq', ('src', s)], [('ps', pb)])
                    if c % 2 == 0:
                        ACT(qT[:, c, :], bank(pb), AF.Copy, [('ps', pb)], ['qT'])
                    else:
                        DVE(lambda e, c=c, pb=pb: e.tensor_copy(qT[:, c, :], bank(pb)), [('ps', pb)], ['qT'])
                for h in range(4):
                    for mt in range(2):
                        pb = mt
                        for dc in range(2):
                            MM(bank(pb), kT[:, h * 2 + dc, mt * 128:(mt + 1) * 128], qT[:, h * 2 + dc, :], dc == 0, dc == 1,
                               ['kT', 'qT'], [('ps', pb)])
                        ACT(E[mt], bank(pb), AF.Exp, [('ps', pb)], [('E', mt)], scale=1.0 / 16.0)
                    for mt in range(2):
                        MM(bank(2), ones_bf, E[mt], mt == 0, mt == 1, [('E', mt), 'cbf'], [('ps', 2)])
                    DVE(lambda e: e.reciprocal(rden, bank(2)), [('ps', 2)], ['rden'])
                    for dc in range(2):
                        pb = 3
                        c = h * 2 + dc
                        for mt in range(2):
                            MM(bank(pb), vv[:, mt, c * 128:(c + 1) * 128], E[mt], mt == 0, mt == 1,
                               ['vv', ('E', mt)], [('ps', pb)])
                        DVE(lambda e, c=c, pb=pb: e.tensor_tensor(omT[:, c, :], bank(pb), rden, ALU.mult),
                            [('ps', pb), 'rden'], ['omT'])
                res_ln_tiles(st, lambda k, tt: omT[:, k, tt * 128:(tt + 1) * 128], ['omT'], wo, 'wo', 8,
                             blk * 512, hbuf, zbuf)
        A.release()
        P.barrier()

    def phase_mixer_zero():
        A.mark()
        zt = A.bf16(8 * 512)
        POOL(lambda e: e.memset(zt, 0.0), [], ['zt'])
        for blk in range(TOK // 512):
            DMA(oT16[:, :, blk * 512:(blk + 1) * 512], zt.rearrange("p (k t) -> p k t", k=8), 'st_z', ['zt'], [('oT16', blk)])
        A.release()
        P.barrier()

    ctx = dict(nc=nc, P=P, A=A, ps=ps, bank=bank, bankbf=bankbf, MM=MM, TR=TR, ACT=ACT, DVE=DVE, POOL=POOL, DMA=DMA,
               cst=cst, cbf=cbf, cst_d=cst_d, ident_bf=ident_bf, ones_bf=ones_bf, tri_bf=tri_bf, load_w=load_w,
               w_in=w_in, gate_w2=gate_w2, gate_b=gate_b, norm_g=norm_g, w_uv=w_uv, hT16=hT16, oT16=oT16,
               bcast_row=bcast_row, one_t=one_t, eps_t=eps_t, x_d=x_d)

    phases = phases or ['ln_in', 'mix', 'wout', 'mem', 'mlp']
    if 'ln_in' in phases:
        phase_ln_in()
    for l in range(DEPTH):
        if 'mix' in phases:
            phase_mixer(ctx, l, MIX_PARTS)
        elif 'mixzero' in phases:
            phase_mixer_zero()
        if 'wout' in phases:
            phase_wout(l)
        if 'mem' in phases:
            phase_mem(l)
        if 'mlp' in phases:
            phase_mlp(l, final=(l == DEPTH - 1) or bool(dbg and dbg.get('layers', DEPTH) == l + 1))
        if dbg and dbg.get('layers', DEPTH) == l + 1:
            break
    P.emit()
    return nc, A


def phase_mixer(ctx, l, parts=('gla', 'dsa', 'sb')):
    for b in range(NSEQ):
        mixer_seq(ctx, l, b, parts)
        ctx['P'].barrier()


def mixer_seq(ctx, l, b, parts):
    nc = ctx['nc']; P = ctx['P']; A = ctx['A']; ps = ctx['ps']
    bank = ctx['bank']; bankbf = ctx['bankbf']
    MM = ctx['MM']; TR = ctx['TR']; ACT = ctx['ACT']; DVE = ctx['DVE']; POOL = ctx['POOL']; DMA = ctx['DMA']
    cst = ctx['cst']; cbf = ctx['cbf']; ident_bf = ctx['ident_bf']; ones_bf = ctx['ones_bf']; tri_bf = ctx['tri_bf']
    load_w = ctx['load_w']; w_in = ctx['w_in']; hT16 = ctx['hT16']; oT16 = ctx['oT16']
    bcast_row = ctx['bcast_row']; one_t = ctx['one_t']; cst_d = ctx['cst_d']
    T0 = b * SEQ

    def V3(ap, a):
        return ap.rearrange("p (a b) -> p a b", a=a)

    A.mark()
    dqT = V3(A.bf16(3 * 2048), 3); dkT2 = A.bf16(2048)
    vlat = V3(A.bf16(16 * 128), 16)
    iqT = V3(A.bf16(4 * 2048), 4); ikT2 = A.bf16(2048)
    iw = V3(A.f32(16 * 8), 16)
    sqT = V3(A.bf16(3 * 2048), 3); skT = V3(A.bf16(3 * 2048), 3); skTn = V3(A.bf16(3 * 2048), 3)
    sv2f = A.bf16(16 * 5 * 128)
    sv2 = sv2f.rearrange("p (a h c) -> p a h c", a=16, h=5)
    POOL(lambda e: e.memset(sv2f, 0.0), [], ['sv2'])
    neg8 = A.bf16(512)
    POOL(lambda e: e.memset(neg8, -0.125), [], ['neg8'])

    A.mark()
    hT = V3(A.bf16(8 * 2048), 8)
    for q in range(4):
        DMA(hT[:, :, q * 512:(q + 1) * 512], hT16[:, :, T0 + q * 512:T0 + (q + 1) * 512], ('ld_hT', q), [], [('hT', q)])
    stage = [A.f32(1168) for _ in range(2)]
    wbf = V3(A.bf16(8 * 1168), 8)
    pbc = [0]
    evc = [0]

    def nb():
        pbc[0] = (pbc[0] + 1) % 4
        return pbc[0]

    def evac(out, in_, R, W, scale=None):
        evc[0] += 1
        if scale is not None:
            P.add('act', lambda e: e.mul(out, in_, scale), R, W)
        elif evc[0] % 2 == 0:
            ACT(out, in_, AF.Copy, R, W)
        else:
            DVE(lambda e: e.tensor_copy(out, in_), R, W)

    def proj_fm(c0, M, ev, wsrc=None, wkey='wbf'):
        wsrc = wbf if wsrc is None else wsrc
        for tc in range(4):
            pb = nb()
            for k in range(8):
                MM(ps[0:M, pb * 512:(pb + 1) * 512], wsrc[:, k, c0:c0 + M], hT[:, k, tc * 512:(tc + 1) * 512],
                   k == 0, k == 7, [wkey, ('hT', tc)], [('ps', pb)])
            ev(pb, tc)

    def proj_tm(c0, N, ev):
        for tt in range(16):
            pb = nb()
            for k in range(8):
                MM(ps[:, pb * 512:pb * 512 + N], hT[:, k, tt * 128:(tt + 1) * 128], wbf[:, k, c0:c0 + N],
                   k == 0, k == 7, ['wbf', ('hT', tt // 4)], [('ps', pb)])
            ev(pb, tt)

    if 'sb' in parts or 'sbproj' in parts:
        load_w(wbf[:, :, 0:960], lambda k: w_in[l, k * 128:(k + 1) * 128, 2264:3224], 8, 960, stage, 'win', 'wbf')
        import os
        lvl = int(os.environ.get("SBLVL", "9"))
        for p in range(3):
            M = 128 if p < 2 else 64
            if lvl < 2 or (lvl < 3 and p == 2):
                continue
            proj_fm(p * 128, M, lambda pb, tc, p=p, M=M: evac(sqT[0:M, p, tc * 512:(tc + 1) * 512],
                                                          ps[0:M, pb * 512:(pb + 1) * 512], [('ps', pb)], ['sqT']))

            def ev_k(pb, tc, p=p, M=M):
                evac(skT[0:M, p, tc * 512:(tc + 1) * 512], ps[0:M, pb * 512:(pb + 1) * 512], [('ps', pb)], ['skT'])
                POOL(lambda e, p=p, M=M, tc=tc: e.tensor_tensor(skTn[0:M, p, tc * 512:(tc + 1) * 512],
                                                                skT[0:M, p, tc * 512:(tc + 1) * 512], neg8[0:M, :], ALU.mult),
                     ['skT', 'neg8'], ['skTn'])
            if lvl >= 4:
                proj_fm(320 + p * 128, M, ev_k)

        def ev_v(pb, tt):
            pv = ps[:, pb * 512:pb * 512 + 320].rearrange("p (h c) -> p h c", h=5)
            evac(sv2[:, tt, 1::2, 0:64], pv[:, 1::2, :], [('ps', pb)], ['sv2'])
            evac(sv2[:, tt, 0::2, 64:128], pv[:, 0::2, :], [('ps', pb)], ['sv2'])
        if lvl >= 5:
            proj_tm(640, 320, ev_v)

    if 'dsa' in parts:
        load_w(wbf[:, :, 0:1096], lambda k: w_in[l, k * 128:(k + 1) * 128, 1168:2264], 8, 1096, stage, 'win', 'wbf')
        A.mark()
        wdk = V3(A.bf16(8 * 128), 8); wik = V3(A.bf16(8 * 128), 8)
        for hh in range(2):
            POOL(lambda e, hh=hh: e.tensor_copy(wdk[:, :, hh * 64:(hh + 1) * 64], wbf[:, :, 320:384]), ['wbf'], ['wdk'])
            POOL(lambda e, hh=hh: e.tensor_copy(wik[:, :, hh * 64:(hh + 1) * 64], wbf[:, :, 1024:1088]), ['wbf'], ['wik'])
        for p in range(3):
            M = 128 if p < 2 else 64
            proj_fm(p * 128, M, lambda pb, tc, p=p, M=M: evac(dqT[0:M, p, tc * 512:(tc + 1) * 512],
                                                          ps[0:M, pb * 512:(pb + 1) * 512], [('ps', pb)], ['dqT']))
        proj_fm(0, 128, lambda pb, tc: evac(dkT2[:, tc * 512:(tc + 1) * 512], bank(pb), [('ps', pb)], ['dkT2']),
                wsrc=wdk, wkey='wdk')
        proj_fm(0, 128, lambda pb, tc: evac(ikT2[:, tc * 512:(tc + 1) * 512], bank(pb), [('ps', pb)], ['ikT2']),
                wsrc=wik, wkey='wik')
        for p in range(4):
            proj_fm(512 + p * 128, 128, lambda pb, tc, p=p: evac(iqT[:, p, tc * 512:(tc + 1) * 512], bank(pb),
                                                              [('ps', pb)], ['iqT']))
        proj_tm(384, 128, lambda pb, tt: evac(vlat[:, tt, :], ps[:, pb * 512:pb * 512 + 128], [('ps', pb)], ['vlat']))
        proj_tm(1088, 8, lambda pb, tt: evac(iw[:, tt, :], ps[:, pb * 512:pb * 512 + 8], [('ps', pb)], ['iw']))
        A.release()
        P.barrier()

    if 'gla' in parts:
        gla_seq(ctx, l, b, hT, wbf, stage, nb, evac, proj_fm)
    else:
        zero_o(ctx, T0, 0, 3, 0, 128)
    A.release()
    P.barrier()

    if 'dsa' in parts:
        dsa_seq(ctx, l, b, dqT, dkT2, vlat, iqT, ikT2, iw)
    else:
        zero_o(ctx, T0, 3, 5, 0, 128)
        zero_o(ctx, T0, 5, 6, 0, 64)
    P.barrier()
    if 'sb' in parts:
        sb_seq(ctx, l, b, sqT, skT, skTn, sv2)
    else:
        zero_o(ctx, T0, 5, 6, 64, 128)
        zero_o(ctx, T0, 6, 8, 0, 128)
    A.release()


def zero_o(ctx, T0, c0, c1, p0, p1):
    A = ctx['A']; POOL = ctx['POOL']; DMA = ctx['DMA']; oT16 = ctx['oT16']
    n = c1 - c0
    zt = A.bf16(n * 512)
    key = ('zt', c0, p0)
    POOL(lambda e: e.memset(zt, 0.0), [], [key])
    for q in range(4):
        DMA(oT16[p0:p1, c0:c1, T0 + q * 512:T0 + (q + 1) * 512], zt[p0:p1, :].rearrange("p (k t) -> p k t", k=n),
            ('st_zt', c0, p0), [key], [('oT16z', c0, p0, q)])


def gla_seq(ctx, l, b, hT, wbf, stage, nb, evac, proj_fm):
    nc = ctx['nc']; P = ctx['P']; A = ctx['A']; ps = ctx['ps']
    bank = ctx['bank']; bankbf = ctx['bankbf']
    MM = ctx['MM']; TR = ctx['TR']; ACT = ctx['ACT']; DVE = ctx['DVE']; POOL = ctx['POOL']; DMA = ctx['DMA']
    cst = ctx['cst']; cbf = ctx['cbf']; ident_bf = ctx['ident_bf']
    load_w = ctx['load_w']; w_in = ctx['w_in']; oT16 = ctx['oT16']
    bcast_row = ctx['bcast_row']; one_t = ctx['one_t']
    gate_w2 = ctx['gate_w2']; gate_b = ctx['gate_b']; norm_g = ctx['norm_g']
    T0 = b * SEQ
    QS = 48 ** -0.5

    def V3(ap, a):
        return ap.rearrange("p (a b) -> p a b", a=a)

    load_w(wbf[:, :, 0:1168], lambda k: w_in[l, k * 128:(k + 1) * 128, 0:1168], 8, 1168, stage, 'win', 'wbf')
    gw2f = A.f32(192); gw2b = A.bf16(192)
    DMA(gw2f[0:16, :], gate_w2[l], 'ld_gw2', [], ['gw2f'])
    POOL(lambda e: e.tensor_copy(gw2b[0:16, :], gw2f[0:16, :]), ['gw2f'], ['gw2b'])
    gb = A.f32(192)
    DMA(gb, bcast_row(gate_b[l:l + 1, :], 192), 'ld_gb', [], ['gb'])
    ng4 = A.f32(384)
    for h in range(4):
        DMA(ng4[:, h * 96:(h + 1) * 96], bcast_row(norm_g[l:l + 1, :], 96), 'ld_ng', [], ['ng4'])
    bt4 = A.f32(512)
    for h in range(4):
        POOL(lambda e, h=h: e.tensor_copy(bt4[:, h * 128:(h + 1) * 128], cst[:, C_BT:C_BT + 128]), ['cst'], ['bt4'])
    glrT = A.bf16(2048)
    proj_fm(768, 16, lambda pb, tc: evac(glrT[0:16, tc * 512:(tc + 1) * 512], ps[0:16, pb * 512:(pb + 1) * 512],
                                         [('ps', pb)], ['glrT']))
    S = A.f32(384); Smid = A.f32(384); S2 = A.bf16(384)
    POOL(lambda e: e.memset(S, 0.0), [], ['S'])
    POOL(lambda e: e.memset(S2, 0.0), [], ['S2'])
    qt2 = A.bf16(512); kt2 = A.bf16(512); kend2 = A.bf16(512); sp2 = A.bf16(512)
    for t_, k_ in ((qt2, 'qt2'), (kt2, 'kt2'), (kend2, 'kend2'), (sp2, 'sp2')):
        POOL(lambda e, t_=t_: e.memset(t_, 0.0), [], [k_])
    qt2v = V3(qt2, 4); kt2v = V3(kt2, 4); kend2v = V3(kend2, 4); sp2v = V3(sp2, 4)
    vbf = A.bf16(384); sg = A.f32(384)
    xb = A.f32(192); ebuf = A.f32(192); spf = A.f32(192); spb = A.bf16(192)
    E1 = A.f32(192); E2 = A.f32(192); E3 = A.f32(192); totS = A.f32(192); dd = A.f32(192)
    dec = A.f32(8); qkT = A.bf16(1024); scm = A.bf16(512)
    ssq = A.f32(4); rstd4 = A.f32(4); junk = A.f32(96); t1 = A.f32(384); og = A.bf16(384)
    oTg = [V3(A.bf16(3 * 512), 3) for _ in range(2)]
    bo_bf = cbf[:, C_BO:C_BO + 128]; bt_bf = cbf[:, C_BT:C_BT + 128]; cs_bf = cbf[:, C_CS:C_CS + 2]

    def h4(ap, r0=0, r1=128):
        return ap[r0:r1, :].rearrange("p (h d) -> p h d", h=4)

    for tt in range(16):
        hk = ('hT', tt // 4)
        tsl = slice(tt * 128, (tt + 1) * 128)
        for k in range(8):
            MM(ps[:, 0:384], hT[:, k, tsl], wbf[:, k, 0:384], k == 0, k == 7, ['wbf', hk], [('ps', 0)])
        for k in range(8):
            MM(ps[:, 512:512 + 384], hT[:, k, tsl], wbf[:, k, 384:768], k == 0, k == 7, ['wbf', hk], [('ps', 1)])
        ACT(vbf, ps[:, 512:512 + 384], AF.Copy, [('ps', 1)], ['vbf'])
        for k in range(8):
            MM(ps[:, 512:512 + 384], hT[:, k, tsl], wbf[:, k, 784:1168], k == 0, k == 7, ['wbf', hk], [('ps', 1)])
        ACT(sg, ps[:, 512:512 + 384], AF.Silu, [('ps', 1)], ['sg'])
        MM(ps[:, 1024:1024 + 192], glrT[0:16, tsl], gw2b[0:16, :], True, True, ['glrT', 'gw2b'], [('ps', 2)])
        DVE(lambda e: e.tensor_tensor(xb, ps[:, 1024:1024 + 192], gb, ALU.add), [('ps', 2), 'gb'], ['xb'])
        ACT(ebuf, xb, AF.Exp, ['xb'], ['ebuf'], scale=-1.0)
        ACT(spf, ebuf, AF.Ln, ['ebuf', 'kc'], ['spf'], bias=one_t)
        POOL(lambda e: e.tensor_copy(spb, spf), ['spf'], ['spb'])
        POOL(lambda e: e.tensor_copy(sp2v[:, :, 0:48], h4(spf)), ['spf'], ['sp2'])
        POOL(lambda e: e.tensor_copy(sp2v[:, :, 64:112], h4(spf)), ['spf'], ['sp2'])
        MM(ps[:, 1024:1024 + 192], bt_bf, spb, True, True, ['spb', 'cbf'], [('ps', 2)])
        MM(ps[:, 1024 + 192:1024 + 384], bo_bf, spb, True, True, ['spb', 'cbf'], [('ps', 2)])
        for h in range(4):
            MM(ps[:, 1536 + 2 * h:1536 + 2 * h + 2], sp2[:, h * 128:(h + 1) * 128], cs_bf, True, True,
               ['sp2', 'cbf'], [('ps', 3)])
        ACT(dec, ps[:, 1536:1536 + 8], AF.Exp, [('ps', 3)], ['dec'], scale=-1.0 / 16)
        cum = ps[:, 1024:1024 + 192]; tot = ps[:, 1024 + 192:1024 + 384]
        ACT(E1, cum, AF.Exp, [('ps', 2)], ['E1'], scale=-1.0 / 16)
        ACT(E2, cum, AF.Exp, [('ps', 2)], ['E2'], scale=1.0 / 16)
        ACT(totS, tot, AF.Copy, [('ps', 2)], ['totS'])
        DVE(lambda e: e.tensor_tensor(dd, cum, totS, ALU.subtract), [('ps', 2), 'totS'], ['dd'])
        ACT(E3, dd, AF.Exp, ['dd'], ['E3'], scale=1.0 / 16)
        qps = ps[:, 0:192]; kps = ps[:, 192:384]
        for half in range(2):
            r0 = 64 * half
            DVE(lambda e, r0=r0, half=half: e.scalar_tensor_tensor(
                qt2v[r0:r0 + 64, :, 64 * half:64 * half + 48], h4(qps, r0, r0 + 64), QS, h4(E1, r0, r0 + 64),
                ALU.mult, ALU.mult), [('ps', 0), 'E1'], ['qt2'])
        for cp in (0, 64):
            DVE(lambda e, cp=cp: e.tensor_tensor(kt2v[:, :, cp:cp + 48], h4(kps), h4(E2), ALU.mult),
                [('ps', 0), 'E2'], ['kt2'])
            DVE(lambda e, cp=cp: e.tensor_tensor(kend2v[:, :, cp:cp + 48], h4(kps), h4(E3), ALU.mult),
                [('ps', 0), 'E3'], ['kend2'])
        pv = bankbf(3)
        for h in range(4):
            TR(pv[:, (2 * h) * 128:(2 * h + 1) * 128], qt2[:, h * 128:(h + 1) * 128], ident_bf, ['qt2', 'cbf'], [('ps', 3)])
            TR(pv[:, (2 * h + 1) * 128:(2 * h + 2) * 128], kt2[:, h * 128:(h + 1) * 128], ident_bf, ['kt2', 'cbf'], [('ps', 3)])
        ACT(qkT, pv, AF.Copy, [('ps', 3)], ['qkT'])
        for h in range(4):
            MM(ps[:, 2048 + h * 128:2048 + (h + 1) * 128], qkT[:, (2 * h + 1) * 128:(2 * h + 2) * 128],
               qkT[:, (2 * h) * 128:(2 * h + 1) * 128], True, True, ['qkT'], [('ps', 4)])
        DVE(lambda e: e.tensor_tensor(scm, ps[:, 2048:2560], bt4, ALU.mult), [('ps', 4), 'bt4'], ['scm'])
        for h in range(4):
            MM(ps[:, 3072 + h * 96:3072 + (h + 1) * 96], kend2[0:64, h * 128:(h + 1) * 128], vbf[0:64, h * 96:(h + 1) * 96],
               True, True, ['kend2', 'vbf'], [('ps', 6)])
        for h in range(4):
            MM(ps[:, 3584 + h * 96:3584 + (h + 1) * 96], kend2[64:128, h * 128:(h + 1) * 128], vbf[64:128, h * 96:(h + 1) * 96],
               True, True, ['kend2', 'vbf'], [('ps', 7)])
        POOL(lambda e: e.tensor_copy(S2[0:48, :], S[0:48, :]), ['S'], ['S2'])
        for h in range(4):
            DVE(lambda e, h=h: e.scalar_tensor_tensor(Smid[:, h * 96:(h + 1) * 96], S[:, h * 96:(h + 1) * 96],
                                                      dec[:, 2 * h:2 * h + 1], ps[:, 3072 + h * 96:3072 + (h + 1) * 96],
                                                      ALU.mult, ALU.add), ['S', 'dec', ('ps', 6)], ['Smid'])
        POOL(lambda e: e.tensor_copy(S2[64:112, :], Smid[64:112, :]), ['Smid'], ['S2'])
        for h in range(4):
            DVE(lambda e, h=h: e.scalar_tensor_tensor(S[:, h * 96:(h + 1) * 96], Smid[:, h * 96:(h + 1) * 96],
                                                      dec[:, 2 * h + 1:2 * h + 2], ps[:, 3584 + h * 96:3584 + (h + 1) * 96],
                                                      ALU.mult, ALU.add), ['Smid', 'dec', ('ps', 7)], ['S'])
        for h in range(4):
            MM(ps[:, 2560 + h * 96:2560 + (h + 1) * 96], scm[:, h * 128:(h + 1) * 128], vbf[:, h * 96:(h + 1) * 96],
               True, False, ['scm', 'vbf'], [('ps', 5)])
            MM(ps[:, 2560 + h * 96:2560 + (h + 1) * 96], qkT[:, (2 * h) * 128:(2 * h + 1) * 128], S2[:, h * 96:(h + 1) * 96],
               False, True, ['qkT', 'S2'], [('ps', 5)])
        POOL(lambda e: e.memset(ssq, 0.0), [], ['ssq'])
        for h in range(4):
            ACT(junk, ps[:, 2560 + h * 96:2560 + (h + 1) * 96], AF.Square, [('ps', 5)], ['junk', 'ssq'], accum=ssq[:, h:h + 1])
        DVE(lambda e: e.tensor_scalar(rstd4, ssq, 1.0 / 96, LN_EPS, ALU.mult, ALU.add), ['ssq'], ['rstd4'])
        ACT(rstd4, rstd4, AF.Ln, ['rstd4'], ['rstd4'])
        ACT(rstd4, rstd4, AF.Exp, ['rstd4'], ['rstd4'], scale=-0.5)
        for h in range(4):
            DVE(lambda e, h=h: e.tensor_scalar(t1[:, h * 96:(h + 1) * 96], ps[:, 2560 + h * 96:2560 + (h + 1) * 96],
                                               rstd4[:, h:h + 1], 1.0, ALU.mult, ALU.mult), [('ps', 5), 'rstd4'], ['t1'])
        POOL(lambda e: e.tensor_tensor(t1, t1, ng4, ALU.mult), ['t1', 'ng4'], ['t1'])
        POOL(lambda e: e.tensor_tensor(og, t1, sg, ALU.mult), ['t1', 'sg'], ['og'])
        pv2 = bankbf(4)
        for c in range(3):
            TR(pv2[:, c * 128:(c + 1) * 128], og[:, c * 128:(c + 1) * 128], ident_bf, ['og', 'cbf'], [('ps', 4)])
        blk = tt // 4; q = tt % 4; s = blk % 2
        ACT(oTg[s][:, :, q * 128:(q + 1) * 128], pv2[:, 0:384].rearrange("p (c t) -> p c t", c=3), AF.Copy,
            [('ps', 4)], [('oTg', s)])
        if q == 3:
            DMA(oT16[:, 0:3, T0 + blk * 512:T0 + (blk + 1) * 512], oTg[s], ('st_oTg', s), [('oTg', s)], [('oT16g', blk)])


def dsa_seq(ctx, l, b, dqT, dkT2, vlat, iqT, ikT2, iw):
    nc = ctx['nc']; P = ctx['P']; A = ctx['A']; ps = ctx['ps']
    bank = ctx['bank']; bankbf = ctx['bankbf']
    MM = ctx['MM']; TR = ctx['TR']; ACT = ctx['ACT']; DVE = ctx['DVE']; POOL = ctx['POOL']; DMA = ctx['DMA']
    cst = ctx['cst']; cbf = ctx['cbf']; ident_bf = ctx['ident_bf']; ones_bf = ctx['ones_bf']
    oT16 = ctx['oT16']; w_uv = ctx['w_uv']; cst_d = ctx['cst_d']
    T0 = b * SEQ

    def V3(ap, a):
        return ap.rearrange("p (a b) -> p a b", a=a)

    A.mark()
    dist = A.f32(2048)
    DMA(dist, cst_d[:, C_DIST:C_DIST + 2048], 'ld_dist', [], ['dist'])
    Th = [A.bf16(2048) for _ in range(5)]
    for h in range(5):
        ACT(Th[h], dist, AF.Exp, ['dist'], [('Th', h)], scale=-SLOPES[h])
    wuvf = A.f32(5 * 64)
    for h in range(5):
        DMA(wuvf[:, h * 64:(h + 1) * 64], w_uv[l, h], 'ld_wuv', [], ['wuvf'])
    wuv2f = A.bf16(5 * 128)
    wuv2 = V3(wuv2f, 5)
    POOL(lambda e: e.memset(wuv2f, 0.0), [], ['wuv2'])
    for h in range(5):
        bs = 64 * (h % 2)
        POOL(lambda e, h=h, bs=bs: e.tensor_copy(wuv2[:, h, bs:bs + 64], wuvf[:, h * 64:(h + 1) * 64]), ['wuvf'], ['wuv2'])
    isc = A.f32(2048); work = A.f32(2048)
    rbuf = [A.f32(512) for _ in range(2)]
    m8 = A.f32(8); thr = A.f32(1)
    maskrow = A.bf16(2048)
    maskT = V3(A.bf16(16 * 512), 16)
    Eb = [A.bf16(512) for _ in range(2)]
    Pm = [A.bf16(512) for _ in range(2)]
    rden = A.f32(512)
    olat = [A.bf16(512) for _ in range(2)]
    oTd = [A.bf16(512) for _ in range(2)]
    negI = cst[:, C_NEGI:C_NEGI + 128]
    ri = 0
    for tg in range(4):
        for ti in range(4):
            i = 4 * tg + ti
            ncols = (i + 1) * 128
            tsl = slice(i * 128, (i + 1) * 128)
            for h in range(8):
                hp, bs = h // 2, 64 * (h % 2)
                for sc in range(0, ncols, 512):
                    w = min(512, ncols - sc)
                    pb = ri % 2
                    rb = ri % 2
                    ri += 1
                    MM(ps[:, pb * 512:pb * 512 + w], iqT[bs:bs + 64, hp, tsl], ikT2[bs:bs + 64, sc:sc + w], True, True,
                       ['iqT', 'ikT2'], [('ps', pb)])
                    ACT(rbuf[rb][:, 0:w], ps[:, pb * 512:pb * 512 + w], AF.Relu, [('ps', pb)], [('rbuf', rb)])
                    if h == 0:
                        DVE(lambda e, rb=rb, sc=sc, w=w, i=i, h=h: e.tensor_scalar(
                            isc[:, sc:sc + w], rbuf[rb][:, 0:w], iw[:, i, h:h + 1], 1.0, ALU.mult, ALU.mult),
                            [('rbuf', rb), 'iw'], ['isc'])
                    else:
                        DVE(lambda e, rb=rb, sc=sc, w=w, i=i, h=h: e.scalar_tensor_tensor(
                            isc[:, sc:sc + w], rbuf[rb][:, 0:w], iw[:, i, h:h + 1], isc[:, sc:sc + w], ALU.mult, ALU.add),
                            [('rbuf', rb), 'iw', 'isc'], ['isc'])
            POOL(lambda e, i=i: e.tensor_tensor(isc[:, i * 128:(i + 1) * 128], isc[:, i * 128:(i + 1) * 128], negI, ALU.add),
                 ['isc', 'cst'], ['isc'])
            if i >= 2:
                cur = isc
                for r in range(32):
                    DVE(lambda e, cur=cur, ncols=ncols: e.max(m8, cur[:, 0:ncols]), ['isc', 'work'], ['m8'])
                    if r < 31:
                        DVE(lambda e, cur=cur, ncols=ncols: e.match_replace(work[:, 0:ncols], m8, cur[:, 0:ncols], NEG),
                            ['m8', 'isc', 'work'], ['work'])
                        cur = work
                DVE(lambda e: e.tensor_reduce(thr, m8, mybir.AxisListType.X, ALU.min), ['m8'], ['thr'])
            else:
                DVE(lambda e: e.memset(thr, -1.0e29), [], ['thr'])
            DVE(lambda e, ncols=ncols: e.tensor_scalar(maskrow[:, 0:ncols], isc[:, 0:ncols], thr, 1.0, ALU.is_ge, ALU.mult),
                ['isc', 'thr'], ['maskrow'])
            for j0 in range(0, i + 1, 8):
                n = min(8, i + 1 - j0)
                pvm = bankbf(2)
                for jj in range(n):
                    j = j0 + jj
                    TR(pvm[:, jj * 128:(jj + 1) * 128], maskrow[:, j * 128:(j + 1) * 128], ident_bf, ['maskrow', 'cbf'], [('ps', 2)])
                POOL_or_ACT = ACT
                ACT(maskT[:, j0:j0 + n, ti * 128:(ti + 1) * 128], pvm[:, 0:n * 128].rearrange("p (j t) -> p j t", j=n),
                    AF.Copy, [('ps', 2)], ['maskT'])
        jmax = 4 * tg + 3
        for h in range(5):
            hp, bs = h // 2, 64 * (h % 2)
            for j in range(jmax + 1):
                c0 = max(j - 4 * tg, 0) * 128
                w = 512 - c0
                tq0 = 512 * tg + c0
                pb = 3 + (j % 2)
                s = j % 2
                MM(ps[:, pb * 512:pb * 512 + w], dkT2[bs:bs + 64, j * 128:(j + 1) * 128], dqT[bs:bs + 64, hp, tq0:tq0 + w],
                   True, True, ['dkT2', 'dqT'], [('ps', pb)])
                ACT(Eb[s][:, 0:w], ps[:, pb * 512:pb * 512 + w], AF.Exp, [('ps', pb)], [('Eb', s)], scale=0.125)
                POOL(lambda e, s=s, w=w, j=j, c0=c0: e.tensor_tensor(Pm[s][:, 0:w], Eb[s][:, 0:w], maskT[:, j, c0:c0 + w],
                                                                    ALU.mult), [('Eb', s), 'maskT'], [('Pm', s)])
                m0 = tq0 - 128 * j
                POOL(lambda e, s=s, w=w, m0=m0, h=h: e.tensor_tensor(Pm[s][:, 0:w], Pm[s][:, 0:w], Th[h][:, m0:m0 + w],
                                                                    ALU.mult), [('Pm', s), ('Th', h)], [('Pm', s)])
                MM(ps[:, 5 * 512 + c0:5 * 512 + 512], vlat[:, j, :], Pm[s][:, 0:w], j == 0, j == jmax,
                   ['vlat', ('Pm', s)], [('ps', 5)])
                MM(ps[:, 6 * 512 + c0:6 * 512 + 512], ones_bf, Pm[s][:, 0:w], j == 0, j == jmax,
                   ['cbf', ('Pm', s)], [('ps', 6)])
            DVE(lambda e: e.reciprocal(rden, bank(6)), [('ps', 6)], ['rden'])
            os_ = h % 2
            DVE(lambda e, os_=os_: e.tensor_tensor(olat[os_], bank(5), rden, ALU.mult), [('ps', 5), 'rden'], [('olat', os_)])
            if h % 2 == 1 or h == 4:
                pr = h // 2
                hs = [h - 1, h] if h % 2 == 1 else [h]
                for n_, hh in enumerate(hs):
                    MM(bank(7), wuv2[:, hh, :], olat[hh % 2], n_ == 0, n_ == len(hs) - 1, ['wuv2', ('olat', hh % 2)], [('ps', 7)])
                od = oTd[pr % 2]
                ACT(od, bank(7), AF.Copy, [('ps', 7)], [('oTd', pr % 2)])
                p1 = 128 if len(hs) == 2 else 64
                DMA(oT16[0:p1, 3 + pr, T0 + tg * 512:T0 + (tg + 1) * 512], od[0:p1, :], ('st_oTd', pr % 2),
                    [('oTd', pr % 2)], [('oT16d', pr, tg)])
    A.release()


def sb_seq(ctx, l, b, sqT, skT, skTn, sv2):
    nc = ctx['nc']; P = ctx['P']; A = ctx['A']; ps = ctx['ps']
    bank = ctx['bank']; bankbf = ctx['bankbf']
    MM = ctx['MM']; TR = ctx['TR']; ACT = ctx['ACT']; DVE = ctx['DVE']; POOL = ctx['POOL']; DMA = ctx['DMA']
    cst = ctx['cst']; cbf = ctx['cbf']; ones_bf = ctx['ones_bf']; tri_bf = ctx['tri_bf']
    oT16 = ctx['oT16']; one_t = ctx['one_t']
    T0 = b * SEQ
    ms_bf = cbf[:, C_MS:C_MS + 128]

    def V3(ap, a):
        return ap.rearrange("p (a b) -> p a b", a=a)

    A.mark()
    wT = [V3(A.bf16(16 * 512), 16) for _ in range(2)]
    Cb = A.bf16(512)
    ebuf = [A.f32(512) for _ in range(2)]
    spm = [A.bf16(512) for _ in range(2)]
    oTs = [A.bf16(512) for _ in range(2)]
    hi = 0
    for tg in range(4):
        jmax = 4 * tg + 3
        for h in range(5):
            hp, bs = h // 2, 64 * (h % 2)
            row = 704 + 64 * h
            chunk, ob = row // 128, row % 128
            ws = hi % 2
            hi += 1
            wkey = ('wT', ws)
            POOL(lambda e: e.memset(Cb, 0.0), [], ['Cb'])
            for js in range(jmax, -1, -1):
                c0 = max(js - 4 * tg, 0) * 128
                w = 512 - c0
                tq0 = 512 * tg + c0
                s = js % 2
                pz = s
                pa = 2 + s
                ksl = slice(js * 128, (js + 1) * 128)
                MM(ps[:, pz * 512:pz * 512 + w], skT[bs:bs + 64, hp, ksl], sqT[bs:bs + 64, hp, tq0:tq0 + w], True, True,
                   ['skT', 'sqT'], [('ps', pz)])
                ACT(ebuf[s][:, 0:w], ps[:, pz * 512:pz * 512 + w], AF.Exp, [('ps', pz)], [('ebuf', s)], scale=0.125)
                ACT(spm[s][:, 0:w], ebuf[s][:, 0:w], AF.Ln, [('ebuf', s), 'kc'], [('spm', s)], bias=one_t)
                diag = js >= 4 * tg
                if diag:
                    POOL(lambda e, s=s: e.tensor_tensor(spm[s][:, 0:128], spm[s][:, 0:128], ms_bf, ALU.mult),
                         [('spm', s), 'cbf'], [('spm', s)])
                first = (js == jmax)
                MM(ps[:, pa * 512:pa * 512 + w], tri_bf, spm[s][:, 0:w], True, False, ['cbf', ('spm', s)], [('ps', pa)])
                if not first:
                    MM(ps[:, pa * 512:pa * 512 + w], ones_bf, Cb[:, c0:c0 + w], False, False, ['cbf', 'Cb'], [('ps', pa)])
                MM(ps[:, pa * 512:pa * 512 + w], skTn[bs:bs + 64, hp, ksl], sqT[bs:bs + 64, hp, tq0:tq0 + w], False, True,
                   ['skTn', 'sqT'], [('ps', pa)])
                ACT(wT[ws][:, js, c0:c0 + w], ps[:, pa * 512:pa * 512 + w], AF.Exp, [('ps', pa)], [wkey], scale=-1.0)
                if diag:
                    POOL(lambda e, ws=ws, js=js, c0=c0: e.tensor_tensor(wT[ws][:, js, c0:c0 + 128], wT[ws][:, js, c0:c0 + 128],
                                                                       ms_bf, ALU.mult), [wkey, 'cbf'], [wkey])
                if js > 0:
                    POOL(lambda e, s=s, c0=c0, w=w: e.tensor_tensor(Cb[:, c0:c0 + w], Cb[:, c0:c0 + w], spm[s][:, 0:w], ALU.add),
                         ['Cb', ('spm', s)], ['Cb'])
            pair_first = (h == 0) or (h % 2 == 1)
            pair_last = (h == 0) or (h % 2 == 0)
            po = 4 + (chunk % 2)
            for js in range(jmax + 1):
                c0 = max(js - 4 * tg, 0) * 128
                w = 512 - c0
                MM(ps[:, po * 512 + c0:po * 512 + 512], sv2[:, js, h, :], wT[ws][:, js, c0:c0 + w],
                   pair_first and js == 0, pair_last and js == jmax, ['sv2', wkey], [('ps', po)])
            if pair_last:
                od = oTs[chunk % 2]
                ACT(od, bank(po), AF.Copy, [('ps', po)], [('oTs', chunk % 2)])
                p0 = 64 if h == 0 else 0
                DMA(oT16[p0:128, chunk, T0 + tg * 512:T0 + (tg + 1) * 512], od[p0:128, :], ('st_oTs', chunk % 2),
                    [('oTs', chunk % 2)], [('oT16s', chunk, tg)])
    A.release()


MIX_PARTS = ('gla', 'dsa', 'sb')

PARAM_NAMES = ['ln_in_g', 'ln_in_b', 'w_in', 'gla_gate_w2', 'gla_gate_b', 'gla_norm_g', 'dsa_w_uv', 'w_out',
               'ln_mix_g', 'ln_mix_b', 'w_mem_q', 'w_mem_kv', 'w_mem_o', 'ln_mem_g', 'ln_mem_b',
               'w_up', 'b_up', 'w_down', 'b_down', 'ln_ffn_g', 'ln_ffn_b']


def make_in_maps(inputs, ncores=8):
    cst = make_consts()
    maps = []
    for c in range(ncores):
        m = {'cst': cst}
        m['x'] = np.ascontiguousarray(inputs['x'][c * NSEQ:(c + 1) * NSEQ]).reshape(TOK, D)
        m['mem'] = np.ascontiguousarray(inputs['mem'][c * NSEQ:(c + 1) * NSEQ]).reshape(NSEQ * MEMT, D)
        for k in PARAM_NAMES:
            a = np.ascontiguousarray(inputs[k], dtype=np.float32)
            if k in ('ln_in_g', 'ln_in_b'):
                a = a.reshape(1, D)
            m[k] = a
        maps.append(m)
    return maps


def kernel(**inputs):
    inputs = {k: np.asarray(v) for k, v in inputs.items()}
    nc, _ = build()
    maps = make_in_maps(inputs, 8)
    res = run_bass_kernel_spmd(nc, maps, core_ids=list(range(8)))
    outs = [r['out'].reshape(NSEQ, SEQ, D) for r in res.results]
    return np.concatenate(outs, axis=0).astype(np.float32)
```

```python
import numpy as np
from contextlib import ExitStack
import concourse.bass as bass
import concourse.mybir as mybir
from concourse.bass_utils import run_bass_kernel_spmd

F32 = mybir.dt.float32
BF16 = mybir.dt.bfloat16
AF = mybir.ActivationFunctionType
ALU = mybir.AluOpType

D = 1024
SEQ = 2048
NSEQ = 2
TOK = NSEQ * SEQ
DEPTH = 2
PIN = 3224
DFF = 4096
MEMT = 256
ALPHA = (2.0 * DEPTH) ** 0.25
LN_EPS = 1e-5
SLOPES = [2.0 ** (-8.0 * (i + 1) / 5) for i in range(5)]
NEG = -1.0e30

C_ID, C_TRI, C_ONE, C_MS, C_BT, C_BO, C_NEGI, C_CS, C_DIST = 0, 128, 256, 384, 512, 640, 768, 896, 1024
CST_N = 1024 + 2048


def make_consts():
    c = np.zeros((128, CST_N), np.float32)
    p = np.arange(128)[:, None]
    q = np.arange(128)[None, :]
    c[:, C_ID:C_ID + 128] = (p == q)
    c[:, C_TRI:C_TRI + 128] = (p >= q)
    c[:, C_ONE:C_ONE + 128] = 1.0
    c[:, C_MS:C_MS + 128] = (p < q)
    c[:, C_BT:C_BT + 128] = (p <= q) & ((p // 64) == (q // 64))
    c[:, C_BO:C_BO + 128] = ((p // 64) == (q // 64))
    c[:, C_NEGI:C_NEGI + 128] = np.where(q <= p, 0.0, NEG)
    c[:, C_CS + 0] = (np.arange(128) < 64)
    c[:, C_CS + 1] = (np.arange(128) >= 64)
    m = np.arange(2048)[None, :]
    c[:, C_DIST:C_DIST + 2048] = m - p
    return c


class Prog:
    ENGS = ['pe', 'act', 'dve', 'pool', 'sp']

    def __init__(self, nc):
        self.nc = nc
        self.ops = []
        self.res = {}
        self.dma_cnt = {}
        self.last_dma = {}
        self.last_op = {}

    def add(self, eng, fn, R=(), W=(), dma=None, extra=()):
        i = len(self.ops)
        deps = {}
        for k in R:
            st = self.res.get(k)
            if st is not None and st[0] is not None:
                deps[st[0]] = deps.get(st[0], 0) | 1
        for k in W:
            st = self.res.get(k)
            if st is not None:
                if st[0] is not None:
                    deps[st[0]] = deps.get(st[0], 0) | 2
                for r in st[1]:
                    deps[r] = deps.get(r, 0) | 2
        for e in extra:
            deps[e] = deps.get(e, 0) | 1
        for k in R:
            st = self.res.setdefault(k, [None, []])
            st[1].append(i)
        for k in W:
            st = self.res.setdefault(k, [None, []])
            st[0] = i
            st[1] = []
        deps.pop(i, None)
        op = dict(id=i, eng=eng, fn=fn, deps=deps, dma=dma, sig=False, val=0)
        if dma is not None:
            c = self.dma_cnt.get(dma, 0) + 16
            self.dma_cnt[dma] = c
            op['val'] = c
            self.last_dma[dma] = i
        else:
            self.last_op[eng] = i
        self.ops.append(op)
        return i

    def barrier(self):
        ex = list(self.last_dma.values()) + list(self.last_op.values())
        x = self.add('sp', lambda e: e.nop(), extra=ex)
        for e in ['pe', 'act', 'dve', 'pool']:
            self.add(e, lambda en: en.nop(), extra=[x])
        self.res = {}

    def emit(self):
        nc = self.nc
        ops = self.ops
        for op in ops:
            need = []
            for pid, kind in op['deps'].items():
                p = ops[pid]
                if p['dma'] is None:
                    if op['dma'] is None and p['eng'] == op['eng']:
                        if op['eng'] in ('pe', 'sp'):
                            continue
                    p['sig'] = True
                need.append(pid)
            op['need'] = need
        cnt = {e: 0 for e in self.ENGS}
        for op in ops:
            if op['dma'] is None and op['sig']:
                cnt[op['eng']] += 1
                op['val'] = cnt[op['eng']]
        with ExitStack() as es:
            esem = {e: es.enter_context(nc.semaphore("s_" + e)) for e in self.ENGS}
            dsem = {k: es.enter_context(nc.semaphore("d_%d" % i)) for i, k in enumerate(self.dma_cnt)}
            block = es.enter_context(nc.Block())

            def run(e, eobj):
                waited = {}
                for op in ops:
                    if op['eng'] != e:
                        continue
                    waits = {}
                    for pid in op['need']:
                        p = ops[pid]
                        s = dsem[p['dma']] if p['dma'] is not None else esem[p['eng']]
                        v = p['val']
                        if waited.get(s.name, 0) >= v:
                            continue
                        if waits.get(s.name, (None, 0))[1] < v:
                            waits[s.name] = (s, v)
                    wl = list(waits.values())
                    for s, v in wl[1:]:
                        eobj.wait_ge(s, v)
                        waited[s.name] = v
                    ins = op['fn'](eobj)
                    if wl:
                        ins._wait_ge(wl[0][0], wl[0][1])
                        waited[wl[0][0].name] = wl[0][1]
                    if op['dma'] is not None:
                        ins.then_inc(dsem[op['dma']], 16)
                    elif op['sig']:
                        ins.then_inc(esem[e], 1)
                if e == 'sp':
                    for k, c in self.dma_cnt.items():
                        if waited.get(dsem[k].name, 0) < c:
                            eobj.wait_ge(dsem[k], c)

            block.tensor(lambda t: run('pe', t))
            block.scalar(lambda t: run('act', t))
            block.vector(lambda t: run('dve', t))
            block.gpsimd(lambda t: run('pool', t))
            block.sync(lambda t: run('sp', t))


class Arena:
    def __init__(self, nc, nwords):
        self.t = nc.alloc_sbuf_tensor("arena", [128, nwords], F32)
        self.n = nwords
        self.top = 0
        self.marks = []
        self.peak = 0

    def _alloc(self, nbytes):
        w = (nbytes + 3) // 4
        w = (w + 7) // 8 * 8
        off = self.top
        self.top += w
        self.peak = max(self.peak, self.top)
        assert self.top <= self.n, ("SBUF arena overflow", self.top * 4)
        return off

    def f32(self, cols):
        off = self._alloc(cols * 4)
        return self.t[:, off:off + cols]

    def bf16(self, cols):
        off = self._alloc(cols * 2)
        return self.t[:, off:off + (cols + 1) // 2].bitcast(BF16)

    def mark(self):
        self.marks.append(self.top)

    def release(self):
        self.top = self.marks.pop()


def build(dbg=None, phases=None):
    nc = bass.Bass("TRN2", target_bir_lowering=False)
    try:
        nc.allow_low_precision("bf16 matmul operands with fp32 accumulation")
    except Exception:
        pass
    try:
        nc.allow_non_contiguous_dma("small strided parameter loads")
    except Exception:
        pass
    P = Prog(nc)
    A = Arena(nc, 52000)
    ps = nc.alloc_psum_tensor("ps", [128, 4096], F32)

    def dt(name, shape, dtype=F32, kind="ExternalInput"):
        return nc.dram_tensor(name, shape, dtype, kind=kind).ap()

    x_d = dt("x", [TOK, D])
    mem_d = dt("mem", [NSEQ * MEMT, D])
    cst_d = dt("cst", [128, CST_N])
    ln_in_g = dt("ln_in_g", [1, D]); ln_in_b = dt("ln_in_b", [1, D])
    w_in = dt("w_in", [DEPTH, D, PIN])
    gate_w2 = dt("gla_gate_w2", [DEPTH, 16, 192]); gate_b = dt("gla_gate_b", [DEPTH, 192])
    norm_g = dt("gla_norm_g", [DEPTH, 96])
    w_uv = dt("dsa_w_uv", [DEPTH, 5, 128, 64])
    w_out = dt("w_out", [DEPTH, D, D])
    ln_mix_g = dt("ln_mix_g", [DEPTH, D]); ln_mix_b = dt("ln_mix_b", [DEPTH, D])
    w_mem_q = dt("w_mem_q", [DEPTH, D, D]); w_mem_kv = dt("w_mem_kv", [DEPTH, D, 2 * D]); w_mem_o = dt("w_mem_o", [DEPTH, D, D])
    ln_mem_g = dt("ln_mem_g", [DEPTH, D]); ln_mem_b = dt("ln_mem_b", [DEPTH, D])
    w_up = dt("w_up", [DEPTH, D, DFF]); b_up = dt("b_up", [DEPTH, DFF])
    w_down = dt("w_down", [DEPTH, DFF, D]); b_down = dt("b_down", [DEPTH, D])
    ln_ffn_g = dt("ln_ffn_g", [DEPTH, D]); ln_ffn_b = dt("ln_ffn_b", [DEPTH, D])
    out_d = dt("out", [TOK, D], kind="ExternalOutput")
    skind = "ExternalOutput" if dbg else "Internal"
    h32 = dt("h32", [TOK, D], kind=skind)
    hT16 = dt("hT16", [128, 8, TOK], BF16, kind=skind)
    oT16 = dt("oT16", [128, 8, TOK], BF16, kind=skind)
    wup16 = dt("wup16", [128, 8, DFF], BF16, kind="Internal")

    def bank(b, n=512):
        return ps[:, b * 512:b * 512 + n]

    def bankbf(b):
        return ps[:, b * 512:(b + 1) * 512].bitcast(BF16)

    def MM(out, lhsT, rhs, start, stop, R, W):
        P.add('pe', lambda e: e.matmul(out, lhsT, rhs, start=start, stop=stop), R, W)

    def TR(out, in_, ident, R, W):
        P.add('pe', lambda e: e.transpose(out, in_, ident), R, W)

    def ACT(out, in_, func, R, W, bias=None, scale=1.0, accum=None):
        kw = {}
        if bias is not None:
            kw['bias'] = bias
        if accum is not None:
            kw['accum_out'] = accum
        P.add('act', lambda e: e.activation(out, in_, func, scale=scale, **kw), R, W)

    def DVE(fn, R, W):
        P.add('dve', fn, R, W)

    def POOL(fn, R, W):
        P.add('pool', fn, R, W)

    def DMA(out, in_, key, R, W, q='sp', slow=False):
        if slow:
            P.add(q, lambda e: e.dma_start(out=out, in_=in_, allow_slow_non_contiguous=True), R, W, dma=key)
        else:
            P.add(q, lambda e: e.dma_start(out=out, in_=in_), R, W, dma=key)

    def bcast_row(ap_row, n):
        return ap_row.to_broadcast([128, n])

    cst = A.f32(1024)
    DMA(cst, cst_d[:, 0:1024], 'cst', ['cst_d'], ['cst'])
    cbf = A.bf16(1024)
    DVE(lambda e: e.tensor_copy(cbf, cst[:, 0:1024]), ['cst'], ['cbf'])
    ident_bf = cbf[:, C_ID:C_ID + 128]
    ones_bf = cbf[:, C_ONE:C_ONE + 128]
    tri_bf = cbf[:, C_TRI:C_TRI + 128]
    kc = A.f32(8)
    POOL(lambda e: e.memset(kc[:, 0:1], LN_EPS), [], ['kc'])
    POOL(lambda e: e.memset(kc[:, 1:2], 1.0), [], ['kc'])
    eps_t = kc[:, 0:1]
    one_t = kc[:, 1:2]
    A.mark()

    class LNState:
        pass

    def ln_setup(g_row, b_row, tag):
        st = LNState()
        st.g = A.f32(D); st.b = A.f32(D)
        DMA(st.g, bcast_row(g_row, D), ('lnp', 'g'), [], ['ln_g'])
        DMA(st.b, bcast_row(b_row, D), ('lnp', 'b'), [], ['ln_b'])
        st.ybf = [A.bf16(D) for _ in range(2)]
        st.stats = [A.f32(16) for _ in range(2)]
        st.hTb = [A.bf16(8 * 512) for _ in range(2)]
        st.n = 0
        return st

    def ln_tile(st, z, zkey, tok0, final=False):
        i = st.n; st.n += 1
        s = i % 2
        y = z; ybf = st.ybf[s]; stt = st.stats[s]
        ky, kyb, kst = zkey, ('ln_ybf', s), ('ln_st', s)
        DVE(lambda e: e.bn_stats(stt[:, 0:6], z[:, 0:512]), [zkey], [kst])
        DVE(lambda e: e.bn_stats(stt[:, 6:12], z[:, 512:1024]), [zkey], [kst])
        DVE(lambda e: e.bn_aggr(stt[:, 12:14], stt[:, 0:12].rearrange("p (a b) -> p a b", a=2)), [kst], [kst])
        ACT(stt[:, 15:16], stt[:, 13:14], AF.Ln, [kst, 'kc'], [kst], bias=eps_t)
        ACT(stt[:, 14:15], stt[:, 15:16], AF.Exp, [kst], [kst], scale=-0.5)
        DVE(lambda e: e.tensor_scalar(y, z, stt[:, 12:13], stt[:, 14:15], ALU.subtract, ALU.mult), [kst], [ky])
        POOL(lambda e: e.tensor_tensor(y, y, st.g, ALU.mult), [ky, 'ln_g'], [ky])
        POOL(lambda e: e.tensor_tensor(y, y, st.b, ALU.add), [ky, 'ln_b'], [ky])
        if final:
            DMA(out_d[tok0:tok0 + 128, :], y, ('st_y', zkey), [ky], [('out', tok0)])
            return
        DMA(h32[tok0:tok0 + 128, :], y, ('st_y', zkey), [ky], [('h32', tok0)])
        ACT(ybf, y, AF.Copy, [ky], [kyb])
        q = (tok0 // 128) % 4
        blk = tok0 // 512
        hs = blk % 2
        hTb = st.hTb[hs].rearrange("p (k t) -> p k t", k=8)
        pb = 6 + (i % 2)
        pv = bankbf(pb)
        for k in range(8):
            TR(pv[:, k * 128:(k + 1) * 128], ybf[:, k * 128:(k + 1) * 128], ident_bf, [kyb, 'cbf'], [('ps', pb)])
        DVE(lambda e: e.tensor_copy(hTb[:, :, q * 128:(q + 1) * 128], pv.rearrange("p (k t) -> p k t", k=8)),
            [('ps', pb)], [('hTb', hs)])
        if q == 3:
            DMA(hT16[:, :, blk * 512:(blk + 1) * 512], hTb, ('st_hT', hs), [('hTb', hs)], [('hT16', blk)])

    def load_w(dst3, src_fn, nk, ncols, stage, tag, wkey, engs=('pool', 'act')):
        for k in range(nk):
            s = k % len(stage)
            sk = ('wst', tag, s)
            DMA(stage[s][:, 0:ncols], src_fn(k), ('wst', tag, s), [], [sk])
            eng = engs[k % len(engs)]
            if eng == 'act':
                ACT(dst3[:, k, :], stage[s][:, 0:ncols], AF.Copy, [sk], [wkey])
            elif eng == 'pool':
                POOL(lambda e, k=k, s=s: e.tensor_copy(dst3[:, k, :], stage[s][:, 0:ncols]), [sk], [wkey])
            else:
                DVE(lambda e, k=k, s=s: e.tensor_copy(dst3[:, k, :], stage[s][:, 0:ncols]), [sk], [wkey])

    def phase_ln_in():
        A.mark()
        st = ln_setup(ln_in_g, ln_in_b, 'in')
        xt = [A.f32(D) for _ in range(2)]
        for i in range(TOK // 128):
            s = i % 2
            DMA(xt[s], x_d[i * 128:(i + 1) * 128, :], ('ld_x', s), [], [('xt', s)])
            ln_tile(st, xt[s], ('xt', s), i * 128)
        A.release()
        P.barrier()

    def res_ln_tiles(st, lhs_fn, lhs_keys, wbf, wkey, nk, tok0_blk, hbuf, zbuf, bias_tile=None, final=False, pairs=((4, 5),)):
        for tt in range(4):
            tok0 = tok0_blk + tt * 128
            s = tt % 2
            DMA(hbuf[s], h32[tok0:tok0 + 128, :], ('ld_h', s), [('h32', tok0)], [('hb', s)])
            for nh in range(2):
                pb = pairs[tt % len(pairs)][nh]
                for k in range(nk):
                    MM(bank(pb), lhs_fn(k, tt), wbf[:, k, nh * 512:(nh + 1) * 512], k == 0, k == nk - 1,
                       lhs_keys + [wkey], [('ps', pb)])
                DVE(lambda e, s=s, nh=nh, pb=pb: e.scalar_tensor_tensor(
                    zbuf[s][:, nh * 512:(nh + 1) * 512], hbuf[s][:, nh * 512:(nh + 1) * 512], ALPHA, bank(pb),
                    ALU.mult, ALU.add), [('hb', s), ('ps', pb)], [('zb', s)])
            if bias_tile is not None:
                POOL(lambda e, s=s: e.tensor_tensor(zbuf[s], zbuf[s], bias_tile, ALU.add), [('zb', s), 'bias_t'], [('zb', s)])
            ln_tile(st, zbuf[s], ('zb', s), tok0, final=final)

    def phase_wout(l):
        A.mark()
        st = ln_setup(ln_mix_g[l:l + 1, :], ln_mix_b[l:l + 1, :], 'mix')
        stage = [A.f32(1024) for _ in range(2)]
        wbf = A.bf16(8 * 1024).rearrange("p (k n) -> p k n", k=8)
        load_w(wbf, lambda k: w_out[l, k * 128:(k + 1) * 128, :], 8, 1024, stage, 'wo', 'wbf')
        src = [A.bf16(8 * 512).rearrange("p (k t) -> p k t", k=8) for _ in range(2)]
        hbuf = [A.f32(D) for _ in range(2)]
        zbuf = [A.f32(D) for _ in range(2)]
        for blk in range(TOK // 512):
            s = blk % 2
            DMA(src[s], oT16[:, :, blk * 512:(blk + 1) * 512], ('ld_src', s), [('oT16', blk)], [('src', s)])
            res_ln_tiles(st, lambda k, tt, s=s: src[s][:, k, tt * 128:(tt + 1) * 128], [('src', s)], wbf, 'wbf', 8,
                         blk * 512, hbuf, zbuf)
        A.release()
        P.barrier()

    def phase_mlp(l, final):
        A.mark()
        big = [A.f32(4096) for _ in range(2)]
        bfb = [A.bf16(4096) for _ in range(2)]
        for k in range(8):
            s = k % 2
            DMA(big[s], w_up[l, k * 128:(k + 1) * 128, :], ('ld_big', s), [], [('big', s)])
            for c in range(4):
                o_, i_ = bfb[s][:, c * 1024:(c + 1) * 1024], big[s][:, c * 1024:(c + 1) * 1024]
                if c % 2 == 0:
                    DVE(lambda e, o_=o_, i_=i_: e.tensor_copy(o_, i_), [('big', s)], [('bfb', s)])
                elif c == 1:
                    ACT(o_, i_, AF.Copy, [('big', s)], [('bfb', s)])
                else:
                    POOL(lambda e, o_=o_, i_=i_: e.tensor_copy(o_, i_), [('big', s)], [('bfb', s)])
            DMA(wup16[:, k, :], bfb[s], ('st_bfb', s), [('bfb', s)], [('wup16', k)])
        A.release()
        P.barrier()
        A.mark()
        st = ln_setup(ln_ffn_g[l:l + 1, :], ln_ffn_b[l:l + 1, :], 'ffn')
        bdt = A.f32(D)
        DMA(bdt, bcast_row(b_down[l:l + 1, :], D), ('lnp', 'bd'), [], ['bias_t'])
        bupT = A.f32(32)
        DMA(bupT, b_up[l].rearrange("(j p) -> p j", p=128), ('lnp', 'bu'), [], ['bupT'], slow=True)
        stage = [A.f32(1024) for _ in range(2)]
        wdn = A.bf16(32 * 1024).rearrange("p (k n) -> p k n", k=32)
        load_w(wdn, lambda k: w_down[l, k * 128:(k + 1) * 128, :], 32, 1024, stage, 'wd', 'wdn', engs=('pool', 'act', 'dve'))
        NW = 3
        wup = [A.bf16(8 * 512).rearrange("p (k n) -> p k n", k=8) for _ in range(NW)]
        src = [A.bf16(8 * 512).rearrange("p (k t) -> p k t", k=8) for _ in range(2)]
        uT = A.bf16(32 * 512).rearrange("p (j t) -> p j t", j=32)
        rbuf = [A.f32(512) for _ in range(2)]
        hbuf = [A.f32(D) for _ in range(2)]
        zbuf = [A.f32(D) for _ in range(2)]
        NB = TOK // 512
        groups = [(blk, g) for blk in range(NB) for g in range(8)]
        issued = [0]
        src_issued = [0]

        def ensure(n):
            while issued[0] < min(n + NW, len(groups)):
                i = issued[0]
                blk_, g_ = groups[i]
                ws_ = i % NW
                DMA(wup[ws_], wup16[:, :, g_ * 512:(g_ + 1) * 512], ('ld_wup', ws_), [], [('wup', ws_)])
                issued[0] += 1

        def ensure_src(blk_):
            while src_issued[0] < min(blk_ + 2, NB):
                b_ = src_issued[0]
                DMA(src[b_ % 2], hT16[:, :, b_ * 512:(b_ + 1) * 512], ('ld_src', b_ % 2), [], [('src', b_ % 2)])
                src_issued[0] += 1

        gi = 0
        for blk in range(NB):
            s = blk % 2
            ensure_src(blk)
            for g in range(8):
                ensure(gi)
                ws = gi % NW
                gi += 1
                for jj in range(4):
                    j = g * 4 + jj
                    pb = j % 4
                    for k in range(8):
                        MM(bank(pb), wup[ws][:, k, jj * 128:(jj + 1) * 128], src[s][:, k, :], k == 0, k == 7,
                           [('wup', ws), ('src', s)], [('ps', pb)])
                    rs = j % 2
                    ACT(rbuf[rs], bank(pb), AF.Relu, [('ps', pb), 'bupT'], [('rb', rs)], bias=bupT[:, j:j + 1])
                    POOL(lambda e, rs=rs, j=j: e.tensor_tensor(uT[:, j, :], rbuf[rs], rbuf[rs], ALU.mult),
                         [('rb', rs)], ['uT'])
            res_ln_tiles(st, lambda k, tt: uT[:, k, tt * 128:(tt + 1) * 128], ['uT'], wdn, 'wdn', 32,
                         blk * 512, hbuf, zbuf, bias_tile=bdt, final=final, pairs=((4, 5), (0, 1), (2, 3)))
        A.release()
        P.barrier()

    def phase_mem(l):
        A.mark()
        st = ln_setup(ln_mem_g[l:l + 1, :], ln_mem_b[l:l + 1, :], 'mem')
        stage = [A.f32(1024) for _ in range(2)]
        wkv = A.bf16(8 * 2048).rearrange("p (k n) -> p k n", k=8)
        load_w(wkv[:, :, 0:1024], lambda k: w_mem_kv[l, k * 128:(k + 1) * 128, 0:1024], 8, 1024, stage, 'wkv', 'wkv')
        load_w(wkv[:, :, 1024:2048], lambda k: w_mem_kv[l, k * 128:(k + 1) * 128, 1024:2048], 8, 1024, stage, 'wkv', 'wkv')
        wq = A.bf16(8 * 1024).rearrange("p (k n) -> p k n", k=8)
        load_w(wq, lambda k: w_mem_q[l, k * 128:(k + 1) * 128, :], 8, 1024, stage, 'wkv', 'wq')
        wo = A.bf16(8 * 1024).rearrange("p (k n) -> p k n", k=8)
        load_w(wo, lambda k: w_mem_o[l, k * 128:(k + 1) * 128, :], 8, 1024, stage, 'wkv', 'wo')
        hbuf = [A.f32(D) for _ in range(2)]
        memf = hbuf
        membf = [A.bf16(D) for _ in range(2)]
        memT = A.bf16(8 * 256).rearrange("p (k m) -> p k m", k=8)
        kT = A.bf16(8 * 256).rearrange("p (c m) -> p c m", c=8)
        vv = A.bf16(2 * 1024).rearrange("p (m n) -> p m n", m=2)
        src = [A.bf16(8 * 512).rearrange("p (k t) -> p k t", k=8) for _ in range(2)]
        qT = A.bf16(8 * 512).rearrange("p (c t) -> p c t", c=8)
        E = [A.bf16(512) for _ in range(2)]
        rden = A.f32(512)
        omT = A.bf16(8 * 512).rearrange("p (c t) -> p c t", c=8)
        zbuf = [A.f32(D) for _ in range(2)]
        for b in range(NSEQ):
            for mt in range(2):
                DMA(memf[mt], mem_d[b * MEMT + mt * 128:b * MEMT + (mt + 1) * 128, :], ('ld_mem', mt), [], [('hb', mt)])
                ACT(membf[mt], memf[mt], AF.Copy, [('hb', mt)], [('membf', mt)])
                pv = bankbf(mt)
                for k in range(8):
                    TR(pv[:, k * 128:(k + 1) * 128], membf[mt][:, k * 128:(k + 1) * 128], ident_bf,
                       [('membf', mt), 'cbf'], [('ps', mt)])
                DVE(lambda e, mt=mt, pv=pv: e.tensor_copy(memT[:, :, mt * 128:(mt + 1) * 128],
                                                       pv.rearrange("p (k t) -> p k t", k=8)),
                    [('ps', mt)], ['memT'])
            for c in range(8):
                pb = c % 2
                for k in range(8):
                    MM(bank(pb, 256), wkv[:, k, c * 128:(c + 1) * 128], memT[:, k, :], k == 0, k == 7,
                       ['wkv', 'memT'], [('ps', pb)])
                DVE(lambda e, c=c, pb=pb: e.tensor_copy(kT[:, c, :], bank(pb, 256)), [('ps', pb)], ['kT'])
            for mt in range(2):
                for nh in range(2):
                    pb = 2 + nh
                    for k in range(8):
                        MM(bank(pb), memT[:, k, mt * 128:(mt + 1) * 128], wkv[:, k, 1024 + nh * 512:1024 + (nh + 1) * 512],
                           k == 0, k == 7, ['wkv', 'memT'], [('ps', pb)])
                    ACT(vv[:, mt, nh * 512:(nh + 1) * 512], bank(pb), AF.Copy, [('ps', pb)], ['vv'])
            for bb in range(4):
                blk = b * 4 + bb
                s = blk % 2
                DMA(src[s], hT16[:, :, blk * 512:(blk + 1) * 512], ('ld_src', s), [('hT16', blk)], [('src', s)])
                for c in range(8):
                    pb = c % 2
                    for k in range(8):
                        MM(bank(pb), wq[:, k, c * 128:(c + 1) * 128], src[s][:, k, :], k == 0, k == 7,
                           ['wq', ('src', s)], [('ps', pb)])
                    if c % 2 == 0:
                        ACT(qT[:, c, :], bank(pb), AF.Copy, [('ps', pb)], ['qT'])
                    else:
                        DVE(lambda e, c=c, pb=pb: e.tensor_copy(qT[:, c, :], bank(pb)), [('ps', pb)], ['qT'])
                for h in range(4):
                    for mt in range(2):
                        pb = mt
                        for dc in range(2):
                            MM(bank(pb), kT[:, h * 2 + dc, mt * 128:(mt + 1) * 128], qT[:, h * 2 + dc, :], dc == 0, dc == 1,
                               ['kT', 'qT'], [('ps', pb)])
                        ACT(E[mt], bank(pb), AF.Exp, [('ps', pb)], [('E', mt)], scale=1.0 / 16.0)
                    for mt in range(2):
                        MM(bank(2), ones_bf, E[mt], mt == 0, mt == 1, [('E', mt), 'cbf'], [('ps', 2)])
                    DVE(lambda e: e.reciprocal(rden, bank(2)), [('ps', 2)], ['rden'])
                    for dc in range(2):
                        pb = 3
                        c = h * 2 + dc
                        for mt in range(2):
                            MM(bank(pb), vv[:, mt, c * 128:(c + 1) * 128], E[mt], mt == 0, mt == 1,
                               ['vv', ('E', mt)], [('ps', pb)])
                        DVE(lambda e, c=c, pb=pb: e.tensor_tensor(omT[:, c, :], bank(pb), rden, ALU.mult),
                            [('ps', pb), 'rden'], ['omT'])
                res_ln_tiles(st, lambda k, tt: omT[:, k, tt * 128:(tt + 1) * 128], ['omT'], wo, 'wo', 8,
                             blk * 512, hbuf, zbuf)
        A.release()
        P.barrier()

    def phase_mixer_zero():
        A.mark()
        zt = A.bf16(8 * 512)
        POOL(lambda e: e.memset(zt, 0.0), [], ['zt'])
        for blk in range(TOK // 512):
            DMA(oT16[:, :, blk * 512:(blk + 1) * 512], zt.rearrange("p (k t) -> p k t", k=8), 'st_z', ['zt'], [('oT16', blk)])
        A.release()
        P.barrier()

    ctx = dict(nc=nc, P=P, A=A, ps=ps, bank=bank, bankbf=bankbf, MM=MM, TR=TR, ACT=ACT, DVE=DVE, POOL=POOL, DMA=DMA,
               cst=cst, cbf=cbf, cst_d=cst_d, ident_bf=ident_bf, ones_bf=ones_bf, tri_bf=tri_bf, load_w=load_w,
               w_in=w_in, gate_w2=gate_w2, gate_b=gate_b, norm_g=norm_g, w_uv=w_uv, hT16=hT16, oT16=oT16,
               bcast_row=bcast_row, one_t=one_t, eps_t=eps_t, x_d=x_d)

    phases = phases or ['ln_in', 'mix', 'wout', 'mem', 'mlp']
    if 'ln_in' in phases:
        phase_ln_in()
    for l in range(DEPTH):
        if 'mix' in phases:
            phase_mixer(ctx, l, MIX_PARTS)
        elif 'mixzero' in phases:
            phase_mixer_zero()
        if 'wout' in phases:
            phase_wout(l)
        if 'mem' in phases:
            phase_mem(l)
        if 'mlp' in phases:
            phase_mlp(l, final=(l == DEPTH - 1) or bool(dbg and dbg.get('layers', DEPTH) == l + 1))
        if dbg and dbg.get('layers', DEPTH) == l + 1:
            break
    P.emit()
    return nc, A


def phase_mixer(ctx, l, parts=('gla', 'dsa', 'sb')):
    for b in range(NSEQ):
        mixer_seq(ctx, l, b, parts)
        ctx['P'].barrier()


def mixer_seq(ctx, l, b, parts):
    nc = ctx['nc']; P = ctx['P']; A = ctx['A']; ps = ctx['ps']
    bank = ctx['bank']; bankbf = ctx['bankbf']
    MM = ctx['MM']; TR = ctx['TR']; ACT = ctx['ACT']; DVE = ctx['DVE']; POOL = ctx['POOL']; DMA = ctx['DMA']
    cst = ctx['cst']; cbf = ctx['cbf']; ident_bf = ctx['ident_bf']; ones_bf = ctx['ones_bf']; tri_bf = ctx['tri_bf']
    load_w = ctx['load_w']; w_in = ctx['w_in']; hT16 = ctx['hT16']; oT16 = ctx['oT16']
    bcast_row = ctx['bcast_row']; one_t = ctx['one_t']; cst_d = ctx['cst_d']
    T0 = b * SEQ

    def V3(ap, a):
        return ap.rearrange("p (a b) -> p a b", a=a)

    A.mark()
    dqT = V3(A.bf16(3 * 2048), 3); dkT2 = A.bf16(2048)
    vlat = V3(A.bf16(16 * 128), 16)
    iqT = V3(A.bf16(4 * 2048), 4); ikT2 = A.bf16(2048)
    iw = V3(A.f32(16 * 8), 16)
    sqT = V3(A.bf16(3 * 2048), 3); skT = V3(A.bf16(3 * 2048), 3); skTn = V3(A.bf16(3 * 2048), 3)
    sv2f = A.bf16(16 * 5 * 128)
    sv2 = sv2f.rearrange("p (a h c) -> p a h c", a=16, h=5)
    POOL(lambda e: e.memset(sv2f, 0.0), [], ['sv2'])
    neg8 = A.bf16(512)
    POOL(lambda e: e.memset(neg8, -0.125), [], ['neg8'])

    A.mark()
    hT = V3(A.bf16(8 * 2048), 8)
    for q in range(4):
        DMA(hT[:, :, q * 512:(q + 1) * 512], hT16[:, :, T0 + q * 512:T0 + (q + 1) * 512], ('ld_hT', q), [], [('hT', q)])
    stage = [A.f32(1168) for _ in range(2)]
    wbf = V3(A.bf16(8 * 1168), 8)
    pbc = [0]
    evc = [0]

    def nb():
        pbc[0] = (pbc[0] + 1) % 4
        return pbc[0]

    def evac(out, in_, R, W, scale=None):
        evc[0] += 1
        if scale is not None:
            P.add('act', lambda e: e.mul(out, in_, scale), R, W)
        elif evc[0] % 2 == 0:
            ACT(out, in_, AF.Copy, R, W)
        else:
            DVE(lambda e: e.tensor_copy(out, in_), R, W)

    def proj_fm(c0, M, ev, wsrc=None, wkey='wbf'):
        wsrc = wbf if wsrc is None else wsrc
        for tc in range(4):
            pb = nb()
            for k in range(8):
                MM(ps[0:M, pb * 512:(pb + 1) * 512], wsrc[:, k, c0:c0 + M], hT[:, k, tc * 512:(tc + 1) * 512],
                   k == 0, k == 7, [wkey, ('hT', tc)], [('ps', pb)])
            ev(pb, tc)

    def proj_tm(c0, N, ev):
        for tt in range(16):
            pb = nb()
            for k in range(8):
                MM(ps[:, pb * 512:pb * 512 + N], hT[:, k, tt * 128:(tt + 1) * 128], wbf[:, k, c0:c0 + N],
                   k == 0, k == 7, ['wbf', ('hT', tt // 4)], [('ps', pb)])
            ev(pb, tt)

    if 'sb' in parts or 'sbproj' in parts:
        load_w(wbf[:, :, 0:960], lambda k: w_in[l, k * 128:(k + 1) * 128, 2264:3224], 8, 960, stage, 'win', 'wbf')
        import os
        lvl = int(os.environ.get("SBLVL", "9"))
        for p in range(3):
            M = 128 if p < 2 else 64
            if lvl < 2 or (lvl < 3 and p == 2):
                continue
            proj_fm(p * 128, M, lambda pb, tc, p=p, M=M: evac(sqT[0:M, p, tc * 512:(tc + 1) * 512],
                                                          ps[0:M, pb * 512:(pb + 1) * 512], [('ps', pb)], ['sqT']))

            def ev_k(pb, tc, p=p, M=M):
                evac(skT[0:M, p, tc * 512:(tc + 1) * 512], ps[0:M, pb * 512:(pb + 1) * 512], [('ps', pb)], ['skT'])
                POOL(lambda e, p=p, M=M, tc=tc: e.tensor_tensor(skTn[0:M, p, tc * 512:(tc + 1) * 512],
                                                                skT[0:M, p, tc * 512:(tc + 1) * 512], neg8[0:M, :], ALU.mult),
                     ['skT', 'neg8'], ['skTn'])
            if lvl >= 4:
                proj_fm(320 + p * 128, M, ev_k)

        def ev_v(pb, tt):
            pv = ps[:, pb * 512:pb * 512 + 320].rearrange("p (h c) -> p h c", h=5)
            evac(sv2[:, tt, 1::2, 0:64], pv[:, 1::2, :], [('ps', pb)], ['sv2'])
            evac(sv2[:, tt, 0::2, 64:128], pv[:, 0::2, :], [('ps', pb)], ['sv2'])
        if lvl >= 5:
            proj_tm(640, 320, ev_v)

    if 'dsa' in parts:
        load_w(wbf[:, :, 0:1096], lambda k: w_in[l, k * 128:(k + 1) * 128, 1168:2264], 8, 1096, stage, 'win', 'wbf')
        A.mark()
        wdk = V3(A.bf16(8 * 128), 8); wik = V3(A.bf16(8 * 128), 8)
        for hh in range(2):
            POOL(lambda e, hh=hh: e.tensor_copy(wdk[:, :, hh * 64:(hh + 1) * 64], wbf[:, :, 320:384]), ['wbf'], ['wdk'])
            POOL(lambda e, hh=hh: e.tensor_copy(wik[:, :, hh * 64:(hh + 1) * 64], wbf[:, :, 1024:1088]), ['wbf'], ['wik'])
        for p in range(3):
            M = 128 if p < 2 else 64
            proj_fm(p * 128, M, lambda pb, tc, p=p, M=M: evac(dqT[0:M, p, tc * 512:(tc + 1) * 512],
                                                          ps[0:M, pb * 512:(pb + 1) * 512], [('ps', pb)], ['dqT']))
        proj_fm(0, 128, lambda pb, tc: evac(dkT2[:, tc * 512:(tc + 1) * 512], bank(pb), [('ps', pb)], ['dkT2']),
                wsrc=wdk, wkey='wdk')
        proj_fm(0, 128, lambda pb, tc: evac(ikT2[:, tc * 512:(tc + 1) * 512], bank(pb), [('ps', pb)], ['ikT2']),
                wsrc=wik, wkey='wik')
        for p in range(4):
            proj_fm(512 + p * 128, 128, lambda pb, tc, p=p: evac(iqT[:, p, tc * 512:(tc + 1) * 512], bank(pb),
                                                              [('ps', pb)], ['iqT']))
        proj_tm(384, 128, lambda pb, tt: evac(vlat[:, tt, :], ps[:, pb * 512:pb * 512 + 128], [('ps', pb)], ['vlat']))
        proj_tm(1088, 8, lambda pb, tt: evac(iw[:, tt, :], ps[:, pb * 512:pb * 512 + 8], [('ps', pb)], ['iw']))
        A.release()
        P.barrier()

    if 'gla' in parts:
        gla_seq(ctx, l, b, hT, wbf, stage, nb, evac, proj_fm)
    else:
        zero_o(ctx, T0, 0, 3, 0, 128)
    A.release()
    P.barrier()

    if 'dsa' in parts:
        dsa_seq(ctx, l, b, dqT, dkT2, vlat, iqT, ikT2, iw)
    else:
        zero_o(ctx, T0, 3, 5, 0, 128)
        zero_o(ctx, T0, 5, 6, 0, 64)
    P.barrier()
    if 'sb' in parts:
        sb_seq(ctx, l, b, sqT, skT, skTn, sv2)
    else:
        zero_o(ctx, T0, 5, 6, 64, 128)
        zero_o(ctx, T0, 6, 8, 0, 128)
    A.release()


def zero_o(ctx, T0, c0, c1, p0, p1):
    A = ctx['A']; POOL = ctx['POOL']; DMA = ctx['DMA']; oT16 = ctx['oT16']
    n = c1 - c0
    zt = A.bf16(n * 512)
    key = ('zt', c0, p0)
    POOL(lambda e: e.memset(zt, 0.0), [], [key])
    for q in range(4):
        DMA(oT16[p0:p1, c0:c1, T0 + q * 512:T0 + (q + 1) * 512], zt[p0:p1, :].rearrange("p (k t) -> p k t", k=n),
            ('st_zt', c0, p0), [key], [('oT16z', c0, p0, q)])


def gla_seq(ctx, l, b, hT, wbf, stage, nb, evac, proj_fm):
    nc = ctx['nc']; P = ctx['P']; A = ctx['A']; ps = ctx['ps']
    bank = ctx['bank']; bankbf = ctx['bankbf']
    MM = ctx['MM']; TR = ctx['TR']; ACT = ctx['ACT']; DVE = ctx['DVE']; POOL = ctx['POOL']; DMA = ctx['DMA']
    cst = ctx['cst']; cbf = ctx['cbf']; ident_bf = ctx['ident_bf']
    load_w = ctx['load_w']; w_in = ctx['w_in']; oT16 = ctx['oT16']
    bcast_row = ctx['bcast_row']; one_t = ctx['one_t']
    gate_w2 = ctx['gate_w2']; gate_b = ctx['gate_b']; norm_g = ctx['norm_g']
    T0 = b * SEQ
    QS = 48 ** -0.5

    def V3(ap, a):
        return ap.rearrange("p (a b) -> p a b", a=a)

    load_w(wbf[:, :, 0:1168], lambda k: w_in[l, k * 128:(k + 1) * 128, 0:1168], 8, 1168, stage, 'win', 'wbf')
    gw2f = A.f32(192); gw2b = A.bf16(192)
    DMA(gw2f[0:16, :], gate_w2[l], 'ld_gw2', [], ['gw2f'])
    POOL(lambda e: e.tensor_copy(gw2b[0:16, :], gw2f[0:16, :]), ['gw2f'], ['gw2b'])
    gb = A.f32(192)
    DMA(gb, bcast_row(gate_b[l:l + 1, :], 192), 'ld_gb', [], ['gb'])
    ng4 = A.f32(384)
    for h in range(4):
        DMA(ng4[:, h * 96:(h + 1) * 96], bcast_row(norm_g[l:l + 1, :], 96), 'ld_ng', [], ['ng4'])
    bt4 = A.f32(512)
    for h in range(4):
        POOL(lambda e, h=h: e.tensor_copy(bt4[:, h * 128:(h + 1) * 128], cst[:, C_BT:C_BT + 128]), ['cst'], ['bt4'])
    glrT = A.bf16(2048)
    proj_fm(768, 16, lambda pb, tc: evac(glrT[0:16, tc * 512:(tc + 1) * 512], ps[0:16, pb * 512:(pb + 1) * 512],
                                         [('ps', pb)], ['glrT']))
    S = A.f32(384); Smid = A.f32(384); S2 = A.bf16(384)
    POOL(lambda e: e.memset(S, 0.0), [], ['S'])
    POOL(lambda e: e.memset(S2, 0.0), [], ['S2'])
    qt2 = A.bf16(512); kt2 = A.bf16(512); kend2 = A.bf16(512); sp2 = A.bf16(512)
    for t_, k_ in ((qt2, 'qt2'), (kt2, 'kt2'), (kend2, 'kend2'), (sp2, 'sp2')):
        POOL(lambda e, t_=t_: e.memset(t_, 0.0), [], [k_])
    qt2v = V3(qt2, 4); kt2v = V3(kt2, 4); kend2v = V3(kend2, 4); sp2v = V3(sp2, 4)
    vbf = A.bf16(384); sg = A.f32(384)
    xb = A.f32(192); ebuf = A.f32(192); spf = A.f32(192); spb = A.bf16(192)
    E1 = A.f32(192); E2 = A.f32(192); E3 = A.f32(192); totS = A.f32(192); dd = A.f32(192)
    dec = A.f32(8); qkT = A.bf16(1024); scm = A.bf16(512)
    ssq = A.f32(4); rstd4 = A.f32(4); junk = A.f32(96); t1 = A.f32(384); og = A.bf16(384)
    oTg = [V3(A.bf16(3 * 512), 3) for _ in range(2)]
    bo_bf = cbf[:, C_BO:C_BO + 128]; bt_bf = cbf[:, C_BT:C_BT + 128]; cs_bf = cbf[:, C_CS:C_CS + 2]

    def h4(ap, r0=0, r1=128):
        return ap[r0:r1, :].rearrange("p (h d) -> p h d", h=4)

    for tt in range(16):
        hk = ('hT', tt // 4)
        tsl = slice(tt * 128, (tt + 1) * 128)
        for k in range(8):
            MM(ps[:, 0:384], hT[:, k, tsl], wbf[:, k, 0:384], k == 0, k == 7, ['wbf', hk], [('ps', 0)])
        for k in range(8):
            MM(ps[:, 512:512 + 384], hT[:, k, tsl], wbf[:, k, 384:768], k == 0, k == 7, ['wbf', hk], [('ps', 1)])
        ACT(vbf, ps[:, 512:512 + 384], AF.Copy, [('ps', 1)], ['vbf'])
        for k in range(8):
            MM(ps[:, 512:512 + 384], hT[:, k, tsl], wbf[:, k, 784:1168], k == 0, k == 7, ['wbf', hk], [('ps', 1)])
        ACT(sg, ps[:, 512:512 + 384], AF.Silu, [('ps', 1)], ['sg'])
        MM(ps[:, 1024:1024 + 192], glrT[0:16, tsl], gw2b[0:16, :], True, True, ['glrT', 'gw2b'], [('ps', 2)])
        DVE(lambda e: e.tensor_tensor(xb, ps[:, 1024:1024 + 192], gb, ALU.add), [('ps', 2), 'gb'], ['xb'])
        ACT(ebuf, xb, AF.Exp, ['xb'], ['ebuf'], scale=-1.0)
        ACT(spf, ebuf, AF.Ln, ['ebuf', 'kc'], ['spf'], bias=one_t)
        POOL(lambda e: e.tensor_copy(spb, spf), ['spf'], ['spb'])
        POOL(lambda e: e.tensor_copy(sp2v[:, :, 0:48], h4(spf)), ['spf'], ['sp2'])
        POOL(lambda e: e.tensor_copy(sp2v[:, :, 64:112], h4(spf)), ['spf'], ['sp2'])
        MM(ps[:, 1024:1024 + 192], bt_bf, spb, True, True, ['spb', 'cbf'], [('ps', 2)])
        MM(ps[:, 1024 + 192:1024 + 384], bo_bf, spb, True, True, ['spb', 'cbf'], [('ps', 2)])
        for h in range(4):
            MM(ps[:, 1536 + 2 * h:1536 + 2 * h + 2], sp2[:, h * 128:(h + 1) * 128], cs_bf, True, True,
               ['sp2', 'cbf'], [('ps', 3)])
        ACT(dec, ps[:, 1536:1536 + 8], AF.Exp, [('ps', 3)], ['dec'], scale=-1.0 / 16)
        cum = ps[:, 1024:1024 + 192]; tot = ps[:, 1024 + 192:1024 + 384]
        ACT(E1, cum, AF.Exp, [('ps', 2)], ['E1'], scale=-1.0 / 16)
        ACT(E2, cum, AF.Exp, [('ps', 2)], ['E2'], scale=1.0 / 16)
        ACT(totS, tot, AF.Copy, [('ps', 2)], ['totS'])
        DVE(lambda e: e.tensor_tensor(dd, cum, totS, ALU.subtract), [('ps', 2), 'totS'], ['dd'])
        ACT(E3, dd, AF.Exp, ['dd'], ['E3'], scale=1.0 / 16)
        qps = ps[:, 0:192]; kps = ps[:, 192:384]
        for half in range(2):
            r0 = 64 * half
            DVE(lambda e, r0=r0, half=half: e.scalar_tensor_tensor(
                qt2v[r0:r0 + 64, :, 64 * half:64 * half + 48], h4(qps, r0, r0 + 64), QS, h4(E1, r0, r0 + 64),
                ALU.mult, ALU.mult), [('ps', 0), 'E1'], ['qt2'])
        for cp in (0, 64):
            DVE(lambda e, cp=cp: e.tensor_tensor(kt2v[:, :, cp:cp + 48], h4(kps), h4(E2), ALU.mult),
                [('ps', 0), 'E2'], ['kt2'])
            DVE(lambda e, cp=cp: e.tensor_tensor(kend2v[:, :, cp:cp + 48], h4(kps), h4(E3), ALU.mult),
                [('ps', 0), 'E3'], ['kend2'])
        pv = bankbf(3)
        for h in range(4):
            TR(pv[:, (2 * h) * 128:(2 * h + 1) * 128], qt2[:, h * 128:(h + 1) * 128], ident_bf, ['qt2', 'cbf'], [('ps', 3)])
            TR(pv[:, (2 * h + 1) * 128:(2 * h + 2) * 128], kt2[:, h * 128:(h + 1) * 128], ident_bf, ['kt2', 'cbf'], [('ps', 3)])
        ACT(qkT, pv, AF.Copy, [('ps', 3)], ['qkT'])
        for h in range(4):
            MM(ps[:, 2048 + h * 128:2048 + (h + 1) * 128], qkT[:, (2 * h + 1) * 128:(2 * h + 2) * 128],
               qkT[:, (2 * h) * 128:(2 * h + 1) * 128], True, True, ['qkT'], [('ps', 4)])
        DVE(lambda e: e.tensor_tensor(scm, ps[:, 2048:2560], bt4, ALU.mult), [('ps', 4), 'bt4'], ['scm'])
        for h in range(4):
            MM(ps[:, 3072 + h * 96:3072 + (h + 1) * 96], kend2[0:64, h * 128:(h + 1) * 128], vbf[0:64, h * 96:(h + 1) * 96],
               True, True, ['kend2', 'vbf'], [('ps', 6)])
        for h in range(4):
            MM(ps[:, 3584 + h * 96:3584 + (h + 1) * 96], kend2[64:128, h * 128:(h + 1) * 128], vbf[64:128, h * 96:(h + 1) * 96],
               True, True, ['kend2', 'vbf'], [('ps', 7)])
        POOL(lambda e: e.tensor_copy(S2[0:48, :], S[0:48, :]), ['S'], ['S2'])
        for h in range(4):
            DVE(lambda e, h=h: e.scalar_tensor_tensor(Smid[:, h * 96:(h + 1) * 96], S[:, h * 96:(h + 1) * 96],
                                                      dec[:, 2 * h:2 * h + 1], ps[:, 3072 + h * 96:3072 + (h + 1) * 96],
                                                      ALU.mult, ALU.add), ['S', 'dec', ('ps', 6)], ['Smid'])
        POOL(lambda e: e.tensor_copy(S2[64:112, :], Smid[64:112, :]), ['Smid'], ['S2'])
        for h in range(4):
            DVE(lambda e, h=h: e.scalar_tensor_tensor(S[:, h * 96:(h + 1) * 96], Smid[:, h * 96:(h + 1) * 96],
                                                      dec[:, 2 * h + 1:2 * h + 2], ps[:, 3584 + h * 96:3584 + (h + 1) * 96],
                                                      ALU.mult, ALU.add), ['Smid', 'dec', ('ps', 7)], ['S'])
        for h in range(4):
            MM(ps[:, 2560 + h * 96:2560 + (h + 1) * 96], scm[:, h * 128:(h + 1) * 128], vbf[:, h * 96:(h + 1) * 96],
               True, False, ['scm', 'vbf'], [('ps', 5)])
            MM(ps[:, 2560 + h * 96:2560 + (h + 1) * 96], qkT[:, (2 * h) * 128:(2 * h + 1) * 128], S2[:, h * 96:(h + 1) * 96],
               False, True, ['qkT', 'S2'], [('ps', 5)])
        POOL(lambda e: e.memset(ssq, 0.0), [], ['ssq'])
        for h in range(4):
            ACT(junk, ps[:, 2560 + h * 96:2560 + (h + 1) * 96], AF.Square, [('ps', 5)], ['junk', 'ssq'], accum=ssq[:, h:h + 1])
        DVE(lambda e: e.tensor_scalar(rstd4, ssq, 1.0 / 96, LN_EPS, ALU.mult, ALU.add), ['ssq'], ['rstd4'])
        ACT(rstd4, rstd4, AF.Ln, ['rstd4'], ['rstd4'])
        ACT(rstd4, rstd4, AF.Exp, ['rstd4'], ['rstd4'], scale=-0.5)
        for h in range(4):
            DVE(lambda e, h=h: e.tensor_scalar(t1[:, h * 96:(h + 1) * 96], ps[:, 2560 + h * 96:2560 + (h + 1) * 96],
                                               rstd4[:, h:h + 1], 1.0, ALU.mult, ALU.mult), [('ps', 5), 'rstd4'], ['t1'])
        POOL(lambda e: e.tensor_tensor(t1, t1, ng4, ALU.mult), ['t1', 'ng4'], ['t1'])
        POOL(lambda e: e.tensor_tensor(og, t1, sg, ALU.mult), ['t1', 'sg'], ['og'])
        pv2 = bankbf(4)
        for c in range(3):
            TR(pv2[:, c * 128:(c + 1) * 128], og[:, c * 128:(c + 1) * 128], ident_bf, ['og', 'cbf'], [('ps', 4)])
        blk = tt // 4; q = tt % 4; s = blk % 2
        ACT(oTg[s][:, :, q * 128:(q + 1) * 128], pv2[:, 0:384].rearrange("p (c t) -> p c t", c=3), AF.Copy,
            [('ps', 4)], [('oTg', s)])
        if q == 3:
            DMA(oT16[:, 0:3, T0 + blk * 512:T0 + (blk + 1) * 512], oTg[s], ('st_oTg', s), [('oTg', s)], [('oT16g', blk)])


def dsa_seq(ctx, l, b, dqT, dkT2, vlat, iqT, ikT2, iw):
    nc = ctx['nc']; P = ctx['P']; A = ctx['A']; ps = ctx['ps']
    bank = ctx['bank']; bankbf = ctx['bankbf']
    MM = ctx['MM']; TR = ctx['TR']; ACT = ctx['ACT']; DVE = ctx['DVE']; POOL = ctx['POOL']; DMA = ctx['DMA']
    cst = ctx['cst']; cbf = ctx['cbf']; ident_bf = ctx['ident_bf']; ones_bf = ctx['ones_bf']
    oT16 = ctx['oT16']; w_uv = ctx['w_uv']; cst_d = ctx['cst_d']
    T0 = b * SEQ

    def V3(ap, a):
        return ap.rearrange("p (a b) -> p a b", a=a)

    A.mark()
    dist = A.f32(2048)
    DMA(dist, cst_d[:, C_DIST:C_DIST + 2048], 'ld_dist', [], ['dist'])
    Th = [A.bf16(2048) for _ in range(5)]
    for h in range(5):
        ACT(Th[h], dist, AF.Exp, ['dist'], [('Th', h)], scale=-SLOPES[h])
    wuvf = A.f32(5 * 64)
    for h in range(5):
        DMA(wuvf[:, h * 64:(h + 1) * 64], w_uv[l, h], 'ld_wuv', [], ['wuvf'])
    wuv2f = A.bf16(5 * 128)
    wuv2 = V3(wuv2f, 5)
    POOL(lambda e: e.memset(wuv2f, 0.0), [], ['wuv2'])
    for h in range(5):
        bs = 64 * (h % 2)
        POOL(lambda e, h=h, bs=bs: e.tensor_copy(wuv2[:, h, bs:bs + 64], wuvf[:, h * 64:(h + 1) * 64]), ['wuvf'], ['wuv2'])
    isc = A.f32(2048)
    rbuf = [A.f32(512) for _ in range(2)]
    thr = A.f32(1)
    KB = 20
    pow2tab = A.f32(KB + 1); w_all = A.f32(KB + 1)
    for k in range(KB + 1):
        POOL(lambda e, k=k: e.memset(pow2tab[:, k:k + 1], 2.0 ** -(k + 1)), [], ['pow2tab'])
    bs_hi = A.f32(1); bs_lo = A.f32(1); bs_w0 = A.f32(1); mid = A.f32(1); cnt = A.f32(1); tt_ = A.f32(1)
    maskrow = A.bf16(2048)
    maskTs = [V3(A.bf16(16 * 512), 16) for _ in range(2)]
    Eb = [A.bf16(512) for _ in range(2)]
    Pm = [A.bf16(512) for _ in range(2)]
    rden = A.f32(512)
    olat = [A.bf16(512) for _ in range(2)]
    oTd = [A.bf16(512) for _ in range(2)]
    negI = cst[:, C_NEGI:C_NEGI + 128]
    ric = [0]

    def gen_isc(tg):
        maskT = maskTs[tg % 2]
        mkey = ('maskT', tg % 2)
        for ti in range(4):
            i = 4 * tg + ti
            ncols = (i + 1) * 128
            tsl = slice(i * 128, (i + 1) * 128)
            for h in range(8):
                hp, bs = h // 2, 64 * (h % 2)
                for sc in range(0, ncols, 512):
                    w = min(512, ncols - sc)
                    pb = ric[0] % 2
                    rb = ric[0] % 2
                    ric[0] += 1
                    MM(ps[:, pb * 512:pb * 512 + w], iqT[bs:bs + 64, hp, tsl], ikT2[bs:bs + 64, sc:sc + w], True, True,
                       ['iqT', 'ikT2'], [('ps', pb)])
                    ACT(rbuf[rb][:, 0:w], ps[:, pb * 512:pb * 512 + w], AF.Relu, [('ps', pb)], [('rbuf', rb)])
                    if h == 0:
                        DVE(lambda e, rb=rb, sc=sc, w=w, i=i, h=h: e.tensor_scalar(
                            isc[:, sc:sc + w], rbuf[rb][:, 0:w], iw[:, i, h:h + 1], 1.0, ALU.mult, ALU.mult),
                            [('rbuf', rb), 'iw'], ['isc'])
                    else:
                        DVE(lambda e, rb=rb, sc=sc, w=w, i=i, h=h: e.scalar_tensor_tensor(
                            isc[:, sc:sc + w], rbuf[rb][:, 0:w], iw[:, i, h:h + 1], isc[:, sc:sc + w], ALU.mult, ALU.add),
                            [('rbuf', rb), 'iw', 'isc'], ['isc'])
                if h == 3:
                    yield
            if i >= 2:
                DVE(lambda e, ncols=ncols: e.tensor_reduce(bs_lo, isc[:, 0:ncols], mybir.AxisListType.X, ALU.min), ['isc'], ['bs_lo'])
            POOL(lambda e, i=i: e.tensor_tensor(isc[:, i * 128:(i + 1) * 128], isc[:, i * 128:(i + 1) * 128], negI, ALU.add),
                 ['isc', 'cst'], ['isc'])
            if i >= 2:
                DVE(lambda e, ncols=ncols: e.tensor_reduce(bs_hi, isc[:, 0:ncols], mybir.AxisListType.X, ALU.max), ['isc'], ['bs_hi'])
                DVE(lambda e: e.tensor_tensor(bs_w0, bs_hi, bs_lo, ALU.subtract), ['bs_hi', 'bs_lo'], ['bs_w0'])
                DVE(lambda e: e.tensor_scalar(w_all, pow2tab, bs_w0, 1.0, ALU.mult, ALU.mult), ['bs_w0', 'pow2tab'], ['w_all'])
                DVE(lambda e: e.tensor_tensor(mid, bs_lo, w_all[:, 0:1], ALU.add), ['bs_lo', 'w_all'], ['mid'])
                for k in range(KB):
                    DVE(lambda e, ncols=ncols: e.tensor_scalar(maskrow[:, 0:ncols], isc[:, 0:ncols], mid, None, ALU.is_ge, ALU.add,
                                                               accum_out=cnt), ['isc', 'mid'], ['maskrow', 'cnt'])
                    DVE(lambda e, k=k: e.scalar_tensor_tensor(tt_, cnt, 255.5, w_all[:, k:k + 1], ALU.is_ge, ALU.mult),
                        ['cnt', 'w_all'], ['tt_'])
                    DVE(lambda e, k=k: e.scalar_tensor_tensor(mid, tt_, w_all[:, k + 1:k + 2], mid, ALU.subtract, ALU.add),
                        ['tt_', 'w_all', 'mid'], ['mid'])
                    if k % 5 == 4:
                        yield
                DVE(lambda e: e.tensor_tensor(thr, mid, w_all[:, KB:KB + 1], ALU.subtract), ['mid', 'w_all'], ['thr'])
            else:
                DVE(lambda e: e.memset(thr, -1.0e29), [], ['thr'])
            DVE(lambda e, ncols=ncols: e.tensor_scalar(maskrow[:, 0:ncols], isc[:, 0:ncols], thr, 1.0, ALU.is_ge, ALU.mult),
                ['isc', 'thr'], ['maskrow'])
            for j0 in range(0, i + 1, 8):
                n = min(8, i + 1 - j0)
                pvm = bankbf(2)
                for jj in range(n):
                    j = j0 + jj
                    TR(pvm[:, jj * 128:(jj + 1) * 128], maskrow[:, j * 128:(j + 1) * 128], ident_bf, ['maskrow', 'cbf'], [('ps', 2)])
                ACT(maskT[:, j0:j0 + n, ti * 128:(ti + 1) * 128], pvm[:, 0:n * 128].rearrange("p (j t) -> p j t", j=n),
                    AF.Copy, [('ps', 2)], [mkey])
            yield

    def gen_att(tg):
        maskT = maskTs[tg % 2]
        mkey = ('maskT', tg % 2)
        jmax = 4 * tg + 3
        for h in range(5):
            hp, bs = h // 2, 64 * (h % 2)
            for j in range(jmax + 1):
                c0 = max(j - 4 * tg, 0) * 128
                w = 512 - c0
                tq0 = 512 * tg + c0
                pb = 3 + (j % 2)
                s = j % 2
                MM(ps[:, pb * 512:pb * 512 + w], dkT2[bs:bs + 64, j * 128:(j + 1) * 128], dqT[bs:bs + 64, hp, tq0:tq0 + w],
                   True, True, ['dkT2', 'dqT'], [('ps', pb)])
                ACT(Eb[s][:, 0:w], ps[:, pb * 512:pb * 512 + w], AF.Exp, [('ps', pb)], [('Eb', s)], scale=0.125)
                POOL(lambda e, s=s, w=w, j=j, c0=c0: e.tensor_tensor(Pm[s][:, 0:w], Eb[s][:, 0:w], maskT[:, j, c0:c0 + w],
                                                                    ALU.mult), [('Eb', s), mkey], [('Pm', s)])
                m0 = tq0 - 128 * j
                POOL(lambda e, s=s, w=w, m0=m0, h=h: e.tensor_tensor(Pm[s][:, 0:w], Pm[s][:, 0:w], Th[h][:, m0:m0 + w],
                                                                    ALU.mult), [('Pm', s), ('Th', h)], [('Pm', s)])
                MM(ps[:, 5 * 512 + c0:5 * 512 + 512], vlat[:, j, :], Pm[s][:, 0:w], j == 0, j == jmax,
                   ['vlat', ('Pm', s)], [('ps', 5)])
                MM(ps[:, 6 * 512 + c0:6 * 512 + 512], ones_bf, Pm[s][:, 0:w], j == 0, j == jmax,
                   ['cbf', ('Pm', s)], [('ps', 6)])
                if j % 4 == 3:
                    yield
            DVE(lambda e: e.reciprocal(rden, bank(6)), [('ps', 6)], ['rden'])
            os_ = h % 2
            DVE(lambda e, os_=os_: e.tensor_tensor(olat[os_], bank(5), rden, ALU.mult), [('ps', 5), 'rden'], [('olat', os_)])
            if h % 2 == 1 or h == 4:
                pr = h // 2
                hs = [h - 1, h] if h % 2 == 1 else [h]
                for n_, hh in enumerate(hs):
                    MM(bank(7), wuv2[:, hh, :], olat[hh % 2], n_ == 0, n_ == len(hs) - 1, ['wuv2', ('olat', hh % 2)], [('ps', 7)])
                od = oTd[pr % 2]
                ACT(od, bank(7), AF.Copy, [('ps', 7)], [('oTd', pr % 2)])
                p1 = 128 if len(hs) == 2 else 64
                DMA(oT16[0:p1, 3 + pr, T0 + tg * 512:T0 + (tg + 1) * 512], od[0:p1, :], ('st_oTd', pr % 2),
                    [('oTd', pr % 2)], [('oT16d', pr, tg)])
            yield

    def interleave(g1, g2):
        a1, a2 = True, True
        while a1 or a2:
            if a1:
                try:
                    next(g1)
                except StopIteration:
                    a1 = False
            if a2:
                try:
                    next(g2)
                except StopIteration:
                    a2 = False

    def empty():
        return
        yield

    interleave(gen_isc(0), empty())
    for tg in range(1, 4):
        interleave(gen_isc(tg), gen_att(tg - 1))
    interleave(empty(), gen_att(3))
    A.release()


def sb_seq(ctx, l, b, sqT, skT, skTn, sv2):
    nc = ctx['nc']; P = ctx['P']; A = ctx['A']; ps = ctx['ps']
    bank = ctx['bank']; bankbf = ctx['bankbf']
    MM = ctx['MM']; TR = ctx['TR']; ACT = ctx['ACT']; DVE = ctx['DVE']; POOL = ctx['POOL']; DMA = ctx['DMA']
    cst = ctx['cst']; cbf = ctx['cbf']; ones_bf = ctx['ones_bf']; tri_bf = ctx['tri_bf']
    oT16 = ctx['oT16']; one_t = ctx['one_t']
    T0 = b * SEQ
    ms_bf = cbf[:, C_MS:C_MS + 128]

    def V3(ap, a):
        return ap.rearrange("p (a b) -> p a b", a=a)

    A.mark()
    wT = [V3(A.bf16(16 * 512), 16) for _ in range(2)]
    Cb = A.bf16(512)
    ebuf = [A.f32(512) for _ in range(2)]
    spm = [A.bf16(512) for _ in range(2)]
    oTs = [A.bf16(512) for _ in range(2)]
    hi = 0
    for tg in range(4):
        jmax = 4 * tg + 3
        for h in range(5):
            hp, bs = h // 2, 64 * (h % 2)
            row = 704 + 64 * h
            chunk, ob = row // 128, row % 128
            ws = hi % 2
            hi += 1
            wkey = ('wT', ws)
            POOL(lambda e: e.memset(Cb, 0.0), [], ['Cb'])
            for js in range(jmax, -1, -1):
                c0 = max(js - 4 * tg, 0) * 128
                w = 512 - c0
                tq0 = 512 * tg + c0
                s = js % 2
                pz = s
                pa = 2 + s
                ksl = slice(js * 128, (js + 1) * 128)
                MM(ps[:, pz * 512:pz * 512 + w], skT[bs:bs + 64, hp, ksl], sqT[bs:bs + 64, hp, tq0:tq0 + w], True, True,
                   ['skT', 'sqT'], [('ps', pz)])
                ACT(ebuf[s][:, 0:w], ps[:, pz * 512:pz * 512 + w], AF.Exp, [('ps', pz)], [('ebuf', s)], scale=0.125)
                ACT(spm[s][:, 0:w], ebuf[s][:, 0:w], AF.Ln, [('ebuf', s), 'kc'], [('spm', s)], bias=one_t)
                diag = js >= 4 * tg
                if diag:
                    POOL(lambda e, s=s: e.tensor_tensor(spm[s][:, 0:128], spm[s][:, 0:128], ms_bf, ALU.mult),
                         [('spm', s), 'cbf'], [('spm', s)])
                first = (js == jmax)
                MM(ps[:, pa * 512:pa * 512 + w], tri_bf, spm[s][:, 0:w], True, False, ['cbf', ('spm', s)], [('ps', pa)])
                if not first:
                    MM(ps[:, pa * 512:pa * 512 + w], ones_bf, Cb[:, c0:c0 + w], False, False, ['cbf', 'Cb'], [('ps', pa)])
                MM(ps[:, pa * 512:pa * 512 + w], skTn[bs:bs + 64, hp, ksl], sqT[bs:bs + 64, hp, tq0:tq0 + w], False, True,
                   ['skTn', 'sqT'], [('ps', pa)])
                ACT(wT[ws][:, js, c0:c0 + w], ps[:, pa * 512:pa * 512 + w], AF.Exp, [('ps', pa)], [wkey], scale=-1.0)
                if diag:
                    POOL(lambda e, ws=ws, js=js, c0=c0: e.tensor_tensor(wT[ws][:, js, c0:c0 + 128], wT[ws][:, js, c0:c0 + 128],
                                                                       ms_bf, ALU.mult), [wkey, 'cbf'], [wkey])
                if js > 0:
                    POOL(lambda e, s=s, c0=c0, w=w: e.tensor_tensor(Cb[:, c0:c0 + w], Cb[:, c0:c0 + w], spm[s][:, 0:w], ALU.add),
                         ['Cb', ('spm', s)], ['Cb'])
            pair_first = (h == 0) or (h % 2 == 1)
            pair_last = (h == 0) or (h % 2 == 0)
            po = 4 + (chunk % 2)
            for js in range(jmax + 1):
                c0 = max(js - 4 * tg, 0) * 128
                w = 512 - c0
                MM(ps[:, po * 512 + c0:po * 512 + 512], sv2[:, js, h, :], wT[ws][:, js, c0:c0 + w],
                   pair_first and js == 0, pair_last and js == jmax, ['sv2', wkey], [('ps', po)])
            if pair_last:
                od = oTs[chunk % 2]
                ACT(od, bank(po), AF.Copy, [('ps', po)], [('oTs', chunk % 2)])
                p0 = 64 if h == 0 else 0
                DMA(oT16[p0:128, chunk, T0 + tg * 512:T0 + (tg + 1) * 512], od[p0:128, :], ('st_oTs', chunk % 2),
                    [('oTs', chunk % 2)], [('oT16s', chunk, tg)])
    A.release()


MIX_PARTS = ('gla', 'dsa', 'sb')

PARAM_NAMES = ['ln_in_g', 'ln_in_b', 'w_in', 'gla_gate_w2', 'gla_gate_b', 'gla_norm_g', 'dsa_w_uv', 'w_out',
               'ln_mix_g', 'ln_mix_b', 'w_mem_q', 'w_mem_kv', 'w_mem_o', 'ln_mem_g', 'ln_mem_b',
               'w_up', 'b_up', 'w_down', 'b_down', 'ln_ffn_g', 'ln_ffn_b']


def make_in_maps(inputs, ncores=8):
    cst = make_consts()
    maps = []
    for c in range(ncores):
        m = {'cst': cst}
        m['x'] = np.ascontiguousarray(inputs['x'][c * NSEQ:(c + 1) * NSEQ]).reshape(TOK, D)
        m['mem'] = np.ascontiguousarray(inputs['mem'][c * NSEQ:(c + 1) * NSEQ]).reshape(NSEQ * MEMT, D)
        for k in PARAM_NAMES:
            a = np.ascontiguousarray(inputs[k], dtype=np.float32)
            if k in ('ln_in_g', 'ln_in_b'):
                a = a.reshape(1, D)
            m[k] = a
        maps.append(m)
    return maps


def kernel(**inputs):
    inputs = {k: np.asarray(v) for k, v in inputs.items()}
    nc, _ = build()
    maps = make_in_maps(inputs, 8)
    res = run_bass_kernel_spmd(nc, maps, core_ids=list(range(8)))
    outs = [r['out'].reshape(NSEQ, SEQ, D) for r in res.results]
    return np.concatenate(outs, axis=0).astype(np.float32)
```

```python
import numpy as np
from contextlib import ExitStack
import concourse.bass as bass
import concourse.mybir as mybir
from concourse.bass_utils import run_bass_kernel_spmd

F32 = mybir.dt.float32
BF16 = mybir.dt.bfloat16
AF = mybir.ActivationFunctionType
ALU = mybir.AluOpType

D = 1024
SEQ = 2048
NSEQ = 2
TOK = NSEQ * SEQ
DEPTH = 2
PIN = 3224
DFF = 4096
MEMT = 256
ALPHA = (2.0 * DEPTH) ** 0.25
LN_EPS = 1e-5
SLOPES = [2.0 ** (-8.0 * (i + 1) / 5) for i in range(5)]
NEG = -1.0e30

C_ID, C_TRI, C_ONE, C_MS, C_BT, C_BO, C_NEGI, C_CS, C_DIST = 0, 128, 256, 384, 512, 640, 768, 896, 1024
CST_N = 1024 + 2048


def make_consts():
    c = np.zeros((128, CST_N), np.float32)
    p = np.arange(128)[:, None]
    q = np.arange(128)[None, :]
    c[:, C_ID:C_ID + 128] = (p == q)
    c[:, C_TRI:C_TRI + 128] = (p >= q)
    c[:, C_ONE:C_ONE + 128] = 1.0
    c[:, C_MS:C_MS + 128] = (p < q)
    c[:, C_BT:C_BT + 128] = (p <= q) & ((p // 64) == (q // 64))
    c[:, C_BO:C_BO + 128] = ((p // 64) == (q // 64))
    c[:, C_NEGI:C_NEGI + 128] = np.where(q <= p, 0.0, NEG)
    c[:, C_CS + 0] = (np.arange(128) < 64)
    c[:, C_CS + 1] = (np.arange(128) >= 64)
    m = np.arange(2048)[None, :]
    c[:, C_DIST:C_DIST + 2048] = m - p
    return c


class Prog:
    ENGS = ['pe', 'act', 'dve', 'pool', 'sp']

    def __init__(self, nc):
        self.nc = nc
        self.ops = []
        self.res = {}
        self.dma_cnt = {}
        self.last_dma = {}
        self.last_op = {}

    def add(self, eng, fn, R=(), W=(), dma=None, extra=()):
        i = len(self.ops)
        deps = {}
        for k in R:
            st = self.res.get(k)
            if st is not None and st[0] is not None:
                deps[st[0]] = deps.get(st[0], 0) | 1
        for k in W:
            st = self.res.get(k)
            if st is not None:
                if st[0] is not None:
                    deps[st[0]] = deps.get(st[0], 0) | 2
                for r in st[1]:
                    deps[r] = deps.get(r, 0) | 2
        for e in extra:
            deps[e] = deps.get(e, 0) | 1
        for k in R:
            st = self.res.setdefault(k, [None, []])
            st[1].append(i)
        for k in W:
            st = self.res.setdefault(k, [None, []])
            st[0] = i
            st[1] = []
        deps.pop(i, None)
        op = dict(id=i, eng=eng, fn=fn, deps=deps, dma=dma, sig=False, val=0)
        if dma is not None:
            c = self.dma_cnt.get(dma, 0) + 16
            self.dma_cnt[dma] = c
            op['val'] = c
            self.last_dma[dma] = i
        else:
            self.last_op[eng] = i
        self.ops.append(op)
        return i

    def barrier(self):
        ex = list(self.last_dma.values()) + list(self.last_op.values())
        x = self.add('sp', lambda e: e.nop(), extra=ex)
        for e in ['pe', 'act', 'dve', 'pool']:
            self.add(e, lambda en: en.nop(), extra=[x])
        self.res = {}

    def emit(self):
        nc = self.nc
        ops = self.ops
        for op in ops:
            need = []
            for pid, kind in op['deps'].items():
                p = ops[pid]
                if p['dma'] is None:
                    if op['dma'] is None and p['eng'] == op['eng']:
                        if op['eng'] in ('pe', 'sp'):
                            continue
                    p['sig'] = True
                need.append(pid)
            op['need'] = need
        cnt = {e: 0 for e in self.ENGS}
        for op in ops:
            if op['dma'] is None and op['sig']:
                cnt[op['eng']] += 1
                op['val'] = cnt[op['eng']]
        with ExitStack() as es:
            esem = {e: es.enter_context(nc.semaphore("s_" + e)) for e in self.ENGS}
            dsem = {k: es.enter_context(nc.semaphore("d_%d" % i)) for i, k in enumerate(self.dma_cnt)}
            block = es.enter_context(nc.Block())

            def run(e, eobj):
                waited = {}
                for op in ops:
                    if op['eng'] != e:
                        continue
                    waits = {}
                    for pid in op['need']:
                        p = ops[pid]
                        s = dsem[p['dma']] if p['dma'] is not None else esem[p['eng']]
                        v = p['val']
                        if waited.get(s.name, 0) >= v:
                            continue
                        if waits.get(s.name, (None, 0))[1] < v:
                            waits[s.name] = (s, v)
                    wl = list(waits.values())
                    for s, v in wl[1:]:
                        eobj.wait_ge(s, v)
                        waited[s.name] = v
                    ins = op['fn'](eobj)
                    if wl:
                        ins._wait_ge(wl[0][0], wl[0][1])
                        waited[wl[0][0].name] = wl[0][1]
                    if op['dma'] is not None:
                        ins.then_inc(dsem[op['dma']], 16)
                    elif op['sig']:
                        ins.then_inc(esem[e], 1)
                if e == 'sp':
                    for k, c in self.dma_cnt.items():
                        if waited.get(dsem[k].name, 0) < c:
                            eobj.wait_ge(dsem[k], c)

            block.tensor(lambda t: run('pe', t))
            block.scalar(lambda t: run('act', t))
            block.vector(lambda t: run('dve', t))
            block.gpsimd(lambda t: run('pool', t))
            block.sync(lambda t: run('sp', t))


class Arena:
    def __init__(self, nc, nwords):
        self.t = nc.alloc_sbuf_tensor("arena", [128, nwords], F32)
        self.n = nwords
        self.top = 0
        self.marks = []
        self.peak = 0

    def _alloc(self, nbytes):
        w = (nbytes + 3) // 4
        w = (w + 7) // 8 * 8
        off = self.top
        self.top += w
        self.peak = max(self.peak, self.top)
        assert self.top <= self.n, ("SBUF arena overflow", self.top * 4)
        return off

    def f32(self, cols):
        off = self._alloc(cols * 4)
        return self.t[:, off:off + cols]

    def bf16(self, cols):
        off = self._alloc(cols * 2)
        return self.t[:, off:off + (cols + 1) // 2].bitcast(BF16)

    def mark(self):
        self.marks.append(self.top)

    def release(self):
        self.top = self.marks.pop()


def build(dbg=None, phases=None):
    nc = bass.Bass("TRN2", target_bir_lowering=False)
    try:
        nc.allow_low_precision("bf16 matmul operands with fp32 accumulation")
    except Exception:
        pass
    try:
        nc.allow_non_contiguous_dma("small strided parameter loads")
    except Exception:
        pass
    P = Prog(nc)
    A = Arena(nc, 52000)
    ps = nc.alloc_psum_tensor("ps", [128, 4096], F32)

    def dt(name, shape, dtype=F32, kind="ExternalInput"):
        return nc.dram_tensor(name, shape, dtype, kind=kind).ap()

    x_d = dt("x", [TOK, D])
    mem_d = dt("mem", [NSEQ * MEMT, D])
    cst_d = dt("cst", [128, CST_N])
    ln_in_g = dt("ln_in_g", [1, D]); ln_in_b = dt("ln_in_b", [1, D])
    w_in = dt("w_in", [DEPTH, D, PIN])
    gate_w2 = dt("gla_gate_w2", [DEPTH, 16, 192]); gate_b = dt("gla_gate_b", [DEPTH, 192])
    norm_g = dt("gla_norm_g", [DEPTH, 96])
    w_uv = dt("dsa_w_uv", [DEPTH, 5, 128, 64])
    w_out = dt("w_out", [DEPTH, D, D])
    ln_mix_g = dt("ln_mix_g", [DEPTH, D]); ln_mix_b = dt("ln_mix_b", [DEPTH, D])
    w_mem_q = dt("w_mem_q", [DEPTH, D, D]); w_mem_kv = dt("w_mem_kv", [DEPTH, D, 2 * D]); w_mem_o = dt("w_mem_o", [DEPTH, D, D])
    ln_mem_g = dt("ln_mem_g", [DEPTH, D]); ln_mem_b = dt("ln_mem_b", [DEPTH, D])
    w_up = dt("w_up", [DEPTH, D, DFF]); b_up = dt("b_up", [DEPTH, DFF])
    w_down = dt("w_down", [DEPTH, DFF, D]); b_down = dt("b_down", [DEPTH, D])
    ln_ffn_g = dt("ln_ffn_g", [DEPTH, D]); ln_ffn_b = dt("ln_ffn_b", [DEPTH, D])
    out_d = dt("out", [TOK, D], kind="ExternalOutput")
    skind = "ExternalOutput" if dbg else "Internal"
    h32 = dt("h32", [TOK, D], kind=skind)
    hT16 = dt("hT16", [128, 8, TOK], BF16, kind=skind)
    oT16 = dt("oT16", [128, 8, TOK], BF16, kind=skind)
    wup16 = dt("wup16", [128, 8, DFF], BF16, kind="Internal")

    def bank(b, n=512):
        return ps[:, b * 512:b * 512 + n]

    def bankbf(b):
        return ps[:, b * 512:(b + 1) * 512].bitcast(BF16)

    def MM(out, lhsT, rhs, start, stop, R, W):
        P.add('pe', lambda e: e.matmul(out, lhsT, rhs, start=start, stop=stop), R, W)

    def TR(out, in_, ident, R, W):
        P.add('pe', lambda e: e.transpose(out, in_, ident), R, W)

    def ACT(out, in_, func, R, W, bias=None, scale=1.0, accum=None):
        kw = {}
        if bias is not None:
            kw['bias'] = bias
        if accum is not None:
            kw['accum_out'] = accum
        P.add('act', lambda e: e.activation(out, in_, func, scale=scale, **kw), R, W)

    def DVE(fn, R, W):
        P.add('dve', fn, R, W)

    def POOL(fn, R, W):
        P.add('pool', fn, R, W)

    def DMA(out, in_, key, R, W, q='sp', slow=False):
        if slow:
            P.add(q, lambda e: e.dma_start(out=out, in_=in_, allow_slow_non_contiguous=True), R, W, dma=key)
        else:
            P.add(q, lambda e: e.dma_start(out=out, in_=in_), R, W, dma=key)

    def bcast_row(ap_row, n):
        return ap_row.to_broadcast([128, n])

    cst = A.f32(1024)
    DMA(cst, cst_d[:, 0:1024], 'cst', ['cst_d'], ['cst'])
    cbf = A.bf16(1024)
    DVE(lambda e: e.tensor_copy(cbf, cst[:, 0:1024]), ['cst'], ['cbf'])
    ident_bf = cbf[:, C_ID:C_ID + 128]
    ones_bf = cbf[:, C_ONE:C_ONE + 128]
    tri_bf = cbf[:, C_TRI:C_TRI + 128]
    kc = A.f32(8)
    POOL(lambda e: e.memset(kc[:, 0:1], LN_EPS), [], ['kc'])
    POOL(lambda e: e.memset(kc[:, 1:2], 1.0), [], ['kc'])
    eps_t = kc[:, 0:1]
    one_t = kc[:, 1:2]
    A.mark()

    class LNState:
        pass

    def ln_setup(g_row, b_row, tag, nb=2):
        st = LNState()
        st.nb = nb
        st.rc = 0
        st.g = A.f32(D); st.b = A.f32(D)
        DMA(st.g, bcast_row(g_row, D), ('lnp', 'g'), [], ['ln_g'])
        DMA(st.b, bcast_row(b_row, D), ('lnp', 'b'), [], ['ln_b'])
        st.ybf = [A.bf16(D) for _ in range(nb)]
        st.stats = [A.f32(16) for _ in range(nb)]
        st.hTb = [A.bf16(8 * 512) for _ in range(2)]
        st.n = 0
        return st

    def ln_tile(st, z, zkey, tok0, final=False):
        i = st.n; st.n += 1
        s = i % st.nb
        y = z; ybf = st.ybf[s]; stt = st.stats[s]
        ky, kyb, kst = zkey, ('ln_ybf', s), ('ln_st', s)
        DVE(lambda e: e.bn_stats(stt[:, 0:6], z[:, 0:512]), [zkey], [kst])
        DVE(lambda e: e.bn_stats(stt[:, 6:12], z[:, 512:1024]), [zkey], [kst])
        DVE(lambda e: e.bn_aggr(stt[:, 12:14], stt[:, 0:12].rearrange("p (a b) -> p a b", a=2)), [kst], [kst])
        ACT(stt[:, 15:16], stt[:, 13:14], AF.Ln, [kst, 'kc'], [kst], bias=eps_t)
        ACT(stt[:, 14:15], stt[:, 15:16], AF.Exp, [kst], [kst], scale=-0.5)
        DVE(lambda e: e.tensor_scalar(y, z, stt[:, 12:13], stt[:, 14:15], ALU.subtract, ALU.mult), [kst], [ky])
        POOL(lambda e: e.tensor_tensor(y, y, st.g, ALU.mult), [ky, 'ln_g'], [ky])
        POOL(lambda e: e.tensor_tensor(y, y, st.b, ALU.add), [ky, 'ln_b'], [ky])
        if final:
            DMA(out_d[tok0:tok0 + 128, :], y, ('st_y', zkey), [ky], [('out', tok0)], q='pool')
            return
        DMA(h32[tok0:tok0 + 128, :], y, ('st_y', zkey), [ky], [('h32', tok0)], q='pool')
        ACT(ybf, y, AF.Copy, [ky], [kyb])
        q = (tok0 // 128) % 4
        blk = tok0 // 512
        hs = blk % 2
        hTb = st.hTb[hs].rearrange("p (k t) -> p k t", k=8)
        pb = 6 + (i % 2)
        pv = bankbf(pb)
        for k in range(8):
            TR(pv[:, k * 128:(k + 1) * 128], ybf[:, k * 128:(k + 1) * 128], ident_bf, [kyb, 'cbf'], [('ps', pb)])
        DVE(lambda e: e.tensor_copy(hTb[:, :, q * 128:(q + 1) * 128], pv.rearrange("p (k t) -> p k t", k=8)),
            [('ps', pb)], [('hTb', hs)])
        if q == 3:
            DMA(hT16[:, :, blk * 512:(blk + 1) * 512], hTb, ('st_hT', hs), [('hTb', hs)], [('hT16', blk)])

    def load_w(dst3, src_fn, nk, ncols, stage, tag, wkey, engs=('pool', 'act')):
        for k in range(nk):
            s = k % len(stage)
            sk = ('wst', tag, s)
            DMA(stage[s][:, 0:ncols], src_fn(k), ('wst', tag, s), [], [sk])
            eng = engs[k % len(engs)]
            if eng == 'act':
                ACT(dst3[:, k, :], stage[s][:, 0:ncols], AF.Copy, [sk], [wkey])
            elif eng == 'pool':
                POOL(lambda e, k=k, s=s: e.tensor_copy(dst3[:, k, :], stage[s][:, 0:ncols]), [sk], [wkey])
            else:
                DVE(lambda e, k=k, s=s: e.tensor_copy(dst3[:, k, :], stage[s][:, 0:ncols]), [sk], [wkey])

    def phase_ln_in():
        A.mark()
        st = ln_setup(ln_in_g, ln_in_b, 'in')
        xt = [A.f32(D) for _ in range(2)]
        for i in range(TOK // 128):
            s = i % 2
            DMA(xt[s], x_d[i * 128:(i + 1) * 128, :], ('ld_x', s), [], [('xt', s)])
            ln_tile(st, xt[s], ('xt', s), i * 128)
        A.release()
        P.barrier()

    def res_ln_tiles(st, lhs_fn, lhs_keys, wbf, wkey, nk, tok0_blk, hbuf, zbuf, bias_tile=None, final=False, pairs=((4, 5),)):
        for tt in range(4):
            tok0 = tok0_blk + tt * 128
            s = st.rc % len(hbuf)
            st.rc += 1
            DMA(hbuf[s], h32[tok0:tok0 + 128, :], ('ld_h', s), [('h32', tok0)], [('hb', s)])
            for nh in range(2):
                pb = pairs[tt % len(pairs)][nh]
                for k in range(nk):
                    MM(bank(pb), lhs_fn(k, tt), wbf[:, k, nh * 512:(nh + 1) * 512], k == 0, k == nk - 1,
                       lhs_keys + [wkey], [('ps', pb)])
                DVE(lambda e, s=s, nh=nh, pb=pb: e.scalar_tensor_tensor(
                    zbuf[s][:, nh * 512:(nh + 1) * 512], hbuf[s][:, nh * 512:(nh + 1) * 512], ALPHA, bank(pb),
                    ALU.mult, ALU.add), [('hb', s), ('ps', pb)], [('zb', s)])
            if bias_tile is not None:
                POOL(lambda e, s=s: e.tensor_tensor(zbuf[s], zbuf[s], bias_tile, ALU.add), [('zb', s), 'bias_t'], [('zb', s)])
            ln_tile(st, zbuf[s], ('zb', s), tok0, final=final)

    def phase_wout(l):
        A.mark()
        st = ln_setup(ln_mix_g[l:l + 1, :], ln_mix_b[l:l + 1, :], 'mix', nb=4)
        stage = [A.f32(1024) for _ in range(4)]
        wbf = A.bf16(8 * 1024).rearrange("p (k n) -> p k n", k=8)
        load_w(wbf, lambda k: w_out[l, k * 128:(k + 1) * 128, :], 8, 1024, stage, 'wo', 'wbf')
        src = [A.bf16(8 * 512).rearrange("p (k t) -> p k t", k=8) for _ in range(2)]
        hbuf = [A.f32(D) for _ in range(4)]
        zbuf = [A.f32(D) for _ in range(4)]
        for blk in range(TOK // 512):
            s = blk % 2
            DMA(src[s], oT16[:, :, blk * 512:(blk + 1) * 512], ('ld_src', s), [('oT16', blk)], [('src', s)])
            res_ln_tiles(st, lambda k, tt, s=s: src[s][:, k, tt * 128:(tt + 1) * 128], [('src', s)], wbf, 'wbf', 8,
                         blk * 512, hbuf, zbuf)
        A.release()
        P.barrier()

    def phase_mlp(l, final):
        A.mark()
        big = [A.f32(4096) for _ in range(2)]
        bfb = [A.bf16(4096) for _ in range(2)]
        for k in range(8):
            s = k % 2
            DMA(big[s], w_up[l, k * 128:(k + 1) * 128, :], ('ld_big', s), [], [('big', s)])
            for c in range(4):
                o_, i_ = bfb[s][:, c * 1024:(c + 1) * 1024], big[s][:, c * 1024:(c + 1) * 1024]
                if c % 2 == 0:
                    DVE(lambda e, o_=o_, i_=i_: e.tensor_copy(o_, i_), [('big', s)], [('bfb', s)])
                elif c == 1:
                    ACT(o_, i_, AF.Copy, [('big', s)], [('bfb', s)])
                else:
                    POOL(lambda e, o_=o_, i_=i_: e.tensor_copy(o_, i_), [('big', s)], [('bfb', s)])
            DMA(wup16[:, k, :], bfb[s], ('st_bfb', s), [('bfb', s)], [('wup16', k)])
        A.release()
        P.barrier()
        A.mark()
        st = ln_setup(ln_ffn_g[l:l + 1, :], ln_ffn_b[l:l + 1, :], 'ffn')
        bdt = A.f32(D)
        DMA(bdt, bcast_row(b_down[l:l + 1, :], D), ('lnp', 'bd'), [], ['bias_t'])
        bupT = A.f32(32)
        DMA(bupT, b_up[l].rearrange("(j p) -> p j", p=128), ('lnp', 'bu'), [], ['bupT'], slow=True)
        stage = [A.f32(1024) for _ in range(2)]
        wdn = A.bf16(32 * 1024).rearrange("p (k n) -> p k n", k=32)
        load_w(wdn, lambda k: w_down[l, k * 128:(k + 1) * 128, :], 32, 1024, stage, 'wd', 'wdn', engs=('pool', 'act', 'dve'))
        NW = 3
        wup = [A.bf16(8 * 512).rearrange("p (k n) -> p k n", k=8) for _ in range(NW)]
        src = [A.bf16(8 * 512).rearrange("p (k t) -> p k t", k=8) for _ in range(2)]
        uT = A.bf16(32 * 512).rearrange("p (j t) -> p j t", j=32)
        rbuf = [A.f32(512) for _ in range(2)]
        hbuf = [A.f32(D) for _ in range(2)]
        zbuf = [A.f32(D) for _ in range(2)]
        NB = TOK // 512
        groups = [(blk, g) for blk in range(NB) for g in range(8)]
        issued = [0]
        src_issued = [0]

        def ensure(n):
            while issued[0] < min(n + NW, len(groups)):
                i = issued[0]
                blk_, g_ = groups[i]
                ws_ = i % NW
                DMA(wup[ws_], wup16[:, :, g_ * 512:(g_ + 1) * 512], ('ld_wup', ws_), [], [('wup', ws_)])
                issued[0] += 1

        def ensure_src(blk_):
            while src_issued[0] < min(blk_ + 2, NB):
                b_ = src_issued[0]
                DMA(src[b_ % 2], hT16[:, :, b_ * 512:(b_ + 1) * 512], ('ld_src', b_ % 2), [], [('src', b_ % 2)])
                src_issued[0] += 1

        gi = 0
        for blk in range(NB):
            s = blk % 2
            ensure_src(blk)
            for g in range(8):
                ensure(gi)
                ws = gi % NW
                gi += 1
                for jj in range(4):
                    j = g * 4 + jj
                    pb = j % 4
                    for k in range(8):
                        MM(bank(pb), wup[ws][:, k, jj * 128:(jj + 1) * 128], src[s][:, k, :], k == 0, k == 7,
                           [('wup', ws), ('src', s)], [('ps', pb)])
                    rs = j % 2
                    ACT(rbuf[rs], bank(pb), AF.Relu, [('ps', pb), 'bupT'], [('rb', rs)], bias=bupT[:, j:j + 1])
                    POOL(lambda e, rs=rs, j=j: e.tensor_tensor(uT[:, j, :], rbuf[rs], rbuf[rs], ALU.mult),
                         [('rb', rs)], ['uT'])
            res_ln_tiles(st, lambda k, tt: uT[:, k, tt * 128:(tt + 1) * 128], ['uT'], wdn, 'wdn', 32,
                         blk * 512, hbuf, zbuf, bias_tile=bdt, final=final, pairs=((4, 5), (0, 1), (2, 3)))
        A.release()
        P.barrier()

    def phase_mem(l):
        A.mark()
        st = ln_setup(ln_mem_g[l:l + 1, :], ln_mem_b[l:l + 1, :], 'mem', nb=3)
        stage = [A.f32(1024) for _ in range(2)]
        wkv = A.bf16(8 * 2048).rearrange("p (k n) -> p k n", k=8)
        load_w(wkv[:, :, 0:1024], lambda k: w_mem_kv[l, k * 128:(k + 1) * 128, 0:1024], 8, 1024, stage, 'wkv', 'wkv')
        load_w(wkv[:, :, 1024:2048], lambda k: w_mem_kv[l, k * 128:(k + 1) * 128, 1024:2048], 8, 1024, stage, 'wkv', 'wkv')
        wq = A.bf16(8 * 1024).rearrange("p (k n) -> p k n", k=8)
        load_w(wq, lambda k: w_mem_q[l, k * 128:(k + 1) * 128, :], 8, 1024, stage, 'wkv', 'wq')
        wo = A.bf16(8 * 1024).rearrange("p (k n) -> p k n", k=8)
        load_w(wo, lambda k: w_mem_o[l, k * 128:(k + 1) * 128, :], 8, 1024, stage, 'wkv', 'wo')
        hbuf = [A.f32(D) for _ in range(3)]
        memf = hbuf
        membf = [A.bf16(D) for _ in range(2)]
        memT = A.bf16(8 * 256).rearrange("p (k m) -> p k m", k=8)
        kT = A.bf16(8 * 256).rearrange("p (c m) -> p c m", c=8)
        vv = A.bf16(2 * 1024).rearrange("p (m n) -> p m n", m=2)
        src = [A.bf16(8 * 512).rearrange("p (k t) -> p k t", k=8) for _ in range(2)]
        qT = A.bf16(8 * 512).rearrange("p (c t) -> p c t", c=8)
        E = [A.bf16(512) for _ in range(2)]
        rden = A.f32(512)
        omT = A.bf16(8 * 512).rearrange("p (c t) -> p c t", c=8)
        zbuf = [A.f32(D) for _ in range(3)]
        for b in range(NSEQ):
            for mt in range(2):
                DMA(memf[mt], mem_d[b * MEMT + mt * 128:b * MEMT + (mt + 1) * 128, :], ('ld_mem', mt), [], [('hb', mt)])
                ACT(membf[mt], memf[mt], AF.Copy, [('hb', mt)], [('membf', mt)])
                pv = bankbf(mt)
                for k in range(8):
                    TR(pv[:, k * 128:(k + 1) * 128], membf[mt][:, k * 128:(k + 1) * 128], ident_bf,
                       [('membf', mt), 'cbf'], [('ps', mt)])
                DVE(lambda e, mt=mt, pv=pv: e.tensor_copy(memT[:, :, mt * 128:(mt + 1) * 128],
                                                       pv.rearrange("p (k t) -> p k t", k=8)),
                    [('ps', mt)], ['memT'])
            for c in range(8):
                pb = c % 2
                for k in range(8):
                    MM(bank(pb, 256), wkv[:, k, c * 128:(c + 1) * 128], memT[:, k, :], k == 0, k == 7,
                       ['wkv', 'memT'], [('ps', pb)])
                DVE(lambda e, c=c, pb=pb: e.tensor_copy(kT[:, c, :], bank(pb, 256)), [('ps', pb)], ['kT'])
            for mt in range(2):
                for nh in range(2):
                    pb = 2 + nh
                    for k in range(8):
                        MM(bank(pb), memT[:, k, mt * 128:(mt + 1) * 128], wkv[:, k, 1024 + nh * 512:1024 + (nh + 1) * 512],
                           k == 0, k == 7, ['wkv', 'memT'], [('ps', pb)])
                    ACT(vv[:, mt, nh * 512:(nh + 1) * 512], bank(pb), AF.Copy, [('ps', pb)], ['vv'])
            for bb in range(4):
                blk = b * 4 + bb
                s = blk % 2
                DMA(src[s], hT16[:, :, blk * 512:(blk + 1) * 512], ('ld_src', s), [('hT16', blk)], [('src', s)])
                for c in range(8):
                    pb = c % 2
                    for k in range(8):
                        MM(bank(pb), wq[:, k, c * 128:(c + 1) * 128], src[s][:, k, :], k == 0, k == 7,
                           ['wq', ('src', s)], [('ps', pb)])
                    if c % 2 == 0:
                        ACT(qT[:, c, :], bank(pb), AF.Copy, [('ps', pb)], ['qT'])
                    else:
                        DVE(lambda e, c=c, pb=pb: e.tensor_copy(qT[:, c, :], bank(pb)), [('ps', pb)], ['qT'])
                for h in range(4):
                    for mt in range(2):
                        pb = mt
                        for dc in range(2):
                            MM(bank(pb), kT[:, h * 2 + dc, mt * 128:(mt + 1) * 128], qT[:, h * 2 + dc, :], dc == 0, dc == 1,
                               ['kT', 'qT'], [('ps', pb)])
                        ACT(E[mt], bank(pb), AF.Exp, [('ps', pb)], [('E', mt)], scale=1.0 / 16.0)
                    for mt in range(2):
                        MM(bank(2), ones_bf, E[mt], mt == 0, mt == 1, [('E', mt), 'cbf'], [('ps', 2)])
                    DVE(lambda e: e.reciprocal(rden, bank(2)), [('ps', 2)], ['rden'])
                    for dc in range(2):
                        pb = 3
                        c = h * 2 + dc
                        for mt in range(2):
                            MM(bank(pb), vv[:, mt, c * 128:(c + 1) * 128], E[mt], mt == 0, mt == 1,
                               ['vv', ('E', mt)], [('ps', pb)])
                        DVE(lambda e, c=c, pb=pb: e.tensor_tensor(omT[:, c, :], bank(pb), rden, ALU.mult),
                            [('ps', pb), 'rden'], ['omT'])
                res_ln_tiles(st, lambda k, tt: omT[:, k, tt * 128:(tt + 1) * 128], ['omT'], wo, 'wo', 8,
                             blk * 512, hbuf, zbuf)
        A.release()
        P.barrier()

    def phase_mixer_zero():
        A.mark()
        zt = A.bf16(8 * 512)
        POOL(lambda e: e.memset(zt, 0.0), [], ['zt'])
        for blk in range(TOK // 512):
            DMA(oT16[:, :, blk * 512:(blk + 1) * 512], zt.rearrange("p (k t) -> p k t", k=8), 'st_z', ['zt'], [('oT16', blk)])
        A.release()
        P.barrier()

    ctx = dict(nc=nc, P=P, A=A, ps=ps, bank=bank, bankbf=bankbf, MM=MM, TR=TR, ACT=ACT, DVE=DVE, POOL=POOL, DMA=DMA,
               cst=cst, cbf=cbf, cst_d=cst_d, ident_bf=ident_bf, ones_bf=ones_bf, tri_bf=tri_bf, load_w=load_w,
               w_in=w_in, gate_w2=gate_w2, gate_b=gate_b, norm_g=norm_g, w_uv=w_uv, hT16=hT16, oT16=oT16,
               bcast_row=bcast_row, one_t=one_t, eps_t=eps_t, x_d=x_d)

    phases = phases or ['ln_in', 'mix', 'wout', 'mem', 'mlp']
    if 'ln_in' in phases:
        phase_ln_in()
    for l in range(DEPTH):
        if 'mix' in phases:
            phase_mixer(ctx, l, MIX_PARTS)
        elif 'mixzero' in phases:
            phase_mixer_zero()
        if 'wout' in phases:
            phase_wout(l)
        if 'mem' in phases:
            phase_mem(l)
        if 'mlp' in phases:
            phase_mlp(l, final=(l == DEPTH - 1) or bool(dbg and dbg.get('layers', DEPTH) == l + 1))
        if dbg and dbg.get('layers', DEPTH) == l + 1:
            break
    P.emit()
    return nc, A


def phase_mixer(ctx, l, parts=('gla', 'dsa', 'sb')):
    for b in range(NSEQ):
        mixer_seq(ctx, l, b, parts)
        ctx['P'].barrier()


def mixer_seq(ctx, l, b, parts):
    nc = ctx['nc']; P = ctx['P']; A = ctx['A']; ps = ctx['ps']
    bank = ctx['bank']; bankbf = ctx['bankbf']
    MM = ctx['MM']; TR = ctx['TR']; ACT = ctx['ACT']; DVE = ctx['DVE']; POOL = ctx['POOL']; DMA = ctx['DMA']
    cst = ctx['cst']; cbf = ctx['cbf']; ident_bf = ctx['ident_bf']; ones_bf = ctx['ones_bf']; tri_bf = ctx['tri_bf']
    load_w = ctx['load_w']; w_in = ctx['w_in']; hT16 = ctx['hT16']; oT16 = ctx['oT16']
    bcast_row = ctx['bcast_row']; one_t = ctx['one_t']; cst_d = ctx['cst_d']
    T0 = b * SEQ

    def V3(ap, a):
        return ap.rearrange("p (a b) -> p a b", a=a)

    A.mark()
    dqT = V3(A.bf16(3 * 2048), 3); dkT2 = A.bf16(2048)
    vlat = V3(A.bf16(16 * 128), 16)
    iqT = V3(A.bf16(4 * 2048), 4); ikT2 = A.bf16(2048)
    iw = V3(A.f32(16 * 8), 16)
    sqT = V3(A.bf16(3 * 2048), 3); skT = V3(A.bf16(3 * 2048), 3); skTn = V3(A.bf16(3 * 2048), 3)
    sv2f = A.bf16(16 * 5 * 128)
    sv2 = sv2f.rearrange("p (a h c) -> p a h c", a=16, h=5)
    POOL(lambda e: e.memset(sv2f, 0.0), [], ['sv2'])
    neg8 = A.bf16(512)
    POOL(lambda e: e.memset(neg8, -0.125), [], ['neg8'])

    A.mark()
    hT = V3(A.bf16(8 * 2048), 8)
    for q in range(4):
        DMA(hT[:, :, q * 512:(q + 1) * 512], hT16[:, :, T0 + q * 512:T0 + (q + 1) * 512], ('ld_hT', q), [], [('hT', q)])
    stage = [A.f32(1168) for _ in range(2)]
    wbf = V3(A.bf16(8 * 1168), 8)
    pbc = [0]
    evc = [0]

    def nb():
        pbc[0] = (pbc[0] + 1) % 4
        return pbc[0]

    def evac(out, in_, R, W, scale=None):
        evc[0] += 1
        if scale is not None:
            P.add('act', lambda e: e.mul(out, in_, scale), R, W)
        elif evc[0] % 2 == 0:
            ACT(out, in_, AF.Copy, R, W)
        else:
            DVE(lambda e: e.tensor_copy(out, in_), R, W)

    def proj_fm(c0, M, ev, wsrc=None, wkey='wbf'):
        wsrc = wbf if wsrc is None else wsrc
        for tc in range(4):
            pb = nb()
            for k in range(8):
                MM(ps[0:M, pb * 512:(pb + 1) * 512], wsrc[:, k, c0:c0 + M], hT[:, k, tc * 512:(tc + 1) * 512],
                   k == 0, k == 7, [wkey, ('hT', tc)], [('ps', pb)])
            ev(pb, tc)

    def proj_tm(c0, N, ev):
        for tt in range(16):
            pb = nb()
            for k in range(8):
                MM(ps[:, pb * 512:pb * 512 + N], hT[:, k, tt * 128:(tt + 1) * 128], wbf[:, k, c0:c0 + N],
                   k == 0, k == 7, ['wbf', ('hT', tt // 4)], [('ps', pb)])
            ev(pb, tt)

    if 'sb' in parts or 'sbproj' in parts:
        load_w(wbf[:, :, 0:960], lambda k: w_in[l, k * 128:(k + 1) * 128, 2264:3224], 8, 960, stage, 'win', 'wbf')
        import os
        lvl = int(os.environ.get("SBLVL", "9"))
        for p in range(3):
            M = 128 if p < 2 else 64
            if lvl < 2 or (lvl < 3 and p == 2):
                continue
            proj_fm(p * 128, M, lambda pb, tc, p=p, M=M: evac(sqT[0:M, p, tc * 512:(tc + 1) * 512],
                                                          ps[0:M, pb * 512:(pb + 1) * 512], [('ps', pb)], ['sqT']))

            def ev_k(pb, tc, p=p, M=M):
                evac(skT[0:M, p, tc * 512:(tc + 1) * 512], ps[0:M, pb * 512:(pb + 1) * 512], [('ps', pb)], ['skT'])
                POOL(lambda e, p=p, M=M, tc=tc: e.tensor_tensor(skTn[0:M, p, tc * 512:(tc + 1) * 512],
                                                                skT[0:M, p, tc * 512:(tc + 1) * 512], neg8[0:M, :], ALU.mult),
                     ['skT', 'neg8'], ['skTn'])
            if lvl >= 4:
                proj_fm(320 + p * 128, M, ev_k)

        def ev_v(pb, tt):
            pv = ps[:, pb * 512:pb * 512 + 320].rearrange("p (h c) -> p h c", h=5)
            evac(sv2[:, tt, 1::2, 0:64], pv[:, 1::2, :], [('ps', pb)], ['sv2'])
            evac(sv2[:, tt, 0::2, 64:128], pv[:, 0::2, :], [('ps', pb)], ['sv2'])
        if lvl >= 5:
            proj_tm(640, 320, ev_v)

    if 'dsa' in parts:
        load_w(wbf[:, :, 0:1096], lambda k: w_in[l, k * 128:(k + 1) * 128, 1168:2264], 8, 1096, stage, 'win', 'wbf')
        A.mark()
        wdk = V3(A.bf16(8 * 128), 8); wik = V3(A.bf16(8 * 128), 8)
        for hh in range(2):
            POOL(lambda e, hh=hh: e.tensor_copy(wdk[:, :, hh * 64:(hh + 1) * 64], wbf[:, :, 320:384]), ['wbf'], ['wdk'])
            POOL(lambda e, hh=hh: e.tensor_copy(wik[:, :, hh * 64:(hh + 1) * 64], wbf[:, :, 1024:1088]), ['wbf'], ['wik'])
        for p in range(3):
            M = 128 if p < 2 else 64
            proj_fm(p * 128, M, lambda pb, tc, p=p, M=M: evac(dqT[0:M, p, tc * 512:(tc + 1) * 512],
                                                          ps[0:M, pb * 512:(pb + 1) * 512], [('ps', pb)], ['dqT']))
        proj_fm(0, 128, lambda pb, tc: evac(dkT2[:, tc * 512:(tc + 1) * 512], bank(pb), [('ps', pb)], ['dkT2']),
                wsrc=wdk, wkey='wdk')
        proj_fm(0, 128, lambda pb, tc: evac(ikT2[:, tc * 512:(tc + 1) * 512], bank(pb), [('ps', pb)], ['ikT2']),
                wsrc=wik, wkey='wik')
        for p in range(4):
            proj_fm(512 + p * 128, 128, lambda pb, tc, p=p: evac(iqT[:, p, tc * 512:(tc + 1) * 512], bank(pb),
                                                              [('ps', pb)], ['iqT']))
        proj_tm(384, 128, lambda pb, tt: evac(vlat[:, tt, :], ps[:, pb * 512:pb * 512 + 128], [('ps', pb)], ['vlat']))
        proj_tm(1088, 8, lambda pb, tt: evac(iw[:, tt, :], ps[:, pb * 512:pb * 512 + 8], [('ps', pb)], ['iw']))
        A.release()
        P.barrier()

    if 'gla' in parts:
        gla_seq(ctx, l, b, hT, wbf, stage, nb, evac, proj_fm)
    else:
        zero_o(ctx, T0, 0, 3, 0, 128)
    A.release()
    P.barrier()

    if 'dsa' in parts:
        dsa_seq(ctx, l, b, dqT, dkT2, vlat, iqT, ikT2, iw)
    else:
        zero_o(ctx, T0, 3, 5, 0, 128)
        zero_o(ctx, T0, 5, 6, 0, 64)
    P.barrier()
    if 'sb' in parts:
        sb_seq(ctx, l, b, sqT, skT, skTn, sv2)
    else:
        zero_o(ctx, T0, 5, 6, 64, 128)
        zero_o(ctx, T0, 6, 8, 0, 128)
    A.release()


def zero_o(ctx, T0, c0, c1, p0, p1):
    A = ctx['A']; POOL = ctx['POOL']; DMA = ctx['DMA']; oT16 = ctx['oT16']
    n = c1 - c0
    zt = A.bf16(n * 512)
    key = ('zt', c0, p0)
    POOL(lambda e: e.memset(zt, 0.0), [], [key])
    for q in range(4):
        DMA(oT16[p0:p1, c0:c1, T0 + q * 512:T0 + (q + 1) * 512], zt[p0:p1, :].rearrange("p (k t) -> p k t", k=n),
            ('st_zt', c0, p0), [key], [('oT16z', c0, p0, q)])


def gla_seq(ctx, l, b, hT, wbf, stage, nb, evac, proj_fm):
    nc = ctx['nc']; P = ctx['P']; A = ctx['A']; ps = ctx['ps']
    bank = ctx['bank']; bankbf = ctx['bankbf']
    MM = ctx['MM']; TR = ctx['TR']; ACT = ctx['ACT']; DVE = ctx['DVE']; POOL = ctx['POOL']; DMA = ctx['DMA']
    cst = ctx['cst']; cbf = ctx['cbf']; ident_bf = ctx['ident_bf']
    load_w = ctx['load_w']; w_in = ctx['w_in']; oT16 = ctx['oT16']
    bcast_row = ctx['bcast_row']; one_t = ctx['one_t']
    gate_w2 = ctx['gate_w2']; gate_b = ctx['gate_b']; norm_g = ctx['norm_g']
    T0 = b * SEQ
    QS = 48 ** -0.5

    def V3(ap, a):
        return ap.rearrange("p (a b) -> p a b", a=a)

    load_w(wbf[:, :, 0:1168], lambda k: w_in[l, k * 128:(k + 1) * 128, 0:1168], 8, 1168, stage, 'win', 'wbf')
    gw2f = A.f32(192); gw2b = A.bf16(192)
    DMA(gw2f[0:16, :], gate_w2[l], 'ld_gw2', [], ['gw2f'])
    POOL(lambda e: e.tensor_copy(gw2b[0:16, :], gw2f[0:16, :]), ['gw2f'], ['gw2b'])
    gb = A.f32(192)
    DMA(gb, bcast_row(gate_b[l:l + 1, :], 192), 'ld_gb', [], ['gb'])
    ng4 = A.f32(384)
    for h in range(4):
        DMA(ng4[:, h * 96:(h + 1) * 96], bcast_row(norm_g[l:l + 1, :], 96), 'ld_ng', [], ['ng4'])
    bt4 = A.f32(512)
    for h in range(4):
        POOL(lambda e, h=h: e.tensor_copy(bt4[:, h * 128:(h + 1) * 128], cst[:, C_BT:C_BT + 128]), ['cst'], ['bt4'])
    glrT = A.bf16(2048)
    proj_fm(768, 16, lambda pb, tc: evac(glrT[0:16, tc * 512:(tc + 1) * 512], ps[0:16, pb * 512:(pb + 1) * 512],
                                         [('ps', pb)], ['glrT']))
    S = A.f32(384); Smid = A.f32(384); S2 = A.bf16(384)
    POOL(lambda e: e.memset(S, 0.0), [], ['S'])
    POOL(lambda e: e.memset(S2, 0.0), [], ['S2'])
    qt2 = A.bf16(512); kt2 = A.bf16(512); kend2 = A.bf16(512); sp2 = A.bf16(512)
    for t_, k_ in ((qt2, 'qt2'), (kt2, 'kt2'), (kend2, 'kend2'), (sp2, 'sp2')):
        POOL(lambda e, t_=t_: e.memset(t_, 0.0), [], [k_])
    qt2v = V3(qt2, 4); kt2v = V3(kt2, 4); kend2v = V3(kend2, 4); sp2v = V3(sp2, 4)
    vbf = A.bf16(384); sg = A.f32(384)
    xb = A.f32(192); ebuf = A.f32(192); spf = A.f32(192); spb = A.bf16(192)
    E1 = A.f32(192); E2 = A.f32(192); E3 = A.f32(192); totS = A.f32(192); dd = A.f32(192)
    dec = A.f32(8); qkT = A.bf16(1024); scm = A.bf16(512)
    ssq = A.f32(4); rstd4 = A.f32(4); junk = A.f32(96); t1 = A.f32(384); og = A.bf16(384)
    oTg = [V3(A.bf16(3 * 512), 3) for _ in range(2)]
    bo_bf = cbf[:, C_BO:C_BO + 128]; bt_bf = cbf[:, C_BT:C_BT + 128]; cs_bf = cbf[:, C_CS:C_CS + 2]

    def h4(ap, r0=0, r1=128):
        return ap[r0:r1, :].rearrange("p (h d) -> p h d", h=4)

    for tt in range(16):
        hk = ('hT', tt // 4)
        tsl = slice(tt * 128, (tt + 1) * 128)
        for k in range(8):
            MM(ps[:, 0:384], hT[:, k, tsl], wbf[:, k, 0:384], k == 0, k == 7, ['wbf', hk], [('ps', 0)])
        for k in range(8):
            MM(ps[:, 512:512 + 384], hT[:, k, tsl], wbf[:, k, 384:768], k == 0, k == 7, ['wbf', hk], [('ps', 1)])
        ACT(vbf, ps[:, 512:512 + 384], AF.Copy, [('ps', 1)], ['vbf'])
        for k in range(8):
            MM(ps[:, 512:512 + 384], hT[:, k, tsl], wbf[:, k, 784:1168], k == 0, k == 7, ['wbf', hk], [('ps', 1)])
        ACT(sg, ps[:, 512:512 + 384], AF.Silu, [('ps', 1)], ['sg'])
        MM(ps[:, 1024:1024 + 192], glrT[0:16, tsl], gw2b[0:16, :], True, True, ['glrT', 'gw2b'], [('ps', 2)])
        DVE(lambda e: e.tensor_tensor(xb, ps[:, 1024:1024 + 192], gb, ALU.add), [('ps', 2), 'gb'], ['xb'])
        ACT(ebuf, xb, AF.Exp, ['xb'], ['ebuf'], scale=-1.0)
        ACT(spf, ebuf, AF.Ln, ['ebuf', 'kc'], ['spf'], bias=one_t)
        POOL(lambda e: e.tensor_copy(spb, spf), ['spf'], ['spb'])
        POOL(lambda e: e.tensor_copy(sp2v[:, :, 0:48], h4(spf)), ['spf'], ['sp2'])
        POOL(lambda e: e.tensor_copy(sp2v[:, :, 64:112], h4(spf)), ['spf'], ['sp2'])
        MM(ps[:, 1024:1024 + 192], bt_bf, spb, True, True, ['spb', 'cbf'], [('ps', 2)])
        MM(ps[:, 1024 + 192:1024 + 384], bo_bf, spb, True, True, ['spb', 'cbf'], [('ps', 2)])
        for h in range(4):
            MM(ps[:, 1536 + 2 * h:1536 + 2 * h + 2], sp2[:, h * 128:(h + 1) * 128], cs_bf, True, True,
               ['sp2', 'cbf'], [('ps', 3)])
        ACT(dec, ps[:, 1536:1536 + 8], AF.Exp, [('ps', 3)], ['dec'], scale=-1.0 / 16)
        cum = ps[:, 1024:1024 + 192]; tot = ps[:, 1024 + 192:1024 + 384]
        ACT(E1, cum, AF.Exp, [('ps', 2)], ['E1'], scale=-1.0 / 16)
        ACT(E2, cum, AF.Exp, [('ps', 2)], ['E2'], scale=1.0 / 16)
        ACT(totS, tot, AF.Copy, [('ps', 2)], ['totS'])
        DVE(lambda e: e.tensor_tensor(dd, cum, totS, ALU.subtract), [('ps', 2), 'totS'], ['dd'])
        ACT(E3, dd, AF.Exp, ['dd'], ['E3'], scale=1.0 / 16)
        qps = ps[:, 0:192]; kps = ps[:, 192:384]
        for half in range(2):
            r0 = 64 * half
            DVE(lambda e, r0=r0, half=half: e.scalar_tensor_tensor(
                qt2v[r0:r0 + 64, :, 64 * half:64 * half + 48], h4(qps, r0, r0 + 64), QS, h4(E1, r0, r0 + 64),
                ALU.mult, ALU.mult), [('ps', 0), 'E1'], ['qt2'])
        for cp in (0, 64):
            DVE(lambda e, cp=cp: e.tensor_tensor(kt2v[:, :, cp:cp + 48], h4(kps), h4(E2), ALU.mult),
                [('ps', 0), 'E2'], ['kt2'])
            DVE(lambda e, cp=cp: e.tensor_tensor(kend2v[:, :, cp:cp + 48], h4(kps), h4(E3), ALU.mult),
                [('ps', 0), 'E3'], ['kend2'])
        pv = bankbf(3)
        for h in range(4):
            TR(pv[:, (2 * h) * 128:(2 * h + 1) * 128], qt2[:, h * 128:(h + 1) * 128], ident_bf, ['qt2', 'cbf'], [('ps', 3)])
            TR(pv[:, (2 * h + 1) * 128:(2 * h + 2) * 128], kt2[:, h * 128:(h + 1) * 128], ident_bf, ['kt2', 'cbf'], [('ps', 3)])
        ACT(qkT, pv, AF.Copy, [('ps', 3)], ['qkT'])
        for h in range(4):
            MM(ps[:, 2048 + h * 128:2048 + (h + 1) * 128], qkT[:, (2 * h + 1) * 128:(2 * h + 2) * 128],
               qkT[:, (2 * h) * 128:(2 * h + 1) * 128], True, True, ['qkT'], [('ps', 4)])
        DVE(lambda e: e.tensor_tensor(scm, ps[:, 2048:2560], bt4, ALU.mult), [('ps', 4), 'bt4'], ['scm'])
        for h in range(4):
            MM(ps[:, 3072 + h * 96:3072 + (h + 1) * 96], kend2[0:64, h * 128:(h + 1) * 128], vbf[0:64, h * 96:(h + 1) * 96],
               True, True, ['kend2', 'vbf'], [('ps', 6)])
        for h in range(4):
            MM(ps[:, 3584 + h * 96:3584 + (h + 1) * 96], kend2[64:128, h * 128:(h + 1) * 128], vbf[64:128, h * 96:(h + 1) * 96],
               True, True, ['kend2', 'vbf'], [('ps', 7)])
        POOL(lambda e: e.tensor_copy(S2[0:48, :], S[0:48, :]), ['S'], ['S2'])
        for h in range(4):
            DVE(lambda e, h=h: e.scalar_tensor_tensor(Smid[:, h * 96:(h + 1) * 96], S[:, h * 96:(h + 1) * 96],
                                                      dec[:, 2 * h:2 * h + 1], ps[:, 3072 + h * 96:3072 + (h + 1) * 96],
                                                      ALU.mult, ALU.add), ['S', 'dec', ('ps', 6)], ['Smid'])
        POOL(lambda e: e.tensor_copy(S2[64:112, :], Smid[64:112, :]), ['Smid'], ['S2'])
        for h in range(4):
            DVE(lambda e, h=h: e.scalar_tensor_tensor(S[:, h * 96:(h + 1) * 96], Smid[:, h * 96:(h + 1) * 96],
                                                      dec[:, 2 * h + 1:2 * h + 2], ps[:, 3584 + h * 96:3584 + (h + 1) * 96],
                                                      ALU.mult, ALU.add), ['Smid', 'dec', ('ps', 7)], ['S'])
        for h in range(4):
            MM(ps[:, 2560 + h * 96:2560 + (h + 1) * 96], scm[:, h * 128:(h + 1) * 128], vbf[:, h * 96:(h + 1) * 96],
               True, False, ['scm', 'vbf'], [('ps', 5)])
            MM(ps[:, 2560 + h * 96:2560 + (h + 1) * 96], qkT[:, (2 * h) * 128:(2 * h + 1) * 128], S2[:, h * 96:(h + 1) * 96],
               False, True, ['qkT', 'S2'], [('ps', 5)])
        POOL(lambda e: e.memset(ssq, 0.0), [], ['ssq'])
        for h in range(4):
            ACT(junk, ps[:, 2560 + h * 96:2560 + (h + 1) * 96], AF.Square, [('ps', 5)], ['junk', 'ssq'], accum=ssq[:, h:h + 1])
        DVE(lambda e: e.tensor_scalar(rstd4, ssq, 1.0 / 96, LN_EPS, ALU.mult, ALU.add), ['ssq'], ['rstd4'])
        ACT(rstd4, rstd4, AF.Ln, ['rstd4'], ['rstd4'])
        ACT(rstd4, rstd4, AF.Exp, ['rstd4'], ['rstd4'], scale=-0.5)
        for h in range(4):
            DVE(lambda e, h=h: e.tensor_scalar(t1[:, h * 96:(h + 1) * 96], ps[:, 2560 + h * 96:2560 + (h + 1) * 96],
                                               rstd4[:, h:h + 1], 1.0, ALU.mult, ALU.mult), [('ps', 5), 'rstd4'], ['t1'])
        POOL(lambda e: e.tensor_tensor(t1, t1, ng4, ALU.mult), ['t1', 'ng4'], ['t1'])
        POOL(lambda e: e.tensor_tensor(og, t1, sg, ALU.mult), ['t1', 'sg'], ['og'])
        pv2 = bankbf(4)
        for c in range(3):
            TR(pv2[:, c * 128:(c + 1) * 128], og[:, c * 128:(c + 1) * 128], ident_bf, ['og', 'cbf'], [('ps', 4)])
        blk = tt // 4; q = tt % 4; s = blk % 2
        ACT(oTg[s][:, :, q * 128:(q + 1) * 128], pv2[:, 0:384].rearrange("p (c t) -> p c t", c=3), AF.Copy,
            [('ps', 4)], [('oTg', s)])
        if q == 3:
            DMA(oT16[:, 0:3, T0 + blk * 512:T0 + (blk + 1) * 512], oTg[s], ('st_oTg', s), [('oTg', s)], [('oT16g', blk)])


def dsa_seq(ctx, l, b, dqT, dkT2, vlat, iqT, ikT2, iw):
    nc = ctx['nc']; P = ctx['P']; A = ctx['A']; ps = ctx['ps']
    bank = ctx['bank']; bankbf = ctx['bankbf']
    MM = ctx['MM']; TR = ctx['TR']; ACT = ctx['ACT']; DVE = ctx['DVE']; POOL = ctx['POOL']; DMA = ctx['DMA']
    cst = ctx['cst']; cbf = ctx['cbf']; ident_bf = ctx['ident_bf']; ones_bf = ctx['ones_bf']
    oT16 = ctx['oT16']; w_uv = ctx['w_uv']; cst_d = ctx['cst_d']
    T0 = b * SEQ

    def V3(ap, a):
        return ap.rearrange("p (a b) -> p a b", a=a)

    A.mark()
    dist = A.f32(2048)
    DMA(dist, cst_d[:, C_DIST:C_DIST + 2048], 'ld_dist', [], ['dist'])
    Th = [A.bf16(2048) for _ in range(5)]
    for h in range(5):
        ACT(Th[h], dist, AF.Exp, ['dist'], [('Th', h)], scale=-SLOPES[h])
    wuvf = A.f32(5 * 64)
    for h in range(5):
        DMA(wuvf[:, h * 64:(h + 1) * 64], w_uv[l, h], 'ld_wuv', [], ['wuvf'])
    wuv2f = A.bf16(5 * 128)
    wuv2 = V3(wuv2f, 5)
    POOL(lambda e: e.memset(wuv2f, 0.0), [], ['wuv2'])
    for h in range(5):
        bs = 64 * (h % 2)
        POOL(lambda e, h=h, bs=bs: e.tensor_copy(wuv2[:, h, bs:bs + 64], wuvf[:, h * 64:(h + 1) * 64]), ['wuvf'], ['wuv2'])
    isc = A.f32(2048)
    rbuf = [A.f32(512) for _ in range(2)]
    thr = A.f32(1)
    KB = 16
    pow2tab = A.f32(KB + 1); w_all = A.f32(KB + 1)
    for k in range(KB + 1):
        POOL(lambda e, k=k: e.memset(pow2tab[:, k:k + 1], 2.0 ** -(k + 1)), [], ['pow2tab'])
    bs_hi = A.f32(1); bs_lo = A.f32(1); bs_w0 = A.f32(1); mid = A.f32(1); cnt = A.f32(1); tt_ = A.f32(1)
    maskrow = A.bf16(2048)
    maskTs = [V3(A.bf16(16 * 512), 16) for _ in range(2)]
    Eb = [A.bf16(512) for _ in range(2)]
    Pm = [A.bf16(512) for _ in range(2)]
    rden = A.f32(512)
    olat = [A.bf16(512) for _ in range(2)]
    oTd = [A.bf16(512) for _ in range(2)]
    negI = cst[:, C_NEGI:C_NEGI + 128]
    ric = [0]

    def gen_isc(tg):
        maskT = maskTs[tg % 2]
        mkey = ('maskT', tg % 2)
        for ti in range(4):
            i = 4 * tg + ti
            ncols = (i + 1) * 128
            tsl = slice(i * 128, (i + 1) * 128)
            for h in range(8):
                hp, bs = h // 2, 64 * (h % 2)
                for sc in range(0, ncols, 512):
                    w = min(512, ncols - sc)
                    pb = ric[0] % 2
                    rb = ric[0] % 2
                    ric[0] += 1
                    MM(ps[:, pb * 512:pb * 512 + w], iqT[bs:bs + 64, hp, tsl], ikT2[bs:bs + 64, sc:sc + w], True, True,
                       ['iqT', 'ikT2'], [('ps', pb)])
                    ACT(rbuf[rb][:, 0:w], ps[:, pb * 512:pb * 512 + w], AF.Relu, [('ps', pb)], [('rbuf', rb)])
                    if h == 0:
                        DVE(lambda e, rb=rb, sc=sc, w=w, i=i, h=h: e.tensor_scalar(
                            isc[:, sc:sc + w], rbuf[rb][:, 0:w], iw[:, i, h:h + 1], 1.0, ALU.mult, ALU.mult),
                            [('rbuf', rb), 'iw'], ['isc'])
                    else:
                        DVE(lambda e, rb=rb, sc=sc, w=w, i=i, h=h: e.scalar_tensor_tensor(
                            isc[:, sc:sc + w], rbuf[rb][:, 0:w], iw[:, i, h:h + 1], isc[:, sc:sc + w], ALU.mult, ALU.add),
                            [('rbuf', rb), 'iw', 'isc'], ['isc'])
                if h == 3:
                    yield
            if i >= 2:
                DVE(lambda e, ncols=ncols: e.tensor_reduce(bs_lo, isc[:, 0:ncols], mybir.AxisListType.X, ALU.min), ['isc'], ['bs_lo'])
            POOL(lambda e, i=i: e.tensor_tensor(isc[:, i * 128:(i + 1) * 128], isc[:, i * 128:(i + 1) * 128], negI, ALU.add),
                 ['isc', 'cst'], ['isc'])
            if i >= 2:
                DVE(lambda e, ncols=ncols: e.tensor_reduce(bs_hi, isc[:, 0:ncols], mybir.AxisListType.X, ALU.max), ['isc'], ['bs_hi'])
                DVE(lambda e: e.tensor_tensor(bs_w0, bs_hi, bs_lo, ALU.subtract), ['bs_hi', 'bs_lo'], ['bs_w0'])
                DVE(lambda e: e.tensor_scalar(w_all, pow2tab, bs_w0, 1.0, ALU.mult, ALU.mult), ['bs_w0', 'pow2tab'], ['w_all'])
                DVE(lambda e: e.tensor_tensor(mid, bs_lo, w_all[:, 0:1], ALU.add), ['bs_lo', 'w_all'], ['mid'])
                for k in range(KB):
                    DVE(lambda e, ncols=ncols: e.tensor_scalar(maskrow[:, 0:ncols], isc[:, 0:ncols], mid, None, ALU.is_ge, ALU.add,
                                                               accum_out=cnt), ['isc', 'mid'], ['maskrow', 'cnt'])
                    DVE(lambda e, k=k: e.scalar_tensor_tensor(tt_, cnt, 255.5, w_all[:, k:k + 1], ALU.is_ge, ALU.mult),
                        ['cnt', 'w_all'], ['tt_'])
                    DVE(lambda e, k=k: e.scalar_tensor_tensor(mid, tt_, w_all[:, k + 1:k + 2], mid, ALU.subtract, ALU.add),
                        ['tt_', 'w_all', 'mid'], ['mid'])
                    if k % 5 == 4:
                        yield
                DVE(lambda e: e.tensor_tensor(thr, mid, w_all[:, KB:KB + 1], ALU.subtract), ['mid', 'w_all'], ['thr'])
            else:
                DVE(lambda e: e.memset(thr, -1.0e29), [], ['thr'])
            DVE(lambda e, ncols=ncols: e.tensor_scalar(maskrow[:, 0:ncols], isc[:, 0:ncols], thr, 1.0, ALU.is_ge, ALU.mult),
                ['isc', 'thr'], ['maskrow'])
            for j0 in range(0, i + 1, 8):
                n = min(8, i + 1 - j0)
                pvm = bankbf(2)
                for jj in range(n):
                    j = j0 + jj
                    TR(pvm[:, jj * 128:(jj + 1) * 128], maskrow[:, j * 128:(j + 1) * 128], ident_bf, ['maskrow', 'cbf'], [('ps', 2)])
                ACT(maskT[:, j0:j0 + n, ti * 128:(ti + 1) * 128], pvm[:, 0:n * 128].rearrange("p (j t) -> p j t", j=n),
                    AF.Copy, [('ps', 2)], [mkey])
            yield

    def gen_att(tg):
        maskT = maskTs[tg % 2]
        mkey = ('maskT', tg % 2)
        jmax = 4 * tg + 3
        for h in range(5):
            hp, bs = h // 2, 64 * (h % 2)
            for j in range(jmax + 1):
                c0 = max(j - 4 * tg, 0) * 128
                w = 512 - c0
                tq0 = 512 * tg + c0
                pb = 3 + (j % 2)
                s = j % 2
                MM(ps[:, pb * 512:pb * 512 + w], dkT2[bs:bs + 64, j * 128:(j + 1) * 128], dqT[bs:bs + 64, hp, tq0:tq0 + w],
                   True, True, ['dkT2', 'dqT'], [('ps', pb)])
                ACT(Eb[s][:, 0:w], ps[:, pb * 512:pb * 512 + w], AF.Exp, [('ps', pb)], [('Eb', s)], scale=0.125)
                POOL(lambda e, s=s, w=w, j=j, c0=c0: e.tensor_tensor(Pm[s][:, 0:w], Eb[s][:, 0:w], maskT[:, j, c0:c0 + w],
                                                                    ALU.mult), [('Eb', s), mkey], [('Pm', s)])
                m0 = tq0 - 128 * j
                POOL(lambda e, s=s, w=w, m0=m0, h=h: e.tensor_tensor(Pm[s][:, 0:w], Pm[s][:, 0:w], Th[h][:, m0:m0 + w],
                                                                    ALU.mult), [('Pm', s), ('Th', h)], [('Pm', s)])
                MM(ps[:, 5 * 512 + c0:5 * 512 + 512], vlat[:, j, :], Pm[s][:, 0:w], j == 0, j == jmax,
                   ['vlat', ('Pm', s)], [('ps', 5)])
                MM(ps[:, 6 * 512 + c0:6 * 512 + 512], ones_bf, Pm[s][:, 0:w], j == 0, j == jmax,
                   ['cbf', ('Pm', s)], [('ps', 6)])
                if j % 4 == 3:
                    yield
            DVE(lambda e: e.reciprocal(rden, bank(6)), [('ps', 6)], ['rden'])
            os_ = h % 2
            DVE(lambda e, os_=os_: e.tensor_tensor(olat[os_], bank(5), rden, ALU.mult), [('ps', 5), 'rden'], [('olat', os_)])
            if h % 2 == 1 or h == 4:
                pr = h // 2
                hs = [h - 1, h] if h % 2 == 1 else [h]
                for n_, hh in enumerate(hs):
                    MM(bank(7), wuv2[:, hh, :], olat[hh % 2], n_ == 0, n_ == len(hs) - 1, ['wuv2', ('olat', hh % 2)], [('ps', 7)])
                od = oTd[pr % 2]
                ACT(od, bank(7), AF.Copy, [('ps', 7)], [('oTd', pr % 2)])
                p1 = 128 if len(hs) == 2 else 64
                DMA(oT16[0:p1, 3 + pr, T0 + tg * 512:T0 + (tg + 1) * 512], od[0:p1, :], ('st_oTd', pr % 2),
                    [('oTd', pr % 2)], [('oT16d', pr, tg)])
            yield

    def interleave(g1, g2):
        a1, a2 = True, True
        while a1 or a2:
            if a1:
                try:
                    next(g1)
                except StopIteration:
                    a1 = False
            if a2:
                try:
                    next(g2)
                except StopIteration:
                    a2 = False

    def empty():
        return
        yield

    interleave(gen_isc(0), empty())
    for tg in range(1, 4):
        interleave(gen_isc(tg), gen_att(tg - 1))
    interleave(empty(), gen_att(3))
    A.release()


def sb_seq(ctx, l, b, sqT, skT, skTn, sv2):
    nc = ctx['nc']; P = ctx['P']; A = ctx['A']; ps = ctx['ps']
    bank = ctx['bank']; bankbf = ctx['bankbf']
    MM = ctx['MM']; TR = ctx['TR']; ACT = ctx['ACT']; DVE = ctx['DVE']; POOL = ctx['POOL']; DMA = ctx['DMA']
    cst = ctx['cst']; cbf = ctx['cbf']; ones_bf = ctx['ones_bf']; tri_bf = ctx['tri_bf']
    oT16 = ctx['oT16']; one_t = ctx['one_t']
    T0 = b * SEQ
    ms_bf = cbf[:, C_MS:C_MS + 128]

    def V3(ap, a):
        return ap.rearrange("p (a b) -> p a b", a=a)

    A.mark()
    wT = [V3(A.bf16(16 * 512), 16) for _ in range(2)]
    Cb = [A.bf16(512) for _ in range(2)]
    ebuf = [[A.f32(512) for _ in range(2)] for _ in range(2)]
    spm = [[A.bf16(512) for _ in range(2)] for _ in range(2)]
    oTs = [A.bf16(512) for _ in range(2)]
    zb = [[0, 1], [6, 7]]

    def step(tg, h, js):
        jmax = 4 * tg + 3
        hp, bs = h // 2, 64 * (h % 2)
        hb = h % 2
        wkey = ('wT', hb)
        c0 = max(js - 4 * tg, 0) * 128
        w = 512 - c0
        tq0 = 512 * tg + c0
        s = js % 2
        pz = zb[hb][s]
        pa = 2 + hb
        eb = ebuf[hb][s]; sp_ = spm[hb][s]
        ke, ks = ('ebuf', hb, s), ('spm', hb, s)
        kc_ = ('Cb', hb)
        ksl = slice(js * 128, (js + 1) * 128)
        MM(ps[:, pz * 512:pz * 512 + w], skT[bs:bs + 64, hp, ksl], sqT[bs:bs + 64, hp, tq0:tq0 + w], True, True,
           ['skT', 'sqT'], [('ps', pz)])
        ACT(eb[:, 0:w], ps[:, pz * 512:pz * 512 + w], AF.Exp, [('ps', pz)], [ke], scale=0.125)
        ACT(sp_[:, 0:w], eb[:, 0:w], AF.Ln, [ke, 'kc'], [ks], bias=one_t)
        diag = js >= 4 * tg
        if diag:
            DVE(lambda e: e.tensor_tensor(sp_[:, 0:128], sp_[:, 0:128], ms_bf, ALU.mult), [ks, 'cbf'], [ks])
        first = (js == jmax)
        MM(ps[:, pa * 512:pa * 512 + w], tri_bf, sp_[:, 0:w], True, False, ['cbf', ks], [('ps', pa)])
        if not first:
            MM(ps[:, pa * 512:pa * 512 + w], ones_bf, Cb[hb][:, c0:c0 + w], False, False, ['cbf', kc_], [('ps', pa)])
        MM(ps[:, pa * 512:pa * 512 + w], skTn[bs:bs + 64, hp, ksl], sqT[bs:bs + 64, hp, tq0:tq0 + w], False, True,
           ['skTn', 'sqT'], [('ps', pa)])
        ACT(wT[hb][:, js, c0:c0 + w], ps[:, pa * 512:pa * 512 + w], AF.Exp, [('ps', pa)], [wkey], scale=-1.0)
        if diag:
            DVE(lambda e: e.tensor_tensor(wT[hb][:, js, c0:c0 + 128], wT[hb][:, js, c0:c0 + 128], ms_bf, ALU.mult),
                 [wkey, 'cbf'], [wkey])
        if js > 0:
            DVE(lambda e: e.tensor_tensor(Cb[hb][:, c0:c0 + w], Cb[hb][:, c0:c0 + w], sp_[:, 0:w], ALU.add),
                 [kc_, ks], [kc_])

    def pv(tg, h):
        jmax = 4 * tg + 3
        hb = h % 2
        wkey = ('wT', hb)
        row = 704 + 64 * h
        chunk = row // 128
        pair_first = (h == 0) or (h % 2 == 1)
        pair_last = (h == 0) or (h % 2 == 0)
        po = 4 + (chunk % 2)
        for js in range(jmax + 1):
            c0 = max(js - 4 * tg, 0) * 128
            w = 512 - c0
            MM(ps[:, po * 512 + c0:po * 512 + 512], sv2[:, js, h, :], wT[hb][:, js, c0:c0 + w],
               pair_first and js == 0, pair_last and js == jmax, ['sv2', wkey], [('ps', po)])
        if pair_last:
            od = oTs[chunk % 2]
            ACT(od, bank(po), AF.Copy, [('ps', po)], [('oTs', chunk % 2)])
            p0 = 64 if h == 0 else 0
            DMA(oT16[p0:128, chunk, T0 + tg * 512:T0 + (tg + 1) * 512], od[p0:128, :], ('st_oTs', chunk % 2),
                [('oTs', chunk % 2)], [('oT16s', chunk, tg)])

    for tg in range(4):
        jmax = 4 * tg + 3
        for hpair in ((0, 1), (2, 3), (4,)):
            for h in hpair:
                POOL(lambda e, h=h: e.memset(Cb[h % 2], 0.0), [], [('Cb', h % 2)])
            for js in range(jmax, -1, -1):
                for h in hpair:
                    step(tg, h, js)
            for h in hpair:
                pv(tg, h)
    A.release()


MIX_PARTS = ('gla', 'dsa', 'sb')

PARAM_NAMES = ['ln_in_g', 'ln_in_b', 'w_in', 'gla_gate_w2', 'gla_gate_b', 'gla_norm_g', 'dsa_w_uv', 'w_out',
               'ln_mix_g', 'ln_mix_b', 'w_mem_q', 'w_mem_kv', 'w_mem_o', 'ln_mem_g', 'ln_mem_b',
               'w_up', 'b_up', 'w_down', 'b_down', 'ln_ffn_g', 'ln_ffn_b']


def make_in_maps(inputs, ncores=8):
    cst = make_consts()
    maps = []
    for c in range(ncores):
        m = {'cst': cst}
        m['x'] = np.ascontiguousarray(inputs['x'][c * NSEQ:(c + 1) * NSEQ]).reshape(TOK, D)
        m['mem'] = np.ascontiguousarray(inputs['mem'][c * NSEQ:(c + 1) * NSEQ]).reshape(NSEQ * MEMT, D)
        for k in PARAM_NAMES:
            a = np.ascontiguousarray(inputs[k], dtype=np.float32)
            if k in ('ln_in_g', 'ln_in_b'):
                a = a.reshape(1, D)
            m[k] = a
        maps.append(m)
    return maps


def kernel(**inputs):
    inputs = {k: np.asarray(v) for k, v in inputs.items()}
    nc, _ = build()
    maps = make_in_maps(inputs, 8)
    res = run_bass_kernel_spmd(nc, maps, core_ids=list(range(8)))
    outs = [r['out'].reshape(NSEQ, SEQ, D) for r in res.results]
    return np.concatenate(outs, axis=0).astype(np.float32)
```
